# Optimizing a Trainium2 kernel written in Bass

```python
import math
import jax, jax.numpy as jnp
from jax import lax
import numpy as np

D_MODEL = 2048
BATCH = 1
SEQ = 8192
DEPTH = 4

BRANCH_WIDTH = D_MODEL // 2
N_BRANCH = 3
ATTN_HEAD_DIM = 128
ATTN_HEADS = BRANCH_WIDTH // ATTN_HEAD_DIM
ATTN_WIDTH = ATTN_HEADS * ATTN_HEAD_DIM
MOBA_BLOCK = 256
MOBA_TOPK = 3
QUERY_CHUNK = 64
GLA_HEADS = 4
GLA_VAL_DIM = BRANCH_WIDTH // GLA_HEADS
GLA_KEY_DIM = GLA_VAL_DIM // 2
GLA_KEY_WIDTH = GLA_HEADS * GLA_KEY_DIM
GLA_VAL_WIDTH = GLA_HEADS * GLA_VAL_DIM
GLA_GATE_RANK = 16
GLA_GATE_TAU = 16.0
GLA_CHUNK = 64
CONV_WIDTH = BRANCH_WIDTH
CONV_K = 3
D_FF = -(-8 * D_MODEL // (3 * 256)) * 256
RMS_EPS = 1e-6
COL_SIZES = (ATTN_WIDTH, ATTN_WIDTH, ATTN_WIDTH,
             GLA_KEY_WIDTH, GLA_KEY_WIDTH, GLA_VAL_WIDTH,
             GLA_VAL_WIDTH, GLA_GATE_RANK,
             CONV_WIDTH, CONV_WIDTH, CONV_WIDTH,
             N_BRANCH * D_MODEL)
N_IN_COLS = sum(COL_SIZES)

kernel_name = "hybrid_moba_gla_shortconv_swiglu"


def rms_norm(x, w):
    xf = x.astype(jnp.float32)
    y = xf * lax.rsqrt(jnp.mean(xf * xf, axis=-1, keepdims=True) + RMS_EPS)
    return (y * w.astype(jnp.float32)).astype(x.dtype)


def moba_attention(q, k, v):
    B_, S, H, dh = q.shape
    f32 = jnp.float32
    nb = -(-S // MOBA_BLOCK)
    s_pad = nb * MOBA_BLOCK
    topk = min(MOBA_TOPK, nb)
    pad = ((0, 0), (0, s_pad - S), (0, 0), (0, 0))
    kp = jnp.pad(k, pad)
    vp = jnp.pad(v, pad)
    kb = kp.reshape(B_, nb, MOBA_BLOCK, H, dh).transpose(0, 3, 1, 2, 4)
    vb = vp.reshape(B_, nb, MOBA_BLOCK, H, dh).transpose(0, 3, 1, 2, 4)
    k_mean = jnp.mean(kb.astype(f32), axis=3)
    slopes = 2.0 ** (-8.0 * jnp.arange(1, H + 1, dtype=f32) / H)
    scale = 1.0 / math.sqrt(dh)
    bi = jnp.arange(B_)[:, None, None, None]
    hi = jnp.arange(H)[None, None, :, None]
    blk_ids = jnp.arange(nb)
    in_blk = jnp.arange(MOBA_BLOCK)

    def one_chunk(c):
        start = c * QUERY_CHUNK
        qf = lax.dynamic_slice_in_dim(q, start, QUERY_CHUNK, axis=1).astype(f32)
        t_pos = start + jnp.arange(QUERY_CHUNK)
        own = start // MOBA_BLOCK
        gate = jnp.einsum("bqhd,bhnd->bqhn", qf, k_mean)
        gate = jnp.where((blk_ids < own)[None, None, None, :], gate, -jnp.inf)
        _, sel = lax.top_k(gate, topk)
        sel_valid = sel < own
        k_sel = kb[bi, hi, sel].astype(f32)
        v_sel = vb[bi, hi, sel].astype(f32)
        s_sel = jnp.einsum("bqhd,bqhnkd->bqhnk", qf, k_sel) * scale
        kpos_sel = sel[..., None] * MOBA_BLOCK + in_blk
        dist_sel = (t_pos[None, :, None, None, None] - kpos_sel).astype(f32)
        s_sel = s_sel - slopes[None, None, :, None, None] * dist_sel
        s_sel = jnp.where(sel_valid[..., None], s_sel, -jnp.inf)
        k_own = lax.dynamic_slice_in_dim(kp, own * MOBA_BLOCK, MOBA_BLOCK, axis=1).astype(f32)
        v_own = lax.dynamic_slice_in_dim(vp, own * MOBA_BLOCK, MOBA_BLOCK, axis=1).astype(f32)
        s_own = jnp.einsum("bqhd,bkhd->bqhk", qf, k_own) * scale
        dist_own = t_pos[:, None] - (own * MOBA_BLOCK + in_blk)[None, :]
        s_own = s_own - slopes[None, None, :, None] * dist_own[None, :, None, :].astype(f32)
        s_own = jnp.where((dist_own >= 0)[None, :, None, :], s_own, -jnp.inf)
        n_sel = topk * MOBA_BLOCK
        p = jax.nn.softmax(jnp.concatenate(
            [s_sel.reshape(B_, QUERY_CHUNK, H, n_sel), s_own], axis=-1), axis=-1)
        p_sel = p[..., :n_sel].reshape(B_, QUERY_CHUNK, H, topk, MOBA_BLOCK)
        out = (jnp.einsum("bqhnk,bqhnkd->bqhd", p_sel, v_sel)
               + jnp.einsum("bqhk,bkhd->bqhd", p[..., n_sel:], v_own))
        return out.astype(q.dtype)

    outs = lax.map(one_chunk, jnp.arange(S // QUERY_CHUNK))
    return outs.transpose(1, 0, 2, 3, 4).reshape(B_, S, H, dh)


def gla_attention(q, k, v, log_a):
    B_, S, H, dk = q.shape
    dv = v.shape[-1]
    f32 = jnp.float32
    n = S // GLA_CHUNK

    def chunked(t):
        return t.astype(f32).reshape(B_, n, GLA_CHUNK, H, t.shape[-1]).transpose(0, 3, 1, 2, 4)

    qc = chunked(q) * (dk ** -0.5)
    kc, vc = chunked(k), chunked(v)
    b = jnp.cumsum(chunked(log_a), axis=3)
    b_last = b[..., -1:, :]
    q_in = qc * jnp.exp(b)
    k_in = kc * jnp.exp(-b)
    k_dec = kc * jnp.exp(b_last - b)
    causal = jnp.tril(jnp.ones((GLA_CHUNK, GLA_CHUNK), dtype=bool))
    a_intra = jnp.where(causal, jnp.einsum("bhncd,bhnsd->bhncs", q_in, k_in), 0.0)
    o_intra = jnp.einsum("bhncs,bhnse->bhnce", a_intra, vc)
    kv = jnp.einsum("bhncd,bhnce->bhnde", k_dec, vc)
    decay = jnp.exp(b_last[..., 0, :])

    def step(state, inp):
        kv_n, d_n = inp
        return d_n[..., None] * state + kv_n, state

    _, s_prev = lax.scan(step, jnp.zeros((B_, H, dk, dv), f32),
                         (kv.transpose(2, 0, 1, 3, 4), decay.transpose(2, 0, 1, 3)))
    s_prev = s_prev.transpose(1, 2, 0, 3, 4)
    o = o_intra + jnp.einsum("bhncd,bhnde->bhnce", q_in, s_prev)
    return o.transpose(0, 2, 3, 1, 4).reshape(B_, S, H, dv)


def short_conv(u, w):
    ch = u.shape[-1]
    return lax.conv_general_dilated(
        u, w[:, None, :].astype(u.dtype), window_strides=(1,), padding=[(CONV_K - 1, 0)],
        dimension_numbers=("NWC", "WIO", "NWC"), feature_group_count=ch)


def setup_inputs(seed: int = 0) -> dict:
    key = jax.random.key(seed)
    ks = jax.random.split(key, 16)
    L, D = DEPTH, D_MODEL
    nrm = lambda k, shape, s: jax.random.normal(k, shape, jnp.float32) * s
    gain = lambda k, shape: 1.0 + 0.02 * jax.random.normal(k, shape, jnp.float32)
    return {
        "x": nrm(ks[0], (BATCH, SEQ, D), 1.0),
        "norm1_w": gain(ks[1], (L, D)),
        "w_in": nrm(ks[2], (L, D, N_IN_COLS), D ** -0.5),
        "gla_w_up": nrm(ks[3], (L, GLA_GATE_RANK, GLA_KEY_WIDTH), GLA_GATE_RANK ** -0.5),
        "gla_b": nrm(ks[4], (L, GLA_KEY_WIDTH), 0.02),
        "gla_norm_w": gain(ks[5], (L, GLA_VAL_DIM)),
        "conv_w": nrm(ks[6], (L, CONV_K, CONV_WIDTH), CONV_K ** -0.5),
        "w_br_attn": nrm(ks[7], (L, ATTN_WIDTH, D), ATTN_WIDTH ** -0.5),
        "w_br_gla": nrm(ks[8], (L, GLA_VAL_WIDTH, D), GLA_VAL_WIDTH ** -0.5),
        "w_br_conv": nrm(ks[9], (L, CONV_WIDTH, D), CONV_WIDTH ** -0.5),
        "w_out": nrm(ks[10], (L, D, D), D ** -0.5),
        "norm2_w": gain(ks[11], (L, D)),
        "w_ffn_gate": nrm(ks[12], (L, D, D_FF), D ** -0.5),
        "w_ffn_up": nrm(ks[13], (L, D, D_FF), D ** -0.5),
        "w_ffn_down": nrm(ks[14], (L, D_FF, D), D_FF ** -0.5),
        "final_norm_w": gain(ks[15], (D,)),
    }


def reference(x, norm1_w, w_in, gla_w_up, gla_b, gla_norm_w, conv_w, w_br_attn, w_br_gla,
              w_br_conv, w_out, norm2_w, w_ffn_gate, w_ffn_up, w_ffn_down, final_norm_w):
    B_, S, D = x.shape
    f32 = jnp.float32
    splits = [int(s) for s in np.cumsum(COL_SIZES)[:-1]]
    for l in range(DEPTH):
        h = rms_norm(x, norm1_w[l])
        proj = h @ w_in[l]
        (q_a, k_a, v_a, q_g, k_g, v_g, r_g, a_down,
         b_gate, c_gate, u_conv, merge) = jnp.split(proj, splits, axis=-1)
        hs_a = (B_, S, ATTN_HEADS, ATTN_HEAD_DIM)
        o_a = moba_attention(q_a.reshape(hs_a), k_a.reshape(hs_a), v_a.reshape(hs_a))
        br_a = o_a.reshape(B_, S, ATTN_WIDTH) @ w_br_attn[l]
        z = (a_down @ gla_w_up[l]).astype(f32) + gla_b[l].astype(f32)
        log_a = jax.nn.log_sigmoid(z) / GLA_GATE_TAU
        hs_k = (B_, S, GLA_HEADS, GLA_KEY_DIM)
        o_g = gla_attention(q_g.reshape(hs_k), k_g.reshape(hs_k),
                            v_g.reshape(B_, S, GLA_HEADS, GLA_VAL_DIM), log_a.reshape(hs_k))
        o_g = o_g * lax.rsqrt(jnp.mean(o_g * o_g, axis=-1, keepdims=True) + RMS_EPS)
        o_g = o_g * gla_norm_w[l].astype(f32)
        o_g = o_g.reshape(B_, S, GLA_VAL_WIDTH) * jax.nn.silu(r_g.astype(f32))
        br_b = o_g.astype(x.dtype) @ w_br_gla[l]
        y_c = b_gate * short_conv(c_gate * u_conv, conv_w[l])
        br_c = y_c @ w_br_conv[l]
        g = jax.nn.sigmoid(merge.astype(f32)).reshape(B_, S, N_BRANCH, D)
        mixed = (g[:, :, 0] * br_a.astype(f32) + g[:, :, 1] * br_b.astype(f32)
                 + g[:, :, 2] * br_c.astype(f32)).astype(x.dtype)
        x = x + mixed @ w_out[l]
        h2 = rms_norm(x, norm2_w[l])
        x = x + (jax.nn.silu(h2 @ w_ffn_gate[l]) * (h2 @ w_ffn_up[l])) @ w_ffn_down[l]
    return rms_norm(x, final_norm_w)
```

```python
import numpy as np
import ml_dtypes
import concourse.bass as bass
import concourse.mybir as mybir
from concourse.bass_utils import run_bass_kernel_spmd

F32 = mybir.dt.float32
BF16 = mybir.dt.bfloat16
AF = mybir.ActivationFunctionType
ALU = mybir.AluOpType
AX = mybir.AxisListType

NCORES = 8
D = 2048
SEQ = 8192
DEPTH = 4
T = SEQ // NCORES
KC = D // 128
NH = T // 512
DFF = 5632
EPS = 1e-6
HEADS = 8
DH = 128
NBLK = 32
GH = 4
GDK = 128
GDV = 256
CH = 64
C_QA, C_KA, C_VA, C_QG, C_KG, C_VG, C_RG, C_AD, C_B, C_C, C_U, C_M = (
    0, 1024, 2048, 3072, 3584, 4096, 5120, 6144, 6160, 7184, 8208, 9232)
NA_COLS = 2048 + 1536 + 16
NB_COLS = 1024 + 512 + 1024 + 3072 + 6144


class Res:
    __slots__ = ("name", "w", "r")

    def __init__(self, name=""):
        self.name = name
        self.w = None
        self.r = {}


class Chan:
    def __init__(self, sch, name):
        self.key = "c_" + name
        self.sem = sch.nc.alloc_semaphore(name=self.key)
        self.cnt = 0
        sch.semobj[self.key] = self.sem


class Sched:
    def __init__(self, nc):
        self.nc = nc
        self.eng = {"pe": nc.tensor, "act": nc.scalar, "dve": nc.vector, "pool": nc.gpsimd,
                    "sp": nc.sync}
        self.semobj = {}
        self.cnt = {}
        self.seen = {k: {} for k in self.eng}
        for k in self.eng:
            self.semobj[k] = nc.alloc_semaphore(name="s_" + k)
            self.cnt[k] = 0
        self.out_tickets = []

    def chan(self, name):
        return Chan(self, name)

    def _wait(self, e, key, val):
        if key == "pe" and e == "pe":
            return
        if self.seen[e].get(key, 0) >= val:
            return
        self.eng[e].wait_ge(self.semobj[key], val)
        self.seen[e][key] = val

    def _deps(self, e, reads, writes):
        for r in reads:
            if r.w is not None:
                self._wait(e, *r.w)
        for w in writes:
            if w.w is not None:
                self._wait(e, *w.w)
            for k, v in w.r.items():
                self._wait(e, k, v)

    def _mark(self, t, reads, writes):
        for r in reads:
            if r.r.get(t[0], 0) < t[1]:
                r.r[t[0]] = t[1]
        for w in writes:
            w.w = t
            w.r = {}

    def op(self, e, fn, reads=(), writes=()):
        self._deps(e, reads, writes)
        ins = fn(self.eng[e])
        self.cnt[e] += 1
        ins.then_inc(self.semobj[e], 1)
        t = (e, self.cnt[e])
        self._mark(t, reads, writes)
        return t

    def dma(self, q, chan, out, in_, reads=(), writes=(), is_output=False):
        self._deps(q, reads, writes)
        ins = self.eng[q].dma_start(out=out, in_=in_)
        chan.cnt += 16
        ins.then_inc(chan.sem, 16)
        t = (chan.key, chan.cnt)
        self._mark(t, reads, writes)
        if is_output:
            self.out_tickets.append(t)
        return t

    def finish(self):
        last = {}
        for k, v in self.out_tickets:
            last[k] = max(last.get(k, 0), v)
        for k, v in last.items():
            self._wait("sp", k, v)


class Ctx:
    def __init__(self, nc):
        self.nc = nc
        self.S = Sched(nc)
        self.stack = []

    def sb(self, name, shape, dt):
        g = self.nc.sbuf_tensor(name, shape, dt)
        t = g.__enter__()
        self.stack.append(g)
        return t

    def ps(self, name, shape, dt):
        g = self.nc.psum_tensor(name, shape, dt)
        t = g.__enter__()
        self.stack.append(g)
        return t

    def close(self):
        for g in reversed(self.stack):
            g.__exit__(None, None, None)
        self.stack = []


def setup_common(cx):
    nc, S = cx.nc, cx.S
    cx.bank = [cx.ps("bank%d" % i, [128, 512], F32) for i in range(8)]
    cx.bankres = [Res("bank%d" % i) for i in range(8)]
    cx.NW = 3
    cx.NWF = 2
    cx.wf = [cx.sb("wf%d" % i, [128, 8, 256], F32) for i in range(cx.NWF)]
    cx.wb = [cx.sb("wb%d" % i, [128, 8, 256], BF16) for i in range(cx.NW)]
    cx.wfres = [Res("wf%d" % i) for i in range(cx.NWF)]
    cx.wbres = [Res("wb%d" % i) for i in range(cx.NW)]
    cx.wchan = [S.chan("w%d" % i) for i in range(cx.NWF)]
    cx.wcount = 0
    cx.gcount = 0
    cx.ones = cx.sb("ones_bf", [128, 128], BF16)
    cx.onesres = Res("ones")
    S.op("pool", lambda e: e.memset(cx.ones[:], 1.0), writes=[cx.onesres])
    cx.epi_flip = 0


def dense_fm(cx, W, col0, ncols, K, act, actres, epi, banks=None, extra=None):
    nc, S = cx.nc, cx.S
    kcs = K // 128
    pieces = [(k0, min(8, kcs - k0)) for k0 in range(0, kcs, 8)]
    ngroups = (ncols + 255) // 256
    for g in range(ngroups):
        gw = min(256, ncols - g * 256)
        if banks is None:
            nsets = len(cx.bank_ids) // 4
            bsel = cx.bank_ids[(cx.gcount % nsets) * 4:(cx.gcount % nsets) * 4 + 4]
        else:
            bsel = banks
        cx.gcount += 1
        nocs = (gw + 127) // 128
        for pi, (k0, kcn) in enumerate(pieces):
            slot = cx.wcount % cx.NW
            fs = cx.wcount % cx.NWF
            cx.wcount += 1
            src = W[k0 * 128:(k0 + kcn) * 128, col0 + g * 256:col0 + g * 256 + gw]
            S.dma("sp", cx.wchan[fs], out=cx.wf[fs][:, :kcn, :gw],
                  in_=src.rearrange("(kc p) n -> p kc n", p=128), writes=[cx.wfres[fs]])
            if cx.wcount % 2 == 0:
                S.op("dve", lambda e: e.tensor_copy(out=cx.wb[slot][:, :kcn, :gw],
                                                    in_=cx.wf[fs][:, :kcn, :gw]),
                     reads=[cx.wfres[fs]], writes=[cx.wbres[slot]])
            else:
                S.op("act", lambda e: e.copy(out=cx.wb[slot][:, :kcn, :gw],
                                             in_=cx.wf[fs][:, :kcn, :gw]),
                     reads=[cx.wfres[fs]], writes=[cx.wbres[slot]])
            for oc in range(nocs):
                cw = min(128, gw - oc * 128)
                for half in range(NH):
                    b = bsel[oc * NH + half]
                    for kc in range(kcn):
                        first = (pi == 0 and kc == 0)
                        last = (pi == len(pieces) - 1 and kc == kcn - 1)
                        S.op("pe", lambda e: e.matmul(
                            cx.bank[b][:cw, :], lhsT=cx.wb[slot][:, kc, oc * 128:oc * 128 + cw],
                            rhs=act[:, k0 + kc, half * 512:(half + 1) * 512],
                            start=first, stop=last),
                            reads=[cx.wbres[slot], actres[k0 + kc] if isinstance(actres, list) else actres],
                            writes=[cx.bankres[b]])
                if extra is not None:
                    xr, xrres, xb, xepi = extra
                    for kc in range(kcn):
                        first = (pi == 0 and kc == 0)
                        last = (pi == len(pieces) - 1 and kc == kcn - 1)
                        S.op("pe", lambda e: e.matmul(
                            cx.bank[xb[oc]][:cw, 0:2],
                            lhsT=cx.wb[slot][:, kc, oc * 128:oc * 128 + cw],
                            rhs=xr[:, k0 + kc, :], start=first, stop=last),
                            reads=[cx.wbres[slot], xrres], writes=[cx.bankres[xb[oc]]])
        for oc in range(nocs):
            cw = min(128, gw - oc * 128)
            for half in range(NH):
                b = bsel[oc * NH + half]
                epi(g * 256 + oc * 128, cw, half, cx.bank[b][:cw, :], cx.bankres[b])
            if extra is not None:
                xr, xrres, xb, xepi = extra
                xepi(g * 256 + oc * 128, cw, cx.bank[xb[oc]][:cw, 0:2], cx.bankres[xb[oc]])


def norm_fm(cx, xsrc, wcol, wres, hT, hres, xring, xres, xchan, sq, sqres, rstd, rstdres, sbanks):
    nc, S = cx.nc, cx.S
    nx = len(xring)
    for c in range(KC):
        sl = c % nx
        S.dma("sp", xchan[sl], out=xring[sl][:], in_=xsrc[c * 128:(c + 1) * 128, :],
              writes=[xres[sl]])
        q = c % 2
        S.op("act", lambda e: e.activation(out=sq[q][:], in_=xring[sl][:], func=AF.Square),
             reads=[xres[sl]], writes=[sqres[q]])
        S.op("dve", lambda e: e.tensor_copy(out=hT[:, c, :], in_=xring[sl][:]),
             reads=[xres[sl]], writes=[hres[c]])
        for half in range(NH):
            b = sbanks[half]
            S.op("pe", lambda e: e.matmul(cx.bank[b][:, :], lhsT=cx.ones[:, :],
                                          rhs=sq[q][:, half * 512:(half + 1) * 512],
                                          start=(c == 0), stop=(c == KC - 1)),
                 reads=[cx.onesres, sqres[q]], writes=[cx.bankres[b]])
    norm_finish(cx, sbanks, rstd, rstdres, D)
    for c in range(KC):
        S.op("dve", lambda e: e.scalar_tensor_tensor(out=hT[:, c, :], in0=hT[:, c, :],
                                                   scalar=wcol[:, c:c + 1], in1=rstd[:, :],
                                                   op0=ALU.mult, op1=ALU.mult),
             reads=[hres[c], wres, rstdres], writes=[hres[c]])


def norm_finish(cx, sbanks, rstd, rstdres, n):
    S = cx.S
    for half in range(NH):
        b = sbanks[half]
        hs = slice(half * 512, (half + 1) * 512)
        S.op("dve", lambda e: e.tensor_scalar(out=rstd[:, hs], in0=cx.bank[b][:, :],
                                              scalar1=1.0 / n, scalar2=EPS, op0=ALU.mult,
                                              op1=ALU.add),
             reads=[cx.bankres[b]], writes=[rstdres])
        S.op("act", lambda e: e.sqrt(out=rstd[:, hs], in_=rstd[:, hs]),
             reads=[rstdres], writes=[rstdres])
        S.op("dve", lambda e: e.reciprocal(out=rstd[:, hs], in_=rstd[:, hs]),
             reads=[rstdres], writes=[rstdres])


def build_A():
    nc = bass.Bass("TRN2", target_bir_lowering=False)
    xT = nc.dram_tensor("xT", [D, T], F32, kind="ExternalInput").ap()
    nw = nc.dram_tensor("nw", [128, KC], F32, kind="ExternalInput").ap()
    wa = nc.dram_tensor("wa", [D, NA_COLS], F32, kind="ExternalInput").ap()
    ya = nc.dram_tensor("ya", [3584, T], BF16, kind="ExternalOutput").ap()
    yad = nc.dram_tensor("yad", [16, T], BF16, kind="ExternalOutput").ap()
    hTo = nc.dram_tensor("hTo", [128, KC, T], BF16, kind="ExternalOutput").ap()
    cx = Ctx(nc)
    S = cx.S
    setup_common(cx)
    cx.bank_ids = [0, 1, 2, 3, 4, 5, 6, 7]
    hT = cx.sb("hT", [128, KC, T], BF16)
    hres = [Res("hT%d" % i) for i in range(KC)]
    wcol = cx.sb("wcol", [128, KC], F32)
    wres = Res("wcol")
    cchan = S.chan("const")
    S.dma("sp", cchan, out=wcol[:], in_=nw[:, :], writes=[wres])
    xring = [cx.sb("xr%d" % i, [128, T], F32) for i in range(3)]
    xres = [Res("xr%d" % i) for i in range(3)]
    xchan = [S.chan("x%d" % i) for i in range(3)]
    sq = [cx.sb("sq%d" % i, [128, T], BF16) for i in range(2)]
    sqres = [Res("sq%d" % i) for i in range(2)]
    rstd = cx.sb("rstd", [128, T], F32)
    rstdres = Res("rstd")
    norm_fm(cx, xT, wcol, wres, hT, hres, xring, xres, xchan, sq, sqres, rstd, rstdres, [6, 7])
    hch = S.chan("hTo")
    for c4 in range(4):
        S.dma("pool", hch, out=hTo[:, c4 * 4:(c4 + 1) * 4, :], in_=hT[:, c4 * 4:(c4 + 1) * 4, :],
              reads=hres[c4 * 4:(c4 + 1) * 4], is_output=True)
    NO = 4
    ost = [cx.sb("ost%d" % i, [128, 512], BF16) for i in range(NO)]
    ores = [Res("ost%d" % i) for i in range(NO)]
    ochan = [S.chan("o%d" % i) for i in range(NO)]
    ostf = cx.sb("ostf", [16, T], BF16)
    ostfres = Res("ostf")
    st = {"n": 0}

    def epi_out(rowbase):
        def epi(c0, cw, half, ps, psres):
            i = st["n"] % NO
            st["n"] += 1
            eng = "act" if (st["n"] % 2) else "dve"
            if eng == "act":
                S.op("act", lambda e: e.copy(out=ost[i][:cw, :], in_=ps), reads=[psres],
                     writes=[ores[i]])
            else:
                S.op("dve", lambda e: e.tensor_copy(out=ost[i][:cw, :], in_=ps), reads=[psres],
                     writes=[ores[i]])
            S.dma("pool", ochan[i], out=ya[rowbase + c0:rowbase + c0 + cw,
                                         half * 512:(half + 1) * 512],
                  in_=ost[i][:cw, :], reads=[ores[i]], is_output=True)
        return epi

    dense_fm(cx, wa, 0, 2048, D, hT, hres, epi_out(0))
    dense_fm(cx, wa, 2048, 1536, D, hT, hres, epi_out(2048))

    def epi_ad(c0, cw, half, ps, psres):
        S.op("act", lambda e: e.copy(out=ostf[:cw, half * 512:(half + 1) * 512], in_=ps),
             reads=[psres], writes=[ostfres])
    dense_fm(cx, wa, 3584, 16, D, hT, hres, epi_ad)
    S.dma("sp", cchan, out=yad[:, :], in_=ostf[:, :], reads=[ostfres], is_output=True)
    S.finish()
    cx.close()
    return nc


class Arena:
    def __init__(self, tile, lo, hi):
        self.t, self.lo, self.hi, self.p = tile, lo, hi, lo

    def take(self, n):
        a = self.p
        self.p += n
        assert self.p <= self.hi, (self.p, self.hi)
        return self.t[:, a:a + n]

    def reset(self):
        self.p = self.lo


def barrier(S, chans):
    for e in S.eng:
        for f in S.eng:
            if f != e and S.cnt[f] > 0:
                S._wait(e, f, S.cnt[f])
        for ch in chans:
            if ch.cnt > 0:
                S._wait(e, ch.key, ch.cnt)


SLOPES = [2.0 ** (-(h + 1)) for h in range(HEADS)]
QK_SCALE = 1.0 / float(np.sqrt(DH))
BIGD = 1.0e6
NEG = -1.0e30


def host_consts():
    cf = np.zeros((128, CF_TOTAL), np.float32)
    k = np.arange(128)[:, None].astype(np.float64)
    for h in range(HEADS):
        for m in range(-3, 64):
            cf[:, CF_BIAS + h * 67 + m + 3] = (SLOPES[h] * (k - 128.0 * m))[:, 0]
    m = np.zeros((8, 32), np.float32)
    own = np.zeros((8, 32), np.float32)
    for qs in range(8):
        m[qs, 28 + qs // 2:] = NEG
        own[qs, 28 + qs // 2] = 1.0
    cf[:, CF_CMASK:CF_CMASK + 256] = m.reshape(1, 256)
    cf[:, CF_OWN:CF_OWN + 256] = own.reshape(1, 256)
    s_ = np.arange(64)[:, None]
    t_ = np.arange(64)[None, :]
    cf[:64, CF_TRI16:CF_TRI16 + 64] = np.where(s_ <= t_, -1.0 / 16.0, 0.0)
    cf[:64, CF_SU16:CF_SU16 + 64] = np.where(s_ > t_, -1.0 / 16.0, 0.0)
    cf[:64, CF_TRI1:CF_TRI1 + 256] = np.tile((s_ <= t_).astype(np.float32), (1, 4))
    cf[:64, CF_NC] = -1.0 / 16.0
    cf[np.arange(128), CF_ID + np.arange(128)] = 1.0
    return cf


def host_masks():
    k = np.arange(128)[:, None]
    q = np.arange(512)[None, :]
    m = np.zeros((128, 4 * 512 + 128), np.float32)
    for i in range(4):
        m[:, i * 512:(i + 1) * 512] = (q >= 128 * i + k)
    m[:, 2048:2176] = (q[:, :128] >= k)
    return m.astype(ml_dtypes.bfloat16)


CF_BIAS = 0
CF_CMASK = 8 * 67
CF_OWN = CF_CMASK + 256
CF_TRI16 = CF_OWN + 256
CF_SU16 = CF_TRI16 + 64
CF_TRI1 = CF_SU16 + 64
CF_NC = CF_TRI1 + 256
CF_ONES = CF_NC + 8
CF_ID = CF_ONES + 128
CF_TOTAL = CF_ID + 128


def attention_phase(cx, qT, qres, KT_d, V_d, oT, ores, ar_b, ar_f, cstf, cstres, ident, blkm, chans,
                    cb16, dbgf=None):
    nc, S = cx.nc, cx.S
    kts = [ar_b.take(8192) for _ in range(2)]
    ktres = [Res("kt%d" % i) for i in range(2)]
    ktchan = [S.chan("kt%d" % i) for i in range(2)]
    NV = 3
    vts = [ar_b.take(16 * 129).rearrange("p (a b) -> p a b", a=16) for _ in range(NV)]
    vres = [Res("v%d" % i) for i in range(NV)]
    vchan = [S.chan("v%d" % i) for i in range(NV)]
    NP = 4
    pts = [ar_b.take(512) for _ in range(NP)]
    pres = [[Res("pt%d_%d" % (i, j)) for j in range(4)] for i in range(NP)]
    kmT = ar_b.take(32)
    kmres = Res("km")
    onorm = [ar_f.take(128) for _ in range(2)]
    onres = [Res("on%d" % i) for i in range(2)]
    chans += ktchan + vchan
    acc = ar_f.take(8 * 129).rearrange("p (a b) -> p a b", a=8)
    accres = [Res("acc%d" % i) for i in range(8)]
    gm = ar_f.take(256)
    gmres = Res("gm")
    sel = ar_f.take(256)
    selres = Res("sel")
    valid = ar_f.take(256)
    mfull = ar_f.take(256)
    mres = Res("mfull")
    top8 = ar_f.take(8)
    top8res = Res("top8")
    kms = ar_f.take(32)
    kmsres = Res("kms")
    rec = ar_f.take(8)
    recres = Res("rec")
    sb_ids = [0, 1]
    ob_ids = [[2, 3], [4, 5]]
    gate_b = 6
    tr_b = 7
    S.op("dve", lambda e: e.tensor_tensor(
        out=mfull.rearrange("p (a b) -> p a b", a=8),
        in0=cstf[:, CF_CMASK:CF_CMASK + 256].rearrange("p (a b) -> p a b", a=8),
        in1=blkm.rearrange("p (a b) -> p a b", a=8), op=ALU.add),
        reads=[cstres], writes=[mres])
    S.op("dve", lambda e: e.tensor_scalar(out=valid, in0=mfull, scalar1=-1.0e29, scalar2=None,
                                          op0=ALU.is_gt), reads=[mres], writes=[mres])
    pcount = 0
    tcount = 0
    vcount = 0
    ocount = 0
    sel2 = [sel, ar_f.take(256)]
    selres2 = [selres, Res("sel1")]

    def gating(h):
        ks = h % 2
        sel = sel2[h % 2]
        selres = selres2[h % 2]
        S.dma("sp", ktchan[ks], out=kts[ks], in_=KT_d[h], writes=[ktres[ks]])
        S.op("dve", lambda e: e.tensor_reduce(out=kms, in_=kts[ks].rearrange("p (a b) -> p a b", a=32),
                                              axis=AX.X, op=ALU.add),
             reads=[ktres[ks]], writes=[kmsres])
        S.op("act", lambda e: e.mul(out=kmT, in_=kms, mul=1.0 / 256.0), reads=[kmsres],
             writes=[kmres])
        for qs in range(8):
            S.op("pe", lambda e: e.matmul(cx.bank[gate_b][:, qs * 32:(qs + 1) * 32],
                                          lhsT=qT[:, h, qs * 128:(qs + 1) * 128], rhs=kmT,
                                          start=True, stop=True),
                 reads=[qres, kmres], writes=[cx.bankres[gate_b]])
        S.op("dve", lambda e: e.tensor_tensor(out=gm, in0=cx.bank[gate_b][:, 0:256], in1=mfull,
                                              op=ALU.add),
             reads=[cx.bankres[gate_b], mres], writes=[gmres])
        for qs in range(8):
            S.op("dve", lambda e: e.max(out=top8, in_=gm[:, qs * 32:(qs + 1) * 32]),
                 reads=[gmres], writes=[top8res])
            S.op("dve", lambda e: e.scalar_tensor_tensor(
                out=sel[:, qs * 32:(qs + 1) * 32], in0=gm[:, qs * 32:(qs + 1) * 32],
                scalar=top8[:, 2:3], in1=valid[:, qs * 32:(qs + 1) * 32], op0=ALU.is_ge,
                op1=ALU.mult), reads=[gmres, top8res, mres], writes=[selres])
        S.op("dve", lambda e: e.tensor_tensor(out=sel, in0=sel, in1=cstf[:, CF_OWN:CF_OWN + 256],
                                              op=ALU.add), reads=[selres, cstres],
             writes=[selres])

    gating(0)
    for h in range(HEADS):
        ks = h % 2
        sel = sel2[h % 2]
        selres = selres2[h % 2]
        if h + 1 < HEADS:
            gating(h + 1)
        for qs in range(8):
            S.op("pool", lambda e: e.memset(acc[:, qs, :], 0.0), writes=[accres[qs]])
        nslope = -SLOPES[h]
        blocks = [(g, s_) for g in range(2) for s_ in range(28 + 2 * g + 2)]
        bstate = {}

        def stage1(bi):
            nonlocal pcount, tcount, vcount, ocount
            g, s_ = blocks[bi]
            qpos0 = 28 * 256 + g * 512
            pis, vsl = [], []
            for kk in range(2):
                kt = 2 * s_ + kk
                if kt % 16 == 0:
                    vs = vcount % NV
                    vcount += 1
                    S.dma("sp", vchan[vs], out=vts[vs], in_=V_d[h, :, kt:kt + 16, :],
                          writes=[vres[vs]])
                    bstate["cur_v"] = vs
                sbk = sb_ids[tcount % 2]
                S.op("pe", lambda e: e.matmul(cx.bank[sbk][:, :],
                                              lhsT=kts[ks][:, kt * 128:(kt + 1) * 128],
                                              rhs=qT[:, h, g * 512:(g + 1) * 512],
                                              start=True, stop=True),
                     reads=[ktres[ks], qres], writes=[cx.bankres[sbk]])
                m0 = (qpos0 - kt * 128) // 128
                tcount += 1
                pi = pcount % NP
                pcount += 1
                if h >= 3:
                    S.op("act", lambda e: e.activation(
                        out=pts[pi], in_=cx.bank[sbk][:, :], func=AF.Exp,
                        bias=cstf[:, CF_BIAS + h * 67 + m0 + 3:CF_BIAS + h * 67 + m0 + 4],
                        scale=QK_SCALE), reads=[cx.bankres[sbk], cstres], writes=pres[pi])
                    if m0 <= 0:
                        i_ = -m0
                        S.op("pool", lambda e: e.tensor_tensor(
                            out=pts[pi], in0=pts[pi], in1=cb16[:, i_ * 512:(i_ + 1) * 512],
                            op=ALU.mult), reads=pres[pi] + [cstres], writes=pres[pi])
                else:
                    for qq in range(4):
                        mq = m0 + qq
                        dst = pts[pi][:, qq * 128:(qq + 1) * 128]
                        if mq < 0:
                            S.op("pool", lambda e: e.memset(dst, 0.0), writes=[pres[pi][qq]])
                            continue
                        S.op("act", lambda e: e.activation(
                            out=dst, in_=cx.bank[sbk][:, qq * 128:(qq + 1) * 128], func=AF.Exp,
                            bias=cstf[:, CF_BIAS + h * 67 + mq + 3:CF_BIAS + h * 67 + mq + 4],
                            scale=QK_SCALE), reads=[cx.bankres[sbk], cstres],
                            writes=[pres[pi][qq]])
                        if mq == 0:
                            S.op("pool", lambda e: e.tensor_tensor(
                                out=dst, in0=dst, in1=cb16[:, 2048:2176], op=ALU.mult),
                                reads=[pres[pi][qq], cstres], writes=[pres[pi][qq]])
                pis.append(pi)
                vsl.append((bstate["cur_v"], kt % 16))
            bstate[bi] = (pis, vsl)

        def stage2(bi):
            nonlocal ocount
            g, s_ = blocks[bi]
            pis, vsl = bstate.pop(bi)
            obs = ob_ids[ocount % 2]
            ocount += 1
            for qq in range(4):
                ob = obs[qq // 2]
                for kk in range(2):
                    S.op("pe", lambda e: e.matmul(
                        cx.bank[ob][:, (qq % 2) * 129:(qq % 2) * 129 + 129],
                        lhsT=pts[pis[kk]][:, qq * 128:(qq + 1) * 128],
                        rhs=vts[vsl[kk][0]][:, vsl[kk][1], :], start=(kk == 0), stop=(kk == 1)),
                        reads=[pres[pis[kk]][qq], vres[vsl[kk][0]]], writes=[cx.bankres[ob]])
            for qq in range(4):
                ob = obs[qq // 2]
                qs = g * 4 + qq
                S.op("dve", lambda e: e.scalar_tensor_tensor(
                    out=acc[:, qs, :], in0=cx.bank[ob][:, (qq % 2) * 129:(qq % 2) * 129 + 129],
                    scalar=sel[:, qs * 32 + s_:qs * 32 + s_ + 1], in1=acc[:, qs, :],
                    op0=ALU.mult, op1=ALU.add),
                    reads=[cx.bankres[ob], selres, accres[qs]], writes=[accres[qs]])

        stage1(0)
        for bi in range(len(blocks)):
            if bi + 1 < len(blocks):
                stage1(bi + 1)
            stage2(bi)
        if dbgf is not None and h == HEADS - 1:
            dch = S.chan("dbgf")
            S.dma("sp", dch, out=dbgf[:, 0:256], in_=sel, reads=[selres], is_output=True)
            S.dma("sp", dch, out=dbgf[:, 256:256 + 1032], in_=acc.rearrange("p a b -> p (a b)"),
                  reads=accres, is_output=True)
            S.dma("sp", dch, out=dbgf[:, 1288:1288 + 256], in_=gm, reads=[gmres], is_output=True)
            S.dma("sp", dch, out=dbgf[:, 1544:1552], in_=top8, reads=[top8res], is_output=True)
            S.dma("sp", dch, out=dbgf[:, 1552:1552 + 32], in_=kms, reads=[kmsres], is_output=True)
        S.op("dve", lambda e: e.reciprocal(out=rec, in_=acc[:, :, 128]),
             reads=accres, writes=[recres])
        for half in range(2):
            for qq in range(4):
                qs = half * 4 + qq
                oi = qs % 2
                S.op("dve", lambda e: e.tensor_scalar(out=onorm[oi], in0=acc[:, qs, 0:128],
                                                      scalar1=rec[:, qs:qs + 1], scalar2=None,
                                                      op0=ALU.mult),
                     reads=[accres[qs], recres], writes=[onres[oi]])
                S.op("pe", lambda e: e.transpose(out=cx.bank[tr_b][:, qq * 128:(qq + 1) * 128],
                                                 in_=onorm[oi], identity=ident),
                     reads=[onres[oi], cstres], writes=[cx.bankres[tr_b]])
            S.op("act", lambda e: e.copy(out=oT[:, h, half * 512:(half + 1) * 512],
                                         in_=cx.bank[tr_b][:, :]),
                 reads=[cx.bankres[tr_b]], writes=[ores])


def build_B(stop_after=None, tail=False):
    nc = bass.Bass("TRN2", target_bir_lowering=False)
    dt = nc.dram_tensor
    xT = dt("xT", [D, T], F32, kind="ExternalInput").ap()
    hTi = dt("hTi", [128, KC, T], BF16, kind="ExternalInput").ap()
    hhi = dt("hhi", [128, KC, 2], BF16, kind="ExternalInput").ap()
    nws = dt("nws", [128, 3 * KC], F32, kind="ExternalInput").ap()
    wb_ = dt("wb", [D, NB_COLS], F32, kind="ExternalInput").ap()
    cst = dt("cst", [128, CF_TOTAL], F32, kind="ExternalInput").ap()
    blk = dt("blk", [128, 256], F32, kind="ExternalInput").ap()
    msk_d = dt("msk", [128, 2176], BF16, kind="ExternalInput").ap()
    KT_d = dt("KT", [HEADS, 128, SEQ], BF16, kind="ExternalInput").ap()
    V_d = dt("VA", [HEADS, 128, 64, 129], BF16, kind="ExternalInput").ap()
    kg_d = dt("kgt", [64, 128, 512], BF16, kind="ExternalInput").ap()
    vg_d = dt("vgt", [64, 128, 1024], BF16, kind="ExternalInput").ap()
    ad_d = dt("adT", [17, SEQ], BF16, kind="ExternalInput").ap()
    wup_d = dt("wup", [17, 512], F32, kind="ExternalInput").ap()
    kgT_d = dt("kgT", [128, 4, T], BF16, kind="ExternalInput").ap()
    gnw_d = dt("gnw", [128, 2], F32, kind="ExternalInput").ap()
    cw_d = dt("cw", [128, 8, 3], F32, kind="ExternalInput").ap()
    wbr = [dt("wbr%d" % i, [1024, D], F32, kind="ExternalInput").ap() for i in range(3)]
    w_out = dt("w_out", [D, D], F32, kind="ExternalInput").ap()
    w_g = dt("w_g", [D, DFF], F32, kind="ExternalInput").ap()
    w_u = dt("w_u", [D, DFF], F32, kind="ExternalInput").ap()
    w_d = dt("w_d", [DFF, D], F32, kind="ExternalInput").ap()
    x1T = dt("x1T", [D, T], F32).ap()
    if stop_after is None:
        x2T = dt("x2T", [D, T], F32, kind="ExternalOutput").ap()
        if tail:
            wan = dt("wan", [D, NA_COLS], F32, kind="ExternalInput").ap()
            ya = dt("ya", [3584, T], BF16, kind="ExternalOutput").ap()
            yad = dt("yad", [16, T], BF16, kind="ExternalOutput").ap()
            hTo = dt("hTo", [128, KC, T], BF16, kind="ExternalOutput").ap()
        else:
            xnT = dt("xnT", [D, T], F32, kind="ExternalOutput").ap()
    else:
        dbg = dt("dbg", [128, 8 * T], BF16, kind="ExternalOutput").ap()
        dbgf = dt("dbgf", [128, 2048], F32, kind="ExternalOutput").ap()
    cx = Ctx(nc)
    S = cx.S
    setup_common(cx)
    cx.bank_ids = [0, 1, 2, 3, 4, 5, 6, 7]
    chans = []
    cchan = S.chan("const")
    cstf = cx.sb("cstf", [128, CF_TOTAL], F32)
    cstres = Res("cst")
    S.dma("sp", cchan, out=cstf[:], in_=cst[:, :], writes=[cstres])
    cb16 = cx.sb("cb16", [128, 2176], BF16)
    S.dma("sp", cchan, out=cb16[:], in_=msk_d[:, :], writes=[cstres])
    blkm = cx.sb("blkm", [128, 256], F32)
    S.dma("sp", cchan, out=blkm[:], in_=blk[:, :], writes=[cstres])
    nwt = cx.sb("nwt", [128, 3 * KC], F32)
    S.dma("sp", cchan, out=nwt[:], in_=nws[:, :], writes=[cstres])
    gnw = cx.sb("gnw_sb", [128, 2], F32)
    S.dma("sp", cchan, out=gnw[:], in_=gnw_d[:, :], writes=[cstres])
    cwt = cx.sb("cwt_sb", [128, 8, 3], F32)
    S.dma("sp", cchan, out=cwt[:], in_=cw_d[:, :, :], writes=[cstres])
    ident = cstf[:, CF_ID:CF_ID + 128]
    xring = [cx.sb("xr%d" % i, [128, T], F32) for i in range(2)]
    xres = [Res("xr%d" % i) for i in range(2)]
    xchan = [S.chan("x%d" % i) for i in range(2)]
    sq = [cx.sb("sq%d" % i, [128, T], BF16) for i in range(2)]
    sqres = [Res("sq%d" % i) for i in range(2)]
    rstd = cx.sb("rstd", [128, T], F32)
    rstdres = Res("rstd")
    hT = cx.sb("hT", [128, KC, T], BF16)
    hres = [Res("hT%d" % i) for i in range(KC)]
    bigb = cx.sb("bigb", [128, 44 * T], BF16)
    fscr = cx.sb("fscr", [128, 6144], F32)
    chans += [cchan] + xchan + cx.wchan

    def rows(a, n):
        return bigb[:, a * T:(a + n) * T]

    oT_a = rows(0, 8).rearrange("p (a b) -> p a b", a=8)
    oares = Res("oT_a")
    qT = rows(8, 8).rearrange("p (a b) -> p a b", a=8)
    qres = Res("qT")
    hich = [S.chan("hTi%d" % i) for i in range(4)]
    chans += hich
    for c4 in range(4):
        S.dma("sp", hich[c4], out=hT[:, c4 * 4:(c4 + 1) * 4, :],
              in_=hTi[:, c4 * 4:(c4 + 1) * 4, :], writes=[hres[c4 * 4]])
        for j_ in range(1, 4):
            hres[c4 * 4 + j_].w = hres[c4 * 4].w
    flip = {"n": 0}

    def evac(out_ap, ps, psres, wres_list):
        flip["n"] += 1
        if flip["n"] % 2:
            S.op("act", lambda e: e.copy(out=out_ap, in_=ps), reads=[psres], writes=wres_list)
        else:
            S.op("dve", lambda e: e.tensor_copy(out=out_ap, in_=ps), reads=[psres],
                 writes=wres_list)

    def epi_q(c0, cw, half, ps, psres):
        evac(qT[:, c0 // 128, half * 512:(half + 1) * 512], ps, psres, [qres])
    dense_fm(cx, wb_, 0, 1024, D, hT, hres, epi_q)
    barrier(S, chans)
    ar_b = Arena(bigb, 16 * T, 44 * T)
    ar_f = Arena(fscr, 0, 6144)
    attention_phase(cx, qT, qres, KT_d, V_d, oT_a, oares, ar_b, ar_f, cstf, cstres, ident, blkm,
                    chans, cb16, dbgf=None)
    barrier(S, chans)
    if stop_after == "attn":
        och = S.chan("dbg")
        S.dma("sp", och, out=dbg[:, :], in_=rows(0, 8), reads=[oares], is_output=True)
        S.finish()
        cx.close()
        return nc
    ogT = rows(8, 8).rearrange("p (a b) -> p a b", a=8)
    ogres = Res("ogT")
    ar_b.reset()
    ar_f.reset()
    qgT = ar_b.take(4 * T).rearrange("p (a b) -> p a b", a=4)
    qgres = Res("qgT")
    kgT = ar_b.take(4 * T).rearrange("p (a b) -> p a b", a=4)
    kgres = Res("kgT")
    wup = ar_f.take(512)[0:17, :]
    wupres = Res("wup")
    gch = S.chan("gla_in")
    chans.append(gch)
    S.dma("sp", gch, out=kgT, in_=kgT_d[:, :, :], writes=[kgres])
    S.dma("sp", gch, out=wup, in_=wup_d[:, :], writes=[wupres])
    for r_ in (kgres, wupres):
        r_.w = (gch.key, gch.cnt)

    def epi_qg(c0, cw, half, ps, psres):
        evac(qgT[:, c0 // 128, half * 512:(half + 1) * 512], ps, psres, [qgres])
    dense_fm(cx, wb_, 1024, 512, D, hT, hres, epi_qg)
    barrier(S, chans)
    gla_phase(cx, qgT, qgres, kgT, kgres, kg_d, vg_d, ad_d, wup, wupres, ogT, ogres, ar_b, ar_f,
              cstf, cstres, chans)
    barrier(S, chans)
    if stop_after == "gla":
        och = S.chan("dbg")
        S.dma("sp", och, out=dbg[:, :], in_=rows(8, 8), reads=[ogres], is_output=True)
        S.finish()
        cx.close()
        return nc
    ar_b.reset()
    ar_f.reset()
    for h in range(GH):
        for half in range(2):
            q_ = half
            S.op("act", lambda e: e.activation(out=sq[q_][:], in_=ogT[:, 2 * h + half, :],
                                               func=AF.Square), reads=[ogres], writes=[sqres[q_]])
            for th in range(NH):
                S.op("pe", lambda e: e.matmul(cx.bank[6 + th][:, :], lhsT=cx.ones[:, :],
                                              rhs=sq[q_][:, th * 512:(th + 1) * 512],
                                              start=(half == 0), stop=(half == 1)),
                     reads=[cx.onesres, sqres[q_]], writes=[cx.bankres[6 + th]])
        norm_finish(cx, [6, 7], rstd, rstdres, GDV)
        for half in range(2):
            S.op("dve", lambda e: e.scalar_tensor_tensor(
                out=ogT[:, 2 * h + half, :], in0=ogT[:, 2 * h + half, :],
                scalar=gnw[:, half:half + 1], in1=rstd[:, :], op0=ALU.mult, op1=ALU.mult),
                reads=[ogres, cstres, rstdres], writes=[ogres])
    gtl = [ar_f.take(512) for _ in range(4)]
    gtres = [Res("gt%d" % i) for i in range(4)]
    gcnt = {"n": 0}

    def epi_rg(c0, cw, half, ps, psres):
        i = gcnt["n"] % 4
        gcnt["n"] += 1
        S.op("act", lambda e: e.activation(out=gtl[i], in_=ps, func=AF.Silu), reads=[psres],
             writes=[gtres[i]])
        dst = ogT[:, c0 // 128, half * 512:(half + 1) * 512]
        S.op("dve", lambda e: e.tensor_tensor(out=dst, in0=dst, in1=gtl[i], op=ALU.mult),
             reads=[gtres[i], ogres], writes=[ogres])
    dense_fm(cx, wb_, 1536, 1024, D, hT, hres, epi_rg)
    barrier(S, chans)
    if stop_after == "glapost":
        och = S.chan("dbg")
        S.dma("sp", och, out=dbg[:, :], in_=rows(8, 8), reads=[ogres], is_output=True)
        S.finish()
        cx.close()
        return nc
    yT = rows(16, 8).rearrange("p (a b) -> p a b", a=8)
    yres = Res("yT")
    cT = rows(24, 8).rearrange("p (a b) -> p a b", a=8)
    cres = Res("cT")
    cu = bigb[:, 32 * T:32 * T + 8 * (T + 2)].rearrange("p (a b) -> p a b", a=8)
    cures = Res("cu")
    ar_f.reset()
    ar_b.p = 41 * T + 64
    hh = ar_b.take(2 * KC).rearrange("p (a b) -> p a b", a=KC)
    hhres = Res("hh")
    hch = S.chan("halo")
    chans.append(hch)
    S.dma("sp", hch, out=hh, in_=hhi[:, :, :], writes=[hhres])
    chalo = ar_f.take(2 * 8).rearrange("p (a b) -> p a b", a=8)
    chres = Res("chalo")

    def epi_c(c0, cw, half, ps, psres):
        evac(cT[:, c0 // 128, half * 512:(half + 1) * 512], ps, psres, [cres])

    def xepi_c(c0, cw, ps, psres):
        S.op("act", lambda e: e.copy(out=chalo[:, c0 // 128, :], in_=ps), reads=[psres],
             writes=[chres])

    def epi_u(c0, cw, half, ps, psres):
        r_ = c0 // 128
        S.op("dve", lambda e: e.tensor_tensor(
            out=cu[:, r_, 2 + half * 512:2 + (half + 1) * 512], in0=ps,
            in1=cT[:, r_, half * 512:(half + 1) * 512], op=ALU.mult),
            reads=[psres, cres], writes=[cures])

    def xepi_u(c0, cw, ps, psres):
        r_ = c0 // 128
        S.op("dve", lambda e: e.tensor_tensor(out=cu[:, r_, 0:2], in0=ps, in1=chalo[:, r_, :],
                                              op=ALU.mult),
             reads=[psres, chres], writes=[cures])
    dense_fm(cx, wb_, 3584, 1024, D, hT, hres, epi_c, banks=[0, 1, 2, 3],
             extra=(hh, hhres, [4, 5], xepi_c))
    dense_fm(cx, wb_, 4608, 1024, D, hT, hres, epi_u, banks=[0, 1, 2, 3],
             extra=(hh, hhres, [4, 5], xepi_u))
    cacc = ar_f.take(T)
    caccres = Res("cacc")
    for r_ in range(8):
        S.op("dve", lambda e: e.tensor_scalar(out=cacc, in0=cu[:, r_, 2:T + 2],
                                              scalar1=cwt[:, r_, 2:3], scalar2=None,
                                              op0=ALU.mult),
             reads=[cures, cstres], writes=[caccres])
        S.op("dve", lambda e: e.scalar_tensor_tensor(out=cacc, in0=cu[:, r_, 1:T + 1],
                                                   scalar=cwt[:, r_, 1:2], in1=cacc,
                                                   op0=ALU.mult, op1=ALU.add),
             reads=[cures, caccres], writes=[caccres])
        S.op("dve", lambda e: e.scalar_tensor_tensor(out=yT[:, r_, :], in0=cu[:, r_, 0:T],
                                                   scalar=cwt[:, r_, 0:1], in1=cacc,
                                                   op0=ALU.mult, op1=ALU.add),
             reads=[cures, caccres], writes=[yres])

    def epi_b(c0, cw, half, ps, psres):
        dst = yT[:, c0 // 128, half * 512:(half + 1) * 512]
        S.op("dve", lambda e: e.tensor_tensor(out=dst, in0=ps, in1=dst, op=ALU.mult),
             reads=[psres, yres], writes=[yres])
    dense_fm(cx, wb_, 2560, 1024, D, hT, hres, epi_b)
    barrier(S, chans)
    if stop_after == "conv":
        och = S.chan("dbg")
        S.dma("sp", och, out=dbg[:, :], in_=rows(16, 8), reads=[yres], is_output=True)
        S.finish()
        cx.close()
        return nc
    mixedT = rows(24, 16).rearrange("p (a b) -> p a b", a=16)
    mixres = Res("mixedT")
    ar_f.reset()
    gt4 = [ar_f.take(512) for _ in range(4)]
    gt4res = [Res("g4_%d" % i) for i in range(4)]
    macc = [ar_f.take(512) for _ in range(4)]
    maccres = [Res("macc%d" % i) for i in range(4)]
    mtmp = [ar_f.take(512) for _ in range(2)]
    mtres = [Res("mtmp%d" % i) for i in range(2)]
    mcnt = {"n": 0}
    branches = [(wbr[0], oT_a, oares), (wbr[1], ogT, ogres), (wbr[2], yT, yres)]
    for jg in range(D // 256):
        for br, (wd, actb, actbres) in enumerate(branches):
            def epi_gate(c0, cw, half, ps, psres):
                i = (c0 // 128) * NH + half
                S.op("act", lambda e: e.activation(out=gt4[i], in_=ps, func=AF.Sigmoid),
                     reads=[psres], writes=[gt4res[i]])
            dense_fm(cx, wb_, 5632 + br * D + jg * 256, 256, D, hT, hres, epi_gate)

            def epi_br(c0, cw, half, ps, psres):
                i = (c0 // 128) * NH + half
                if br == 0:
                    S.op("dve", lambda e: e.tensor_tensor(out=macc[i], in0=ps, in1=gt4[i],
                                                          op=ALU.mult),
                         reads=[psres, gt4res[i]], writes=[maccres[i]])
                else:
                    k_ = mcnt["n"] % 2
                    mcnt["n"] += 1
                    S.op("dve", lambda e: e.tensor_tensor(out=mtmp[k_], in0=ps, in1=gt4[i],
                                                          op=ALU.mult),
                         reads=[psres, gt4res[i]], writes=[mtres[k_]])
                    if br == 1:
                        S.op("pool", lambda e: e.tensor_tensor(out=macc[i], in0=macc[i],
                                                               in1=mtmp[k_], op=ALU.add),
                             reads=[maccres[i], mtres[k_]], writes=[maccres[i]])
                    else:
                        S.op("pool", lambda e: e.tensor_tensor(
                            out=mixedT[:, jg * 2 + c0 // 128, half * 512:(half + 1) * 512],
                            in0=macc[i], in1=mtmp[k_], op=ALU.add),
                            reads=[maccres[i], mtres[k_]], writes=[mixres])
            dense_fm(cx, wd, jg * 256, 256, 1024, actb, actbres, epi_br)
    barrier(S, chans)
    cx.bank_ids = [0, 1, 2, 3, 6, 7, 0, 1, 2, 3, 6, 7]
    ar_f.reset()
    x1s = [ar_f.take(512) for _ in range(3)]
    x1res = [Res("x1s%d" % i) for i in range(3)]
    x1ch = [S.chan("x1o%d" % i) for i in range(3)]
    chans += x1ch
    st7 = {"n": 0, "slot": 0}

    def make_resid_epi(src, dst, want_h, is_out):
        def prefetch(r_):
            sl = r_ % 2
            S.dma("pool", xchan[sl], out=xring[sl][:], in_=src[r_ * 128:(r_ + 1) * 128, :],
                  writes=[xres[sl]])
        prefetch(0)

        def epi(c0, cw, half, ps, psres):
            r_ = c0 // 128
            if half == 0 and r_ + 1 < KC:
                prefetch(r_ + 1)
            sl = r_ % 2
            i = st7["n"] % 3
            st7["n"] += 1
            hs = slice(half * 512, (half + 1) * 512)
            S.op("dve", lambda e: e.tensor_tensor(out=x1s[i], in0=ps, in1=xring[sl][:, hs],
                                                  op=ALU.add),
                 reads=[psres, xres[sl]], writes=[x1res[i]])
            S.dma("pool", x1ch[i], out=dst[r_ * 128:(r_ + 1) * 128, hs], in_=x1s[i],
                  reads=[x1res[i]], is_output=is_out)
            q_ = st7["n"] % 2
            S.op("act", lambda e: e.activation(out=sq[q_][:, 0:512], in_=x1s[i], func=AF.Square),
                 reads=[x1res[i]], writes=[sqres[q_]])
            S.op("pe", lambda e: e.matmul(cx.bank[4 + half][:, :], lhsT=cx.ones[:, :],
                                          rhs=sq[q_][:, 0:512], start=(r_ == 0),
                                          stop=(r_ == KC - 1)),
                 reads=[cx.onesres, sqres[q_]], writes=[cx.bankres[4 + half]])
            if want_h:
                S.op("pool", lambda e: e.tensor_copy(out=hT[:, r_, hs], in_=x1s[i]),
                     reads=[x1res[i]], writes=[hres[r_]])
        return epi
    dense_fm(cx, w_out, 0, D, D, mixedT, mixres, make_resid_epi(xT, x1T, True, False))
    norm_finish(cx, [4, 5], rstd, rstdres, D)
    for c in range(KC):
        S.op("dve", lambda e: e.scalar_tensor_tensor(out=hT[:, c, :], in0=hT[:, c, :],
                                                   scalar=nwt[:, KC + c:KC + c + 1],
                                                   in1=rstd[:, :], op0=ALU.mult, op1=ALU.mult),
             reads=[hres[c], cstres, rstdres], writes=[hres[c]])
    barrier(S, chans)
    actT = bigb[:, :].rearrange("p (a b) -> p a b", a=44)
    actres = Res("actT")
    cx.bank_ids = [0, 1, 2, 3, 4, 5, 6, 7]
    ar_f.reset()
    gs4 = [ar_f.take(512) for _ in range(4)]
    gs4res = [Res("gs4_%d" % i) for i in range(4)]
    for fg in range(DFF // 256):
        def epi_g(c0, cw, half, ps, psres):
            i = (c0 // 128) * NH + half
            S.op("act", lambda e: e.activation(out=gs4[i], in_=ps, func=AF.Silu), reads=[psres],
                 writes=[gs4res[i]])
        dense_fm(cx, w_g, fg * 256, 256, D, hT, hres, epi_g)

        def epi_up(c0, cw, half, ps, psres):
            i = (c0 // 128) * NH + half
            S.op("dve", lambda e: e.tensor_tensor(
                out=actT[:, fg * 2 + c0 // 128, half * 512:(half + 1) * 512], in0=ps, in1=gs4[i],
                op=ALU.mult), reads=[psres, gs4res[i]], writes=[actres])
        dense_fm(cx, w_u, fg * 256, 256, D, hT, hres, epi_up)
    barrier(S, chans)
    cx.bank_ids = [0, 1, 2, 3, 6, 7, 0, 1, 2, 3, 6, 7]
    st7["n"] = 0
    x1s_ = x1s
    dense_fm(cx, w_d, 0, D, DFF, actT, actres, make_resid_epi(x1T, x2T, tail, True))
    norm_finish(cx, [4, 5], rstd, rstdres, D)
    barrier(S, chans)
    if tail:
        for c in range(KC):
            S.op("dve", lambda e: e.scalar_tensor_tensor(out=hT[:, c, :], in0=hT[:, c, :],
                                                       scalar=nwt[:, c:c + 1], in1=rstd[:, :],
                                                       op0=ALU.mult, op1=ALU.mult),
                 reads=[hres[c], cstres, rstdres], writes=[hres[c]])
        hoch = S.chan("hTo")
        for c4 in range(4):
            S.dma("pool", hoch, out=hTo[:, c4 * 4:(c4 + 1) * 4, :],
                  in_=hT[:, c4 * 4:(c4 + 1) * 4, :], reads=hres[c4 * 4:(c4 + 1) * 4],
                  is_output=True)
        cx.bank_ids = [0, 1, 2, 3, 4, 5, 6, 7]
        NO = 4
        ost = [bigb[:, i * 512:(i + 1) * 512] for i in range(NO)]
        ostres = [Res("ost%d" % i) for i in range(NO)]
        ostch = [S.chan("o%d" % i) for i in range(NO)]
        ostf = bigb[0:16, 4 * 512:4 * 512 + T]
        ostfres = Res("ostf")
        stt_ = {"n": 0}

        def epi_out(rowbase):
            def epi(c0, cw, half, ps, psres):
                i = stt_["n"] % NO
                stt_["n"] += 1
                evac(ost[i][:cw, :], ps, psres, [ostres[i]])
                S.dma("pool", ostch[i], out=ya[rowbase + c0:rowbase + c0 + cw,
                                               half * 512:(half + 1) * 512],
                      in_=ost[i][:cw, :], reads=[ostres[i]], is_output=True)
            return epi
        dense_fm(cx, wan, 0, 2048, D, hT, hres, epi_out(0))
        dense_fm(cx, wan, 2048, 1536, D, hT, hres, epi_out(2048))

        def epi_ad(c0, cw, half, ps, psres):
            S.op("act", lambda e: e.copy(out=ostf[:cw, half * 512:(half + 1) * 512], in_=ps),
                 reads=[psres], writes=[ostfres])
        dense_fm(cx, wan, 3584, 16, D, hT, hres, epi_ad)
        S.dma("pool", hoch, out=yad[:, :], in_=ostf, reads=[ostfres], is_output=True)
        S.finish()
        cx.close()
        return nc
    fo = [ar_f.take(T) for _ in range(2)]
    fores = [Res("fo%d" % i) for i in range(2)]
    foch = [S.chan("fo%d" % i) for i in range(2)]
    for c in range(KC):
        sl = c % 2
        S.dma("sp", xchan[sl], out=xring[sl][:], in_=x2T[c * 128:(c + 1) * 128, :],
              writes=[xres[sl]])
        S.op("dve", lambda e: e.scalar_tensor_tensor(out=fo[sl], in0=xring[sl][:],
                                                   scalar=nwt[:, 2 * KC + c:2 * KC + c + 1],
                                                   in1=rstd[:, :], op0=ALU.mult, op1=ALU.mult),
             reads=[xres[sl], cstres, rstdres], writes=[fores[sl]])
        S.dma("sp", foch[sl], out=xnT[c * 128:(c + 1) * 128, :], in_=fo[sl], reads=[fores[sl]],
              is_output=True)
    S.finish()
    cx.close()
    return nc


def gla_phase(cx, qgT, qgres, kgT, kgres, kg_d, vg_d, ad_d, wup, wupres, ogT, ogres, ar_b, ar_f,
              cstf, cstres, chans):
    nc, S = cx.nc, cx.S
    NCHK = 128
    OWN0 = NCHK - T // CH
    k4 = [ar_b.take(2048)[0:64, :].rearrange("p (a b) -> p a b", a=4) for _ in range(2)]
    v4 = [ar_b.take(4096)[0:64, :].rearrange("p (a b) -> p a b", a=4) for _ in range(2)]
    kres4 = [Res("k4_%d" % i) for i in range(2)]
    vres4 = [Res("v4_%d" % i) for i in range(2)]
    adres4 = [Res("ad4_%d" % i) for i in range(2)]
    kch = [S.chan("k4_%d" % i) for i in range(2)]
    vch = [S.chan("v4_%d" % i) for i in range(2)]
    adch = [S.chan("ad4_%d" % i) for i in range(2)]
    kdec = [ar_b.take(512)[0:64, :] for _ in range(2)]
    kdres = [Res("kdec%d" % i) for i in range(2)]
    qin = [ar_b.take(256).rearrange("p (a b) -> p a b", a=4) for _ in range(2)]
    kin = [ar_b.take(256).rearrange("p (a b) -> p a b", a=4) for _ in range(2)]
    qkres = [Res("qk%d" % i) for i in range(2)]
    atm = [ar_b.take(256)[0:64, :].rearrange("p (a b) -> p a b", a=4) for _ in range(2)]
    atres = [Res("atm%d" % i) for i in range(2)]
    Sb = ar_b.take(1024).rearrange("p (a b) -> p a b", a=4)
    sbres = Res("Sb")
    ad4 = [ar_b.take(256)[0:17, :] for _ in range(2)]
    NL = 3
    Lr = [ar_b.take(512)[0:64, :] for _ in range(NL)]
    lres = [Res("L%d" % i) for i in range(NL)]
    et = [ar_f.take(512)[0:64, :] for _ in range(2)]
    etres = [Res("et%d" % i) for i in range(2)]
    cb = ar_b.take(136)
    cbres = Res("cb")
    wupb = ar_b.take(512)[0:17, :]
    E3 = [ar_f.take(512)[0:64, :] for _ in range(2)]
    e3res = [Res("E3_%d" % i) for i in range(2)]
    E1 = [ar_f.take(256).rearrange("p (a b) -> p a b", a=4) for _ in range(2)]
    E2 = [ar_f.take(256).rearrange("p (a b) -> p a b", a=4) for _ in range(2)]
    e12res = [Res("E12_%d" % i) for i in range(2)]
    dec = [ar_f.take(4) for _ in range(2)]
    decres = [Res("dec%d" % i) for i in range(2)]
    St = ar_f.take(1024).rearrange("p (a b) -> p a b", a=4)
    stres = [Res("S%d" % i) for i in range(4)]
    chans += kch + vch + adch
    ZB, RB, BB, OB = 0, 1, 2, 3
    KVB = [[4, 5], [6, 7]]
    S.op("dve", lambda e: e.tensor_copy(out=cb[0:64, 0:128], in_=cstf[0:64, CF_TRI16:CF_TRI16 + 128]),
         reads=[cstres], writes=[cbres])
    S.op("dve", lambda e: e.tensor_copy(out=cb[0:64, 128:129], in_=cstf[0:64, CF_NC:CF_NC + 1]),
         reads=[cstres], writes=[cbres])
    S.op("dve", lambda e: e.tensor_copy(out=wupb, in_=wup), reads=[wupres], writes=[cbres])
    tri16 = cb[0:64, 0:64]
    su16 = cb[0:64, 64:128]
    ncol = cb[0:64, 128:129]
    tri1 = cstf[0:64, CF_TRI1:CF_TRI1 + 256].rearrange("p (a b) -> p a b", a=4)
    for h in range(4):
        S.op("pool", lambda e: e.memset(St[:, h, :], 0.0), writes=[stres[h]])
    S.op("pool", lambda e: e.memset(Sb, 0.0), writes=[sbres])

    def stageA(n):
        g4, j = n // 4, n % 4
        sl = g4 % 2
        if j == 0:
            S.dma("sp", kch[sl], out=k4[sl], in_=kg_d[:, n:n + 4, :], writes=[kres4[sl]])
            S.dma("sp", vch[sl], out=v4[sl], in_=vg_d[:, n:n + 4, :], writes=[vres4[sl]])
            S.dma("sp", adch[sl], out=ad4[sl], in_=ad_d[:, n * CH:(n + 4) * CH],
                  writes=[adres4[sl]])
        S.op("pe", lambda e: e.matmul(cx.bank[ZB][0:64, :], lhsT=ad4[sl][:, j * CH:(j + 1) * CH],
                                      rhs=wupb, start=True, stop=True),
             reads=[adres4[sl], cbres], writes=[cx.bankres[ZB]])
        li = n % NL
        ei = n % 2
        S.op("act", lambda e: e.activation(out=et[ei], in_=cx.bank[ZB][0:64, :], func=AF.Exp,
                                           scale=-1.0), reads=[cx.bankres[ZB]], writes=[etres[ei]])
        S.op("act", lambda e: e.activation(out=Lr[li], in_=et[ei], func=AF.Ln, bias=1.0),
             reads=[etres[ei]], writes=[lres[li]])

    def stageB(n):
        own = n >= OWN0
        g4, j = n // 4, n % 4
        sl = g4 % 2
        li = n % NL
        p2 = n % 2
        S.op("pe", lambda e: e.matmul(cx.bank[RB][0:64, :], lhsT=su16, rhs=Lr[li], start=True,
                                      stop=True),
             reads=[cbres, lres[li]], writes=[cx.bankres[RB]])
        for h in range(4):
            if own:
                S.op("pe", lambda e: e.matmul(cx.bank[BB][:, h * 64:(h + 1) * 64],
                                              lhsT=Lr[li][:, h * 128:(h + 1) * 128], rhs=tri16,
                                              start=True, stop=True),
                     reads=[cbres, lres[li]], writes=[cx.bankres[BB]])
            else:
                S.op("pe", lambda e: e.matmul(cx.bank[BB][:, h:h + 1],
                                              lhsT=Lr[li][:, h * 128:(h + 1) * 128], rhs=ncol,
                                              start=True, stop=True),
                     reads=[cbres, lres[li]], writes=[cx.bankres[BB]])
        S.op("act", lambda e: e.activation(out=E3[p2], in_=cx.bank[RB][0:64, :], func=AF.Exp),
             reads=[cx.bankres[RB]], writes=[e3res[p2]])
        if own:
            S.op("act", lambda e: e.activation(out=E1[p2].rearrange("p a b -> p (a b)"),
                                               in_=cx.bank[BB][:, 0:256], func=AF.Exp),
                 reads=[cx.bankres[BB]], writes=[e12res[p2]])
            S.op("act", lambda e: e.activation(out=E2[p2].rearrange("p a b -> p (a b)"),
                                               in_=cx.bank[BB][:, 0:256], func=AF.Exp, scale=-1.0),
                 reads=[cx.bankres[BB]], writes=[e12res[p2]])
            S.op("act", lambda e: e.copy(out=dec[p2], in_=E1[p2][:, :, 63]),
                 reads=[e12res[p2]], writes=[decres[p2]])
            t0 = (n - OWN0) * CH
            S.op("dve", lambda e: e.scalar_tensor_tensor(
                out=qin[p2], in0=qgT[:, :, t0:t0 + CH], scalar=float(GDK) ** -0.5, in1=E1[p2],
                op0=ALU.mult, op1=ALU.mult), reads=[qgres, e12res[p2]], writes=[qkres[p2]])
            S.op("dve", lambda e: e.tensor_tensor(out=kin[p2], in0=kgT[:, :, t0:t0 + CH],
                                                  in1=E2[p2], op=ALU.mult),
                 reads=[kgres, e12res[p2]], writes=[qkres[p2]])
        else:
            S.op("act", lambda e: e.activation(out=dec[p2], in_=cx.bank[BB][:, 0:4], func=AF.Exp),
                 reads=[cx.bankres[BB]], writes=[decres[p2]])
        S.op("dve", lambda e: e.tensor_tensor(out=kdec[p2], in0=k4[sl][:, j, :], in1=E3[p2],
                                              op=ALU.mult),
             reads=[kres4[sl], e3res[p2]], writes=[kdres[p2]])

    def stageC(n):
        own = n >= OWN0
        g4, j = n // 4, n % 4
        sl = g4 % 2
        p2 = n % 2
        if own:
            t0 = (n - OWN0) * CH
            for h in range(4):
                S.op("pe", lambda e: e.matmul(cx.bank[BB][0:64, 256 + h * 64:256 + (h + 1) * 64],
                                              lhsT=kin[p2][:, h, :], rhs=qin[p2][:, h, :],
                                              start=True, stop=True),
                     reads=[qkres[p2]], writes=[cx.bankres[BB]])
            S.op("dve", lambda e: e.tensor_tensor(
                out=atm[p2], in0=cx.bank[BB][0:64, 256:512].rearrange("p (a b) -> p a b", a=4),
                in1=tri1, op=ALU.mult), reads=[cx.bankres[BB], cstres], writes=[atres[p2]])
            for h in range(4):
                for half in range(2):
                    col = (h * 2 + half) * 64
                    S.op("pe", lambda e: e.matmul(
                        cx.bank[OB][:, col:col + 64],
                        lhsT=v4[sl][:, j, h * 256 + half * 128:h * 256 + (half + 1) * 128],
                        rhs=atm[p2][:, h, :], start=True, stop=False),
                        reads=[vres4[sl], atres[p2]], writes=[cx.bankres[OB]])
                    S.op("pe", lambda e: e.matmul(
                        cx.bank[OB][:, col:col + 64], lhsT=Sb[:, h, half * 128:(half + 1) * 128],
                        rhs=qin[p2][:, h, :], start=False, stop=True),
                        reads=[sbres, qkres[p2]], writes=[cx.bankres[OB]])
            S.op("act", lambda e: e.copy(out=ogT[:, :, t0:t0 + CH],
                                         in_=cx.bank[OB][:, :].rearrange("p (a b) -> p a b", a=8)),
                 reads=[cx.bankres[OB]], writes=[ogres])
        kb = KVB[n % 2]
        for h in range(4):
            b = kb[h // 2]
            S.op("pe", lambda e: e.matmul(cx.bank[b][:, (h % 2) * 256:(h % 2) * 256 + 256],
                                          lhsT=kdec[p2][:, h * 128:(h + 1) * 128],
                                          rhs=v4[sl][:, j, h * 256:(h + 1) * 256],
                                          start=True, stop=True),
                 reads=[kdres[p2], vres4[sl]], writes=[cx.bankres[b]])
        for h in range(4):
            b = kb[h // 2]
            S.op("dve", lambda e: e.scalar_tensor_tensor(
                out=St[:, h, :], in0=St[:, h, :], scalar=dec[p2][:, h:h + 1],
                in1=cx.bank[b][:, (h % 2) * 256:(h % 2) * 256 + 256], op0=ALU.mult, op1=ALU.add),
                reads=[stres[h], decres[p2], cx.bankres[b]], writes=[stres[h]])
        if n >= OWN0 - 1 and n < NCHK - 1:
            S.op("act", lambda e: e.copy(out=Sb.rearrange("p a b -> p (a b)"),
                                         in_=St.rearrange("p a b -> p (a b)")),
                 reads=stres, writes=[sbres])

    for i in range(-2, NCHK):
        if i + 2 < NCHK:
            stageA(i + 2)
        if 0 <= i + 1 < NCHK:
            stageB(i + 1)
        if i >= 0:
            stageC(i)


_PROGS = {}


def _prog(name):
    if name not in _PROGS:
        if name == "A":
            _PROGS[name] = build_A()
        else:
            _PROGS[name] = build_B(tail=(name == "Bt"))
    return _PROGS[name]


def _fm(w):
    w = np.asarray(w, np.float32)
    return np.ascontiguousarray(w.reshape(-1, 128).T)


def _slots(a, c):
    pad = np.zeros((7 * T,) + a.shape[1:], a.dtype)
    return np.concatenate([pad, a], 0)[c * T:c * T + SEQ]


def _wa_cols(wl):
    return np.ascontiguousarray(np.concatenate(
        [wl[:, C_KA:C_QG], wl[:, C_KG:C_RG], wl[:, C_AD:C_B]], 1))


def kernel(x, norm1_w, w_in, gla_w_up, gla_b, gla_norm_w, conv_w, w_br_attn, w_br_gla, w_br_conv,
           w_out, norm2_w, w_ffn_gate, w_ffn_up, w_ffn_down, final_norm_w):
    f32 = np.float32
    bf = ml_dtypes.bfloat16
    x = np.asarray(x, f32)
    xcur = x[0]
    cst = host_consts()
    msk = host_masks()
    cores = list(range(NCORES))
    in_a = [{"xT": np.ascontiguousarray(xcur[c * T:(c + 1) * T].T), "nw": _fm(norm1_w[0]),
             "wa": _wa_cols(np.asarray(w_in[0], f32))} for c in cores]
    prev = run_bass_kernel_spmd(_prog("A"), in_a, core_ids=cores).results
    xn = None
    for l in range(DEPTH):
        last = l == DEPTH - 1
        wl = np.asarray(w_in[l], f32)
        ya = np.concatenate([np.asarray(r["ya"]).T for r in prev], 0)
        ad = np.concatenate([np.asarray(r["yad"]).T for r in prev], 0)
        hts = [np.asarray(r["hTo"]) for r in prev]
        k_a, v_a = ya[:, 0:1024], ya[:, 1024:2048]
        k_g, v_g = ya[:, 2048:2560], ya[:, 2560:3584]
        wbm = np.ascontiguousarray(np.concatenate(
            [wl[:, C_QA:C_KA], wl[:, C_QG:C_KG], wl[:, C_RG:C_AD], wl[:, C_B:C_M], wl[:, C_M:]], 1))
        nxt = norm1_w[l + 1] if not last else final_norm_w
        nws = np.ascontiguousarray(np.concatenate(
            [_fm(nxt), _fm(norm2_w[l]), _fm(final_norm_w)], 1))
        wup = np.ascontiguousarray(np.concatenate(
            [np.asarray(gla_w_up[l], f32), np.asarray(gla_b[l], f32)[None, :]], 0))
        gnw = _fm(gla_norm_w[l])
        cw = np.ascontiguousarray(np.asarray(conv_w[l], f32).T.reshape(8, 128, 3).transpose(1, 0, 2))
        shared = {"nws": nws, "wb": wbm, "cst": cst, "msk": msk, "wup": wup, "gnw": gnw, "cw": cw,
                  "wbr0": np.asarray(w_br_attn[l], f32), "wbr1": np.asarray(w_br_gla[l], f32),
                  "wbr2": np.asarray(w_br_conv[l], f32), "w_out": np.asarray(w_out[l], f32),
                  "w_g": np.asarray(w_ffn_gate[l], f32), "w_u": np.asarray(w_ffn_up[l], f32),
                  "w_d": np.asarray(w_ffn_down[l], f32)}
        if not last:
            shared["wan"] = _wa_cols(np.asarray(w_in[l + 1], f32))
        in_b = []
        for c in cores:
            ks = _slots(k_a, c).reshape(SEQ, HEADS, DH)
            vs = _slots(v_a, c).reshape(SEQ, HEADS, DH)
            vaug = np.concatenate([vs, np.ones((SEQ, HEADS, 1), bf)], -1)
            kgs = _slots(k_g, c)
            vgs = _slots(v_g, c)
            ads = _slots(ad, c)
            blk = np.zeros((NBLK,), f32)
            blk[:max(0, 28 - 4 * c)] = NEG
            hh = np.zeros((128, KC, 2), bf)
            if c > 0:
                hh = np.ascontiguousarray(hts[c - 1][:, :, T - 2:T])
            m = dict(shared)
            m.update({
                "xT": np.ascontiguousarray(xcur[c * T:(c + 1) * T].T), "hTi": hts[c], "hhi": hh,
                "blk": np.ascontiguousarray(np.tile(blk, 8)[None, :].repeat(128, 0)),
                "KT": np.ascontiguousarray(ks.transpose(1, 2, 0)),
                "VA": np.ascontiguousarray(vaug.reshape(64, 128, HEADS, DH + 1).transpose(2, 1, 0, 3)),
                "kgt": np.ascontiguousarray(kgs.reshape(128, CH, 512).transpose(1, 0, 2)),
                "vgt": np.ascontiguousarray(vgs.reshape(128, CH, 1024).transpose(1, 0, 2)),
                "adT": np.ascontiguousarray(np.concatenate([ads.T, np.ones((1, SEQ), bf)], 0)),
                "kgT": np.ascontiguousarray(
                    k_g[c * T:(c + 1) * T].T.reshape(GH, 128, T).transpose(1, 0, 2)),
            })
            in_b.append(m)
        prev = run_bass_kernel_spmd(_prog("Bf" if last else "Bt"), in_b, core_ids=cores).results
        if last:
            xn = np.concatenate([np.asarray(r["xnT"]).T for r in prev], 0)
        else:
            xcur = np.concatenate([np.asarray(r["x2T"]).T for r in prev], 0)
    return np.ascontiguousarray(xn[None].astype(f32))
```

```python
import numpy as np
import ml_dtypes
import concourse.bass as bass
import concourse.mybir as mybir
from concourse.bass_utils import run_bass_kernel_spmd

F32 = mybir.dt.float32
BF16 = mybir.dt.bfloat16
AF = mybir.ActivationFunctionType
ALU = mybir.AluOpType
AX = mybir.AxisListType

NCORES = 8
D = 2048
SEQ = 8192
DEPTH = 4
T = SEQ // NCORES
KC = D // 128
NH = T // 512
DFF = 5632
EPS = 1e-6
HEADS = 8
DH = 128
NBLK = 32
GH = 4
GDK = 128
GDV = 256
CH = 64
C_QA, C_KA, C_VA, C_QG, C_KG, C_VG, C_RG, C_AD, C_B, C_C, C_U, C_M = (
    0, 1024, 2048, 3072, 3584, 4096, 5120, 6144, 6160, 7184, 8208, 9232)
NA_COLS = 2048 + 1536 + 16
NB_COLS = 1024 + 512 + 1024 + 3072 + 6144


class Res:
    __slots__ = ("name", "w", "r")

    def __init__(self, name=""):
        self.name = name
        self.w = None
        self.r = {}


class Chan:
    def __init__(self, sch, name):
        self.key = "c_" + name
        self.sem = sch.nc.alloc_semaphore(name=self.key)
        self.cnt = 0
        sch.semobj[self.key] = self.sem


class Sched:
    def __init__(self, nc):
        self.nc = nc
        self.eng = {"pe": nc.tensor, "act": nc.scalar, "dve": nc.vector, "pool": nc.gpsimd,
                    "sp": nc.sync}
        self.semobj = {}
        self.cnt = {}
        self.seen = {k: {} for k in self.eng}
        for k in self.eng:
            self.semobj[k] = nc.alloc_semaphore(name="s_" + k)
            self.cnt[k] = 0
        self.out_tickets = []

    def chan(self, name):
        return Chan(self, name)

    def _wait(self, e, key, val):
        if key == "pe" and e == "pe":
            return
        if self.seen[e].get(key, 0) >= val:
            return
        self.eng[e].wait_ge(self.semobj[key], val)
        self.seen[e][key] = val

    def _deps(self, e, reads, writes):
        for r in reads:
            if r.w is not None:
                self._wait(e, *r.w)
        for w in writes:
            if w.w is not None:
                self._wait(e, *w.w)
            for k, v in w.r.items():
                self._wait(e, k, v)

    def _mark(self, t, reads, writes):
        for r in reads:
            if r.r.get(t[0], 0) < t[1]:
                r.r[t[0]] = t[1]
        for w in writes:
            w.w = t
            w.r = {}

    def op(self, e, fn, reads=(), writes=()):
        self._deps(e, reads, writes)
        ins = fn(self.eng[e])
        self.cnt[e] += 1
        ins.then_inc(self.semobj[e], 1)
        t = (e, self.cnt[e])
        self._mark(t, reads, writes)
        return t

    def dma(self, q, chan, out, in_, reads=(), writes=(), is_output=False):
        self._deps(q, reads, writes)
        ins = self.eng[q].dma_start(out=out, in_=in_)
        chan.cnt += 16
        ins.then_inc(chan.sem, 16)
        t = (chan.key, chan.cnt)
        self._mark(t, reads, writes)
        if is_output:
            self.out_tickets.append(t)
        return t

    def finish(self):
        last = {}
        for k, v in self.out_tickets:
            last[k] = max(last.get(k, 0), v)
        for k, v in last.items():
            self._wait("sp", k, v)


class Ctx:
    def __init__(self, nc):
        self.nc = nc
        self.S = Sched(nc)
        self.stack = []

    def sb(self, name, shape, dt):
        g = self.nc.sbuf_tensor(name, shape, dt)
        t = g.__enter__()
        self.stack.append(g)
        return t

    def ps(self, name, shape, dt):
        g = self.nc.psum_tensor(name, shape, dt)
        t = g.__enter__()
        self.stack.append(g)
        return t

    def close(self):
        for g in reversed(self.stack):
            g.__exit__(None, None, None)
        self.stack = []


def setup_common(cx):
    nc, S = cx.nc, cx.S
    cx.bank = [cx.ps("bank%d" % i, [128, 512], F32) for i in range(8)]
    cx.bankres = [Res("bank%d" % i) for i in range(8)]
    cx.NW = 3
    cx.NWF = 2
    cx.wf = [cx.sb("wf%d" % i, [128, 8, 256], F32) for i in range(cx.NWF)]
    cx.wb = [cx.sb("wb%d" % i, [128, 8, 256], BF16) for i in range(cx.NW)]
    cx.wfres = [Res("wf%d" % i) for i in range(cx.NWF)]
    cx.wbres = [Res("wb%d" % i) for i in range(cx.NW)]
    cx.wchan = [S.chan("w%d" % i) for i in range(cx.NWF)]
    cx.wcount = 0
    cx.gcount = 0
    cx.ones = cx.sb("ones_bf", [128, 128], BF16)
    cx.onesres = Res("ones")
    S.op("pool", lambda e: e.memset(cx.ones[:], 1.0), writes=[cx.onesres])
    cx.epi_flip = 0


def dense_fm(cx, W, col0, ncols, K, act, actres, epi, banks=None, extra=None):
    nc, S = cx.nc, cx.S
    kcs = K // 128
    pieces = [(k0, min(8, kcs - k0)) for k0 in range(0, kcs, 8)]
    ngroups = (ncols + 255) // 256
    for g in range(ngroups):
        gw = min(256, ncols - g * 256)
        if banks is None:
            nsets = len(cx.bank_ids) // 4
            bsel = cx.bank_ids[(cx.gcount % nsets) * 4:(cx.gcount % nsets) * 4 + 4]
        else:
            bsel = banks
        cx.gcount += 1
        nocs = (gw + 127) // 128
        for pi, (k0, kcn) in enumerate(pieces):
            slot = cx.wcount % cx.NW
            fs = cx.wcount % cx.NWF
            cx.wcount += 1
            src = W[k0 * 128:(k0 + kcn) * 128, col0 + g * 256:col0 + g * 256 + gw]
            S.dma("sp", cx.wchan[fs], out=cx.wf[fs][:, :kcn, :gw],
                  in_=src.rearrange("(kc p) n -> p kc n", p=128), writes=[cx.wfres[fs]])
            if cx.wcount % 2 == 0:
                S.op("dve", lambda e: e.tensor_copy(out=cx.wb[slot][:, :kcn, :gw],
                                                    in_=cx.wf[fs][:, :kcn, :gw]),
                     reads=[cx.wfres[fs]], writes=[cx.wbres[slot]])
            else:
                S.op("act", lambda e: e.copy(out=cx.wb[slot][:, :kcn, :gw],
                                             in_=cx.wf[fs][:, :kcn, :gw]),
                     reads=[cx.wfres[fs]], writes=[cx.wbres[slot]])
            for oc in range(nocs):
                cw = min(128, gw - oc * 128)
                for half in range(NH):
                    b = bsel[oc * NH + half]
                    for kc in range(kcn):
                        first = (pi == 0 and kc == 0)
                        last = (pi == len(pieces) - 1 and kc == kcn - 1)
                        S.op("pe", lambda e: e.matmul(
                            cx.bank[b][:cw, :], lhsT=cx.wb[slot][:, kc, oc * 128:oc * 128 + cw],
                            rhs=act[:, k0 + kc, half * 512:(half + 1) * 512],
                            start=first, stop=last),
                            reads=[cx.wbres[slot], actres[k0 + kc] if isinstance(actres, list) else actres],
                            writes=[cx.bankres[b]])
                if extra is not None:
                    xr, xrres, xb, xepi = extra
                    for kc in range(kcn):
                        first = (pi == 0 and kc == 0)
                        last = (pi == len(pieces) - 1 and kc == kcn - 1)
                        S.op("pe", lambda e: e.matmul(
                            cx.bank[xb[oc]][:cw, 0:2],
                            lhsT=cx.wb[slot][:, kc, oc * 128:oc * 128 + cw],
                            rhs=xr[:, k0 + kc, :], start=first, stop=last),
                            reads=[cx.wbres[slot], xrres], writes=[cx.bankres[xb[oc]]])
        for oc in range(nocs):
            cw = min(128, gw - oc * 128)
            for half in range(NH):
                b = bsel[oc * NH + half]
                epi(g * 256 + oc * 128, cw, half, cx.bank[b][:cw, :], cx.bankres[b])
            if extra is not None:
                xr, xrres, xb, xepi = extra
                xepi(g * 256 + oc * 128, cw, cx.bank[xb[oc]][:cw, 0:2], cx.bankres[xb[oc]])


def norm_fm(cx, xsrc, wcol, wres, hT, hres, xring, xres, xchan, sq, sqres, rstd, rstdres, sbanks):
    nc, S = cx.nc, cx.S
    nx = len(xring)
    for c in range(KC):
        sl = c % nx
        S.dma("sp", xchan[sl], out=xring[sl][:], in_=xsrc[c * 128:(c + 1) * 128, :],
              writes=[xres[sl]])
        q = c % 2
        S.op("act", lambda e: e.activation(out=sq[q][:], in_=xring[sl][:], func=AF.Square),
             reads=[xres[sl]], writes=[sqres[q]])
        S.op("dve", lambda e: e.tensor_copy(out=hT[:, c, :], in_=xring[sl][:]),
             reads=[xres[sl]], writes=[hres[c]])
        for half in range(NH):
            b = sbanks[half]
            S.op("pe", lambda e: e.matmul(cx.bank[b][:, :], lhsT=cx.ones[:, :],
                                          rhs=sq[q][:, half * 512:(half + 1) * 512],
                                          start=(c == 0), stop=(c == KC - 1)),
                 reads=[cx.onesres, sqres[q]], writes=[cx.bankres[b]])
    norm_finish(cx, sbanks, rstd, rstdres, D)
    for c in range(KC):
        S.op("dve", lambda e: e.scalar_tensor_tensor(out=hT[:, c, :], in0=hT[:, c, :],
                                                   scalar=wcol[:, c:c + 1], in1=rstd[:, :],
                                                   op0=ALU.mult, op1=ALU.mult),
             reads=[hres[c], wres, rstdres], writes=[hres[c]])


def norm_finish(cx, sbanks, rstd, rstdres, n):
    S = cx.S
    for half in range(NH):
        b = sbanks[half]
        hs = slice(half * 512, (half + 1) * 512)
        S.op("dve", lambda e: e.tensor_scalar(out=rstd[:, hs], in0=cx.bank[b][:, :],
                                              scalar1=1.0 / n, scalar2=EPS, op0=ALU.mult,
                                              op1=ALU.add),
             reads=[cx.bankres[b]], writes=[rstdres])
        S.op("act", lambda e: e.sqrt(out=rstd[:, hs], in_=rstd[:, hs]),
             reads=[rstdres], writes=[rstdres])
        S.op("dve", lambda e: e.reciprocal(out=rstd[:, hs], in_=rstd[:, hs]),
             reads=[rstdres], writes=[rstdres])


def build_A():
    nc = bass.Bass("TRN2", target_bir_lowering=False)
    xT = nc.dram_tensor("xT", [D, T], F32, kind="ExternalInput").ap()
    nw = nc.dram_tensor("nw", [128, KC], F32, kind="ExternalInput").ap()
    wa = nc.dram_tensor("wa", [D, NA_COLS], F32, kind="ExternalInput").ap()
    ya = nc.dram_tensor("ya", [3584, T], BF16, kind="ExternalOutput").ap()
    yad = nc.dram_tensor("yad", [16, T], BF16, kind="ExternalOutput").ap()
    hTo = nc.dram_tensor("hTo", [128, KC, T], BF16, kind="ExternalOutput").ap()
    cx = Ctx(nc)
    S = cx.S
    setup_common(cx)
    cx.bank_ids = [0, 1, 2, 3, 4, 5, 6, 7]
    hT = cx.sb("hT", [128, KC, T], BF16)
    hres = [Res("hT%d" % i) for i in range(KC)]
    wcol = cx.sb("wcol", [128, KC], F32)
    wres = Res("wcol")
    cchan = S.chan("const")
    S.dma("sp", cchan, out=wcol[:], in_=nw[:, :], writes=[wres])
    xring = [cx.sb("xr%d" % i, [128, T], F32) for i in range(3)]
    xres = [Res("xr%d" % i) for i in range(3)]
    xchan = [S.chan("x%d" % i) for i in range(3)]
    sq = [cx.sb("sq%d" % i, [128, T], BF16) for i in range(2)]
    sqres = [Res("sq%d" % i) for i in range(2)]
    rstd = cx.sb("rstd", [128, T], F32)
    rstdres = Res("rstd")
    norm_fm(cx, xT, wcol, wres, hT, hres, xring, xres, xchan, sq, sqres, rstd, rstdres, [6, 7])
    hch = S.chan("hTo")
    for c4 in range(4):
        S.dma("pool", hch, out=hTo[:, c4 * 4:(c4 + 1) * 4, :], in_=hT[:, c4 * 4:(c4 + 1) * 4, :],
              reads=hres[c4 * 4:(c4 + 1) * 4], is_output=True)
    NO = 4
    ost = [cx.sb("ost%d" % i, [128, 512], BF16) for i in range(NO)]
    ores = [Res("ost%d" % i) for i in range(NO)]
    ochan = [S.chan("o%d" % i) for i in range(NO)]
    ostf = cx.sb("ostf", [16, T], BF16)
    ostfres = Res("ostf")
    st = {"n": 0}

    def epi_out(rowbase):
        def epi(c0, cw, half, ps, psres):
            i = st["n"] % NO
            st["n"] += 1
            eng = "act" if (st["n"] % 2) else "dve"
            if eng == "act":
                S.op("act", lambda e: e.copy(out=ost[i][:cw, :], in_=ps), reads=[psres],
                     writes=[ores[i]])
            else:
                S.op("dve", lambda e: e.tensor_copy(out=ost[i][:cw, :], in_=ps), reads=[psres],
                     writes=[ores[i]])
            S.dma("pool", ochan[i], out=ya[rowbase + c0:rowbase + c0 + cw,
                                         half * 512:(half + 1) * 512],
                  in_=ost[i][:cw, :], reads=[ores[i]], is_output=True)
        return epi

    dense_fm(cx, wa, 0, 2048, D, hT, hres, epi_out(0))
    dense_fm(cx, wa, 2048, 1536, D, hT, hres, epi_out(2048))

    def epi_ad(c0, cw, half, ps, psres):
        S.op("act", lambda e: e.copy(out=ostf[:cw, half * 512:(half + 1) * 512], in_=ps),
             reads=[psres], writes=[ostfres])
    dense_fm(cx, wa, 3584, 16, D, hT, hres, epi_ad)
    S.dma("sp", cchan, out=yad[:, :], in_=ostf[:, :], reads=[ostfres], is_output=True)
    S.finish()
    cx.close()
    return nc


class Arena:
    def __init__(self, tile, lo, hi):
        self.t, self.lo, self.hi, self.p = tile, lo, hi, lo

    def take(self, n):
        a = self.p
        self.p += n
        assert self.p <= self.hi, (self.p, self.hi)
        return self.t[:, a:a + n]

    def reset(self):
        self.p = self.lo


def barrier(S, chans):
    for e in S.eng:
        for f in S.eng:
            if f != e and S.cnt[f] > 0:
                S._wait(e, f, S.cnt[f])
        for ch in chans:
            if ch.cnt > 0:
                S._wait(e, ch.key, ch.cnt)


SLOPES = [2.0 ** (-(h + 1)) for h in range(HEADS)]
QK_SCALE = 1.0 / float(np.sqrt(DH))
BIGD = 1.0e6
NEG = -1.0e30


def host_consts():
    cf = np.zeros((128, CF_TOTAL), np.float32)
    k = np.arange(128)[:, None].astype(np.float64)
    for h in range(HEADS):
        for m in range(-3, 64):
            cf[:, CF_BIAS + h * 67 + m + 3] = (SLOPES[h] * (k - 128.0 * m))[:, 0]
    m = np.zeros((8, 32), np.float32)
    own = np.zeros((8, 32), np.float32)
    for qs in range(8):
        m[qs, 28 + qs // 2:] = NEG
        own[qs, 28 + qs // 2] = 1.0
    cf[:, CF_CMASK:CF_CMASK + 256] = m.reshape(1, 256)
    cf[:, CF_OWN:CF_OWN + 256] = own.reshape(1, 256)
    s_ = np.arange(64)[:, None]
    t_ = np.arange(64)[None, :]
    cf[:64, CF_TRI16:CF_TRI16 + 64] = np.where(s_ <= t_, -1.0 / 16.0, 0.0)
    cf[:64, CF_SU16:CF_SU16 + 64] = np.where(s_ > t_, -1.0 / 16.0, 0.0)
    cf[:64, CF_TRI1:CF_TRI1 + 256] = np.tile((s_ <= t_).astype(np.float32), (1, 4))
    cf[:64, CF_NC] = -1.0 / 16.0
    cf[np.arange(128), CF_ID + np.arange(128)] = 1.0
    return cf


def host_masks():
    k = np.arange(128)[:, None]
    q = np.arange(512)[None, :]
    m = np.zeros((128, 4 * 512 + 128), np.float32)
    for i in range(4):
        m[:, i * 512:(i + 1) * 512] = (q >= 128 * i + k)
    m[:, 2048:2176] = (q[:, :128] >= k)
    return m.astype(ml_dtypes.bfloat16)


CF_BIAS = 0
CF_CMASK = 8 * 67
CF_OWN = CF_CMASK + 256
CF_TRI16 = CF_OWN + 256
CF_SU16 = CF_TRI16 + 64
CF_TRI1 = CF_SU16 + 64
CF_NC = CF_TRI1 + 256
CF_ONES = CF_NC + 8
CF_ID = CF_ONES + 128
CF_TOTAL = CF_ID + 128


def attention_phase(cx, qT, qres, KT_d, V_d, oT, ores, ar_b, ar_f, cstf, cstres, ident, blkm, chans,
                    cb16, dbgf=None):
    nc, S = cx.nc, cx.S
    kts = [ar_b.take(8192) for _ in range(2)]
    ktres = [Res("kt%d" % i) for i in range(2)]
    ktchan = [S.chan("kt%d" % i) for i in range(2)]
    NV = 3
    vts = [ar_b.take(16 * 129).rearrange("p (a b) -> p a b", a=16) for _ in range(NV)]
    vres = [Res("v%d" % i) for i in range(NV)]
    vchan = [S.chan("v%d" % i) for i in range(NV)]
    NP = 4
    pts = [ar_b.take(512) for _ in range(NP)]
    pres = [[Res("pt%d_%d" % (i, j)) for j in range(4)] for i in range(NP)]
    kmT = ar_b.take(32)
    kmres = Res("km")
    onorm = [ar_f.take(128) for _ in range(2)]
    onres = [Res("on%d" % i) for i in range(2)]
    chans += ktchan + vchan
    acc = ar_f.take(8 * 129).rearrange("p (a b) -> p a b", a=8)
    accres = [Res("acc%d" % i) for i in range(8)]
    gm = ar_f.take(256)
    gmres = Res("gm")
    sel = ar_f.take(256)
    selres = Res("sel")
    valid = ar_f.take(256)
    mfull = ar_f.take(256)
    mres = Res("mfull")
    top8 = ar_f.take(8)
    top8res = Res("top8")
    kms = ar_f.take(32)
    kmsres = Res("kms")
    rec = ar_f.take(8)
    recres = Res("rec")
    sb_ids = [0, 1]
    ob_ids = [[2, 3], [4, 5]]
    gate_b = 6
    tr_b = 7
    S.op("dve", lambda e: e.tensor_tensor(
        out=mfull.rearrange("p (a b) -> p a b", a=8),
        in0=cstf[:, CF_CMASK:CF_CMASK + 256].rearrange("p (a b) -> p a b", a=8),
        in1=blkm.rearrange("p (a b) -> p a b", a=8), op=ALU.add),
        reads=[cstres], writes=[mres])
    S.op("dve", lambda e: e.tensor_scalar(out=valid, in0=mfull, scalar1=-1.0e29, scalar2=None,
                                          op0=ALU.is_gt), reads=[mres], writes=[mres])
    pcount = 0
    tcount = 0
    vcount = 0
    ocount = 0
    sel2 = [sel, ar_f.take(256)]
    selres2 = [selres, Res("sel1")]

    def gating(h):
        ks = h % 2
        sel = sel2[h % 2]
        selres = selres2[h % 2]
        S.dma("sp", ktchan[ks], out=kts[ks], in_=KT_d[h], writes=[ktres[ks]])
        S.op("dve", lambda e: e.tensor_reduce(out=kms, in_=kts[ks].rearrange("p (a b) -> p a b", a=32),
                                              axis=AX.X, op=ALU.add),
             reads=[ktres[ks]], writes=[kmsres])
        S.op("act", lambda e: e.mul(out=kmT, in_=kms, mul=1.0 / 256.0), reads=[kmsres],
             writes=[kmres])
        for qs in range(8):
            S.op("pe", lambda e: e.matmul(cx.bank[gate_b][:, qs * 32:(qs + 1) * 32],
                                          lhsT=qT[:, h, qs * 128:(qs + 1) * 128], rhs=kmT,
                                          start=True, stop=True),
                 reads=[qres, kmres], writes=[cx.bankres[gate_b]])
        S.op("dve", lambda e: e.tensor_tensor(out=gm, in0=cx.bank[gate_b][:, 0:256], in1=mfull,
                                              op=ALU.add),
             reads=[cx.bankres[gate_b], mres], writes=[gmres])
        for qs in range(8):
            S.op("dve", lambda e: e.max(out=top8, in_=gm[:, qs * 32:(qs + 1) * 32]),
                 reads=[gmres], writes=[top8res])
            S.op("dve", lambda e: e.scalar_tensor_tensor(
                out=sel[:, qs * 32:(qs + 1) * 32], in0=gm[:, qs * 32:(qs + 1) * 32],
                scalar=top8[:, 2:3], in1=valid[:, qs * 32:(qs + 1) * 32], op0=ALU.is_ge,
                op1=ALU.mult), reads=[gmres, top8res, mres], writes=[selres])
        S.op("dve", lambda e: e.tensor_tensor(out=sel, in0=sel, in1=cstf[:, CF_OWN:CF_OWN + 256],
                                              op=ALU.add), reads=[selres, cstres],
             writes=[selres])

    gating(0)
    for h in range(HEADS):
        ks = h % 2
        sel = sel2[h % 2]
        selres = selres2[h % 2]
        if h + 1 < HEADS:
            gating(h + 1)
        for qs in range(8):
            S.op("pool", lambda e: e.memset(acc[:, qs, :], 0.0), writes=[accres[qs]])
        nslope = -SLOPES[h]
        blocks = [(g, s_) for g in range(2) for s_ in range(28 + 2 * g + 2)]
        bstate = {}

        def stage1(bi):
            nonlocal pcount, tcount, vcount, ocount
            g, s_ = blocks[bi]
            qpos0 = 28 * 256 + g * 512
            pis, vsl = [], []
            for kk in range(2):
                kt = 2 * s_ + kk
                if kt % 16 == 0:
                    vs = vcount % NV
                    vcount += 1
                    S.dma("sp", vchan[vs], out=vts[vs], in_=V_d[h, :, kt:kt + 16, :],
                          writes=[vres[vs]])
                    bstate["cur_v"] = vs
                sbk = sb_ids[tcount % 2]
                S.op("pe", lambda e: e.matmul(cx.bank[sbk][:, :],
                                              lhsT=kts[ks][:, kt * 128:(kt + 1) * 128],
                                              rhs=qT[:, h, g * 512:(g + 1) * 512],
                                              start=True, stop=True),
                     reads=[ktres[ks], qres], writes=[cx.bankres[sbk]])
                m0 = (qpos0 - kt * 128) // 128
                tcount += 1
                pi = pcount % NP
                pcount += 1
                if h >= 3:
                    S.op("act", lambda e: e.activation(
                        out=pts[pi], in_=cx.bank[sbk][:, :], func=AF.Exp,
                        bias=cstf[:, CF_BIAS + h * 67 + m0 + 3:CF_BIAS + h * 67 + m0 + 4],
                        scale=QK_SCALE), reads=[cx.bankres[sbk], cstres], writes=pres[pi])
                    if m0 <= 0:
                        i_ = -m0
                        S.op("pool", lambda e: e.tensor_tensor(
                            out=pts[pi], in0=pts[pi], in1=cb16[:, i_ * 512:(i_ + 1) * 512],
                            op=ALU.mult), reads=pres[pi] + [cstres], writes=pres[pi])
                elif h >= 1:
                    for hq in range(2):
                        mh = m0 + 2 * hq
                        dst = pts[pi][:, hq * 256:(hq + 1) * 256]
                        rr = pres[pi][2 * hq:2 * hq + 2]
                        if mh < -1:
                            S.op("pool", lambda e: e.memset(dst, 0.0), writes=rr)
                            continue
                        S.op("act", lambda e: e.activation(
                            out=dst, in_=cx.bank[sbk][:, hq * 256:(hq + 1) * 256], func=AF.Exp,
                            bias=cstf[:, CF_BIAS + h * 67 + mh + 3:CF_BIAS + h * 67 + mh + 4],
                            scale=QK_SCALE), reads=[cx.bankres[sbk], cstres], writes=rr)
                        if mh <= 0:
                            i_ = -mh
                            S.op("pool", lambda e: e.tensor_tensor(
                                out=dst, in0=dst, in1=cb16[:, i_ * 512:i_ * 512 + 256],
                                op=ALU.mult), reads=rr + [cstres], writes=rr)
                else:
                    for qq in range(4):
                        mq = m0 + qq
                        dst = pts[pi][:, qq * 128:(qq + 1) * 128]
                        if mq < 0:
                            S.op("pool", lambda e: e.memset(dst, 0.0), writes=[pres[pi][qq]])
                            continue
                        S.op("act", lambda e: e.activation(
                            out=dst, in_=cx.bank[sbk][:, qq * 128:(qq + 1) * 128], func=AF.Exp,
                            bias=cstf[:, CF_BIAS + h * 67 + mq + 3:CF_BIAS + h * 67 + mq + 4],
                            scale=QK_SCALE), reads=[cx.bankres[sbk], cstres],
                            writes=[pres[pi][qq]])
                        if mq == 0:
                            S.op("pool", lambda e: e.tensor_tensor(
                                out=dst, in0=dst, in1=cb16[:, 2048:2176], op=ALU.mult),
                                reads=[pres[pi][qq], cstres], writes=[pres[pi][qq]])
                pis.append(pi)
                vsl.append((bstate["cur_v"], kt % 16))
            bstate[bi] = (pis, vsl)

        def stage2(bi):
            nonlocal ocount
            g, s_ = blocks[bi]
            pis, vsl = bstate.pop(bi)
            obs = ob_ids[ocount % 2]
            ocount += 1
            for qq in range(4):
                ob = obs[qq // 2]
                for kk in range(2):
                    S.op("pe", lambda e: e.matmul(
                        cx.bank[ob][:, (qq % 2) * 129:(qq % 2) * 129 + 129],
                        lhsT=pts[pis[kk]][:, qq * 128:(qq + 1) * 128],
                        rhs=vts[vsl[kk][0]][:, vsl[kk][1], :], start=(kk == 0), stop=(kk == 1)),
                        reads=[pres[pis[kk]][qq], vres[vsl[kk][0]]], writes=[cx.bankres[ob]])
            for qq in range(4):
                ob = obs[qq // 2]
                qs = g * 4 + qq
                S.op("dve", lambda e: e.scalar_tensor_tensor(
                    out=acc[:, qs, :], in0=cx.bank[ob][:, (qq % 2) * 129:(qq % 2) * 129 + 129],
                    scalar=sel[:, qs * 32 + s_:qs * 32 + s_ + 1], in1=acc[:, qs, :],
                    op0=ALU.mult, op1=ALU.add),
                    reads=[cx.bankres[ob], selres, accres[qs]], writes=[accres[qs]])

        stage1(0)
        for bi in range(len(blocks)):
            if bi + 1 < len(blocks):
                stage1(bi + 1)
            stage2(bi)
        if dbgf is not None and h == HEADS - 1:
            dch = S.chan("dbgf")
            S.dma("sp", dch, out=dbgf[:, 0:256], in_=sel, reads=[selres], is_output=True)
            S.dma("sp", dch, out=dbgf[:, 256:256 + 1032], in_=acc.rearrange("p a b -> p (a b)"),
                  reads=accres, is_output=True)
            S.dma("sp", dch, out=dbgf[:, 1288:1288 + 256], in_=gm, reads=[gmres], is_output=True)
            S.dma("sp", dch, out=dbgf[:, 1544:1552], in_=top8, reads=[top8res], is_output=True)
            S.dma("sp", dch, out=dbgf[:, 1552:1552 + 32], in_=kms, reads=[kmsres], is_output=True)
        S.op("dve", lambda e: e.reciprocal(out=rec, in_=acc[:, :, 128]),
             reads=accres, writes=[recres])
        for half in range(2):
            for qq in range(4):
                qs = half * 4 + qq
                oi = qs % 2
                S.op("dve", lambda e: e.tensor_scalar(out=onorm[oi], in0=acc[:, qs, 0:128],
                                                      scalar1=rec[:, qs:qs + 1], scalar2=None,
                                                      op0=ALU.mult),
                     reads=[accres[qs], recres], writes=[onres[oi]])
                S.op("pe", lambda e: e.transpose(out=cx.bank[tr_b][:, qq * 128:(qq + 1) * 128],
                                                 in_=onorm[oi], identity=ident),
                     reads=[onres[oi], cstres], writes=[cx.bankres[tr_b]])
            S.op("act", lambda e: e.copy(out=oT[:, h, half * 512:(half + 1) * 512],
                                         in_=cx.bank[tr_b][:, :]),
                 reads=[cx.bankres[tr_b]], writes=[ores])


def build_B(stop_after=None, tail=False):
    nc = bass.Bass("TRN2", target_bir_lowering=False)
    dt = nc.dram_tensor
    xT = dt("xT", [D, T], F32, kind="ExternalInput").ap()
    hTi = dt("hTi", [128, KC, T], BF16, kind="ExternalInput").ap()
    hhi = dt("hhi", [128, KC, 2], BF16, kind="ExternalInput").ap()
    nws = dt("nws", [128, 3 * KC], F32, kind="ExternalInput").ap()
    wb_ = dt("wb", [D, NB_COLS], F32, kind="ExternalInput").ap()
    cst = dt("cst", [128, CF_TOTAL], F32, kind="ExternalInput").ap()
    blk = dt("blk", [128, 256], F32, kind="ExternalInput").ap()
    msk_d = dt("msk", [128, 2176], BF16, kind="ExternalInput").ap()
    KT_d = dt("KT", [HEADS, 128, SEQ], BF16, kind="ExternalInput").ap()
    V_d = dt("VA", [HEADS, 128, 64, 129], BF16, kind="ExternalInput").ap()
    kg_d = dt("kgt", [64, 128, 512], BF16, kind="ExternalInput").ap()
    vg_d = dt("vgt", [64, 128, 1024], BF16, kind="ExternalInput").ap()
    ad_d = dt("adT", [17, SEQ], BF16, kind="ExternalInput").ap()
    wup_d = dt("wup", [17, 512], F32, kind="ExternalInput").ap()
    kgT_d = dt("kgT", [128, 4, T], BF16, kind="ExternalInput").ap()
    gnw_d = dt("gnw", [128, 2], F32, kind="ExternalInput").ap()
    cw_d = dt("cw", [128, 8, 3], F32, kind="ExternalInput").ap()
    wbr = [dt("wbr%d" % i, [1024, D], F32, kind="ExternalInput").ap() for i in range(3)]
    w_out = dt("w_out", [D, D], F32, kind="ExternalInput").ap()
    w_g = dt("w_g", [D, DFF], F32, kind="ExternalInput").ap()
    w_u = dt("w_u", [D, DFF], F32, kind="ExternalInput").ap()
    w_d = dt("w_d", [DFF, D], F32, kind="ExternalInput").ap()
    x1T = dt("x1T", [D, T], F32).ap()
    if stop_after is None:
        x2T = dt("x2T", [D, T], F32, kind="ExternalOutput").ap()
        if tail:
            wan = dt("wan", [D, NA_COLS], F32, kind="ExternalInput").ap()
            ya = dt("ya", [3584, T], BF16, kind="ExternalOutput").ap()
            yad = dt("yad", [16, T], BF16, kind="ExternalOutput").ap()
            hTo = dt("hTo", [128, KC, T], BF16, kind="ExternalOutput").ap()
        else:
            xnT = dt("xnT", [D, T], F32, kind="ExternalOutput").ap()
    else:
        dbg = dt("dbg", [128, 8 * T], BF16, kind="ExternalOutput").ap()
        dbgf = dt("dbgf", [128, 2048], F32, kind="ExternalOutput").ap()
    cx = Ctx(nc)
    S = cx.S
    setup_common(cx)
    cx.bank_ids = [0, 1, 2, 3, 4, 5, 6, 7]
    chans = []
    cchan = S.chan("const")
    cstf = cx.sb("cstf", [128, CF_TOTAL], F32)
    cstres = Res("cst")
    S.dma("sp", cchan, out=cstf[:], in_=cst[:, :], writes=[cstres])
    cb16 = cx.sb("cb16", [128, 2176], BF16)
    S.dma("sp", cchan, out=cb16[:], in_=msk_d[:, :], writes=[cstres])
    blkm = cx.sb("blkm", [128, 256], F32)
    S.dma("sp", cchan, out=blkm[:], in_=blk[:, :], writes=[cstres])
    nwt = cx.sb("nwt", [128, 3 * KC], F32)
    S.dma("sp", cchan, out=nwt[:], in_=nws[:, :], writes=[cstres])
    gnw = cx.sb("gnw_sb", [128, 2], F32)
    S.dma("sp", cchan, out=gnw[:], in_=gnw_d[:, :], writes=[cstres])
    cwt = cx.sb("cwt_sb", [128, 8, 3], F32)
    S.dma("sp", cchan, out=cwt[:], in_=cw_d[:, :, :], writes=[cstres])
    ident = cstf[:, CF_ID:CF_ID + 128]
    xring = [cx.sb("xr%d" % i, [128, T], F32) for i in range(2)]
    xres = [Res("xr%d" % i) for i in range(2)]
    xchan = [S.chan("x%d" % i) for i in range(2)]
    sq = [cx.sb("sq%d" % i, [128, T], BF16) for i in range(2)]
    sqres = [Res("sq%d" % i) for i in range(2)]
    rstd = cx.sb("rstd", [128, T], F32)
    rstdres = Res("rstd")
    hT = cx.sb("hT", [128, KC, T], BF16)
    hres = [Res("hT%d" % i) for i in range(KC)]
    bigb = cx.sb("bigb", [128, 44 * T], BF16)
    fscr = cx.sb("fscr", [128, 6144], F32)
    chans += [cchan] + xchan + cx.wchan

    def rows(a, n):
        return bigb[:, a * T:(a + n) * T]

    oT_a = rows(0, 8).rearrange("p (a b) -> p a b", a=8)
    oares = Res("oT_a")
    qT = rows(8, 8).rearrange("p (a b) -> p a b", a=8)
    qres = Res("qT")
    hich = [S.chan("hTi%d" % i) for i in range(4)]
    chans += hich
    for c4 in range(4):
        S.dma("sp", hich[c4], out=hT[:, c4 * 4:(c4 + 1) * 4, :],
              in_=hTi[:, c4 * 4:(c4 + 1) * 4, :], writes=[hres[c4 * 4]])
        for j_ in range(1, 4):
            hres[c4 * 4 + j_].w = hres[c4 * 4].w
    flip = {"n": 0}

    def evac(out_ap, ps, psres, wres_list):
        flip["n"] += 1
        if flip["n"] % 2:
            S.op("act", lambda e: e.copy(out=out_ap, in_=ps), reads=[psres], writes=wres_list)
        else:
            S.op("dve", lambda e: e.tensor_copy(out=out_ap, in_=ps), reads=[psres],
                 writes=wres_list)

    def epi_q(c0, cw, half, ps, psres):
        evac(qT[:, c0 // 128, half * 512:(half + 1) * 512], ps, psres, [qres])
    dense_fm(cx, wb_, 0, 1024, D, hT, hres, epi_q)
    barrier(S, chans)
    ar_b = Arena(bigb, 16 * T, 44 * T)
    ar_f = Arena(fscr, 0, 6144)
    attention_phase(cx, qT, qres, KT_d, V_d, oT_a, oares, ar_b, ar_f, cstf, cstres, ident, blkm,
                    chans, cb16, dbgf=None)
    barrier(S, chans)
    if stop_after == "attn":
        och = S.chan("dbg")
        S.dma("sp", och, out=dbg[:, :], in_=rows(0, 8), reads=[oares], is_output=True)
        S.finish()
        cx.close()
        return nc
    ogT = rows(8, 8).rearrange("p (a b) -> p a b", a=8)
    ogres = Res("ogT")
    ar_b.reset()
    ar_f.reset()
    qgT = ar_b.take(4 * T).rearrange("p (a b) -> p a b", a=4)
    qgres = Res("qgT")
    kgT = ar_b.take(4 * T).rearrange("p (a b) -> p a b", a=4)
    kgres = Res("kgT")
    wup = ar_f.take(512)[0:17, :]
    wupres = Res("wup")
    gch = S.chan("gla_in")
    chans.append(gch)
    S.dma("sp", gch, out=kgT, in_=kgT_d[:, :, :], writes=[kgres])
    S.dma("sp", gch, out=wup, in_=wup_d[:, :], writes=[wupres])
    for r_ in (kgres, wupres):
        r_.w = (gch.key, gch.cnt)

    def epi_qg(c0, cw, half, ps, psres):
        evac(qgT[:, c0 // 128, half * 512:(half + 1) * 512], ps, psres, [qgres])
    dense_fm(cx, wb_, 1024, 512, D, hT, hres, epi_qg)
    barrier(S, chans)
    gla_phase(cx, qgT, qgres, kgT, kgres, kg_d, vg_d, ad_d, wup, wupres, ogT, ogres, ar_b, ar_f,
              cstf, cstres, chans)
    barrier(S, chans)
    if stop_after == "gla":
        och = S.chan("dbg")
        S.dma("sp", och, out=dbg[:, :], in_=rows(8, 8), reads=[ogres], is_output=True)
        S.finish()
        cx.close()
        return nc
    ar_b.reset()
    ar_f.reset()
    for h in range(GH):
        for half in range(2):
            q_ = half
            S.op("act", lambda e: e.activation(out=sq[q_][:], in_=ogT[:, 2 * h + half, :],
                                               func=AF.Square), reads=[ogres], writes=[sqres[q_]])
            for th in range(NH):
                S.op("pe", lambda e: e.matmul(cx.bank[6 + th][:, :], lhsT=cx.ones[:, :],
                                              rhs=sq[q_][:, th * 512:(th + 1) * 512],
                                              start=(half == 0), stop=(half == 1)),
                     reads=[cx.onesres, sqres[q_]], writes=[cx.bankres[6 + th]])
        norm_finish(cx, [6, 7], rstd, rstdres, GDV)
        for half in range(2):
            S.op("dve", lambda e: e.scalar_tensor_tensor(
                out=ogT[:, 2 * h + half, :], in0=ogT[:, 2 * h + half, :],
                scalar=gnw[:, half:half + 1], in1=rstd[:, :], op0=ALU.mult, op1=ALU.mult),
                reads=[ogres, cstres, rstdres], writes=[ogres])
    gtl = [ar_f.take(512) for _ in range(4)]
    gtres = [Res("gt%d" % i) for i in range(4)]
    gcnt = {"n": 0}

    def epi_rg(c0, cw, half, ps, psres):
        i = gcnt["n"] % 4
        gcnt["n"] += 1
        S.op("act", lambda e: e.activation(out=gtl[i], in_=ps, func=AF.Silu), reads=[psres],
             writes=[gtres[i]])
        dst = ogT[:, c0 // 128, half * 512:(half + 1) * 512]
        S.op("dve", lambda e: e.tensor_tensor(out=dst, in0=dst, in1=gtl[i], op=ALU.mult),
             reads=[gtres[i], ogres], writes=[ogres])
    dense_fm(cx, wb_, 1536, 1024, D, hT, hres, epi_rg)
    barrier(S, chans)
    if stop_after == "glapost":
        och = S.chan("dbg")
        S.dma("sp", och, out=dbg[:, :], in_=rows(8, 8), reads=[ogres], is_output=True)
        S.finish()
        cx.close()
        return nc
    yT = rows(16, 8).rearrange("p (a b) -> p a b", a=8)
    yres = Res("yT")
    cT = rows(24, 8).rearrange("p (a b) -> p a b", a=8)
    cres = Res("cT")
    cu = bigb[:, 32 * T:32 * T + 8 * (T + 2)].rearrange("p (a b) -> p a b", a=8)
    cures = Res("cu")
    ar_f.reset()
    ar_b.p = 41 * T + 64
    hh = ar_b.take(2 * KC).rearrange("p (a b) -> p a b", a=KC)
    hhres = Res("hh")
    hch = S.chan("halo")
    chans.append(hch)
    S.dma("sp", hch, out=hh, in_=hhi[:, :, :], writes=[hhres])
    chalo = ar_f.take(2 * 8).rearrange("p (a b) -> p a b", a=8)
    chres = Res("chalo")

    def epi_c(c0, cw, half, ps, psres):
        evac(cT[:, c0 // 128, half * 512:(half + 1) * 512], ps, psres, [cres])

    def xepi_c(c0, cw, ps, psres):
        S.op("act", lambda e: e.copy(out=chalo[:, c0 // 128, :], in_=ps), reads=[psres],
             writes=[chres])

    def epi_u(c0, cw, half, ps, psres):
        r_ = c0 // 128
        S.op("dve", lambda e: e.tensor_tensor(
            out=cu[:, r_, 2 + half * 512:2 + (half + 1) * 512], in0=ps,
            in1=cT[:, r_, half * 512:(half + 1) * 512], op=ALU.mult),
            reads=[psres, cres], writes=[cures])

    def xepi_u(c0, cw, ps, psres):
        r_ = c0 // 128
        S.op("dve", lambda e: e.tensor_tensor(out=cu[:, r_, 0:2], in0=ps, in1=chalo[:, r_, :],
                                              op=ALU.mult),
             reads=[psres, chres], writes=[cures])
    dense_fm(cx, wb_, 3584, 1024, D, hT, hres, epi_c, banks=[0, 1, 2, 3],
             extra=(hh, hhres, [4, 5], xepi_c))
    dense_fm(cx, wb_, 4608, 1024, D, hT, hres, epi_u, banks=[0, 1, 2, 3],
             extra=(hh, hhres, [4, 5], xepi_u))
    cacc = ar_f.take(T)
    caccres = Res("cacc")
    for r_ in range(8):
        S.op("dve", lambda e: e.tensor_scalar(out=cacc, in0=cu[:, r_, 2:T + 2],
                                              scalar1=cwt[:, r_, 2:3], scalar2=None,
                                              op0=ALU.mult),
             reads=[cures, cstres], writes=[caccres])
        S.op("dve", lambda e: e.scalar_tensor_tensor(out=cacc, in0=cu[:, r_, 1:T + 1],
                                                   scalar=cwt[:, r_, 1:2], in1=cacc,
                                                   op0=ALU.mult, op1=ALU.add),
             reads=[cures, caccres], writes=[caccres])
        S.op("dve", lambda e: e.scalar_tensor_tensor(out=yT[:, r_, :], in0=cu[:, r_, 0:T],
                                                   scalar=cwt[:, r_, 0:1], in1=cacc,
                                                   op0=ALU.mult, op1=ALU.add),
             reads=[cures, caccres], writes=[yres])

    def epi_b(c0, cw, half, ps, psres):
        dst = yT[:, c0 // 128, half * 512:(half + 1) * 512]
        S.op("dve", lambda e: e.tensor_tensor(out=dst, in0=ps, in1=dst, op=ALU.mult),
             reads=[psres, yres], writes=[yres])
    dense_fm(cx, wb_, 2560, 1024, D, hT, hres, epi_b)
    barrier(S, chans)
    if stop_after == "conv":
        och = S.chan("dbg")
        S.dma("sp", och, out=dbg[:, :], in_=rows(16, 8), reads=[yres], is_output=True)
        S.finish()
        cx.close()
        return nc
    mixedT = rows(24, 16).rearrange("p (a b) -> p a b", a=16)
    mixres = Res("mixedT")
    ar_f.reset()
    gt4 = [ar_f.take(512) for _ in range(4)]
    gt4res = [Res("g4_%d" % i) for i in range(4)]
    macc = [ar_f.take(512) for _ in range(4)]
    maccres = [Res("macc%d" % i) for i in range(4)]
    mtmp = [ar_f.take(512) for _ in range(2)]
    mtres = [Res("mtmp%d" % i) for i in range(2)]
    mcnt = {"n": 0}
    branches = [(wbr[0], oT_a, oares), (wbr[1], ogT, ogres), (wbr[2], yT, yres)]
    for jg in range(D // 256):
        for br, (wd, actb, actbres) in enumerate(branches):
            def epi_gate(c0, cw, half, ps, psres):
                i = (c0 // 128) * NH + half
                S.op("act", lambda e: e.activation(out=gt4[i], in_=ps, func=AF.Sigmoid),
                     reads=[psres], writes=[gt4res[i]])
            dense_fm(cx, wb_, 5632 + br * D + jg * 256, 256, D, hT, hres, epi_gate)

            def epi_br(c0, cw, half, ps, psres):
                i = (c0 // 128) * NH + half
                if br == 0:
                    S.op("dve", lambda e: e.tensor_tensor(out=macc[i], in0=ps, in1=gt4[i],
                                                          op=ALU.mult),
                         reads=[psres, gt4res[i]], writes=[maccres[i]])
                else:
                    k_ = mcnt["n"] % 2
                    mcnt["n"] += 1
                    S.op("dve", lambda e: e.tensor_tensor(out=mtmp[k_], in0=ps, in1=gt4[i],
                                                          op=ALU.mult),
                         reads=[psres, gt4res[i]], writes=[mtres[k_]])
                    if br == 1:
                        S.op("pool", lambda e: e.tensor_tensor(out=macc[i], in0=macc[i],
                                                               in1=mtmp[k_], op=ALU.add),
                             reads=[maccres[i], mtres[k_]], writes=[maccres[i]])
                    else:
                        S.op("pool", lambda e: e.tensor_tensor(
                            out=mixedT[:, jg * 2 + c0 // 128, half * 512:(half + 1) * 512],
                            in0=macc[i], in1=mtmp[k_], op=ALU.add),
                            reads=[maccres[i], mtres[k_]], writes=[mixres])
            dense_fm(cx, wd, jg * 256, 256, 1024, actb, actbres, epi_br)
    barrier(S, chans)
    cx.bank_ids = [0, 1, 2, 3, 6, 7, 0, 1, 2, 3, 6, 7]
    ar_f.reset()
    x1s = [ar_f.take(512) for _ in range(3)]
    x1res = [Res("x1s%d" % i) for i in range(3)]
    x1ch = [S.chan("x1o%d" % i) for i in range(3)]
    chans += x1ch
    st7 = {"n": 0, "slot": 0}

    def make_resid_epi(src, dst, want_h, is_out):
        def prefetch(r_):
            sl = r_ % 2
            S.dma("pool", xchan[sl], out=xring[sl][:], in_=src[r_ * 128:(r_ + 1) * 128, :],
                  writes=[xres[sl]])
        prefetch(0)

        def epi(c0, cw, half, ps, psres):
            r_ = c0 // 128
            if half == 0 and r_ + 1 < KC:
                prefetch(r_ + 1)
            sl = r_ % 2
            i = st7["n"] % 3
            st7["n"] += 1
            hs = slice(half * 512, (half + 1) * 512)
            S.op("dve", lambda e: e.tensor_tensor(out=x1s[i], in0=ps, in1=xring[sl][:, hs],
                                                  op=ALU.add),
                 reads=[psres, xres[sl]], writes=[x1res[i]])
            S.dma("pool", x1ch[i], out=dst[r_ * 128:(r_ + 1) * 128, hs], in_=x1s[i],
                  reads=[x1res[i]], is_output=is_out)
            q_ = st7["n"] % 2
            S.op("act", lambda e: e.activation(out=sq[q_][:, 0:512], in_=x1s[i], func=AF.Square),
                 reads=[x1res[i]], writes=[sqres[q_]])
            S.op("pe", lambda e: e.matmul(cx.bank[4 + half][:, :], lhsT=cx.ones[:, :],
                                          rhs=sq[q_][:, 0:512], start=(r_ == 0),
                                          stop=(r_ == KC - 1)),
                 reads=[cx.onesres, sqres[q_]], writes=[cx.bankres[4 + half]])
            if want_h:
                S.op("pool", lambda e: e.tensor_copy(out=hT[:, r_, hs], in_=x1s[i]),
                     reads=[x1res[i]], writes=[hres[r_]])
        return epi
    dense_fm(cx, w_out, 0, D, D, mixedT, mixres, make_resid_epi(xT, x1T, True, False))
    norm_finish(cx, [4, 5], rstd, rstdres, D)
    for c in range(KC):
        S.op("dve", lambda e: e.scalar_tensor_tensor(out=hT[:, c, :], in0=hT[:, c, :],
                                                   scalar=nwt[:, KC + c:KC + c + 1],
                                                   in1=rstd[:, :], op0=ALU.mult, op1=ALU.mult),
             reads=[hres[c], cstres, rstdres], writes=[hres[c]])
    barrier(S, chans)
    actT = bigb[:, :].rearrange("p (a b) -> p a b", a=44)
    actres = Res("actT")
    cx.bank_ids = [0, 1, 2, 3, 4, 5, 6, 7]
    ar_f.reset()
    gs4 = [ar_f.take(512) for _ in range(4)]
    gs4res = [Res("gs4_%d" % i) for i in range(4)]
    for fg in range(DFF // 256):
        def epi_g(c0, cw, half, ps, psres):
            i = (c0 // 128) * NH + half
            S.op("act", lambda e: e.activation(out=gs4[i], in_=ps, func=AF.Silu), reads=[psres],
                 writes=[gs4res[i]])
        dense_fm(cx, w_g, fg * 256, 256, D, hT, hres, epi_g)

        def epi_up(c0, cw, half, ps, psres):
            i = (c0 // 128) * NH + half
            S.op("dve", lambda e: e.tensor_tensor(
                out=actT[:, fg * 2 + c0 // 128, half * 512:(half + 1) * 512], in0=ps, in1=gs4[i],
                op=ALU.mult), reads=[psres, gs4res[i]], writes=[actres])
        dense_fm(cx, w_u, fg * 256, 256, D, hT, hres, epi_up)
    barrier(S, chans)
    cx.bank_ids = [0, 1, 2, 3, 6, 7, 0, 1, 2, 3, 6, 7]
    st7["n"] = 0
    x1s_ = x1s
    dense_fm(cx, w_d, 0, D, DFF, actT, actres, make_resid_epi(x1T, x2T, tail, True))
    norm_finish(cx, [4, 5], rstd, rstdres, D)
    barrier(S, chans)
    if tail:
        for c in range(KC):
            S.op("dve", lambda e: e.scalar_tensor_tensor(out=hT[:, c, :], in0=hT[:, c, :],
                                                       scalar=nwt[:, c:c + 1], in1=rstd[:, :],
                                                       op0=ALU.mult, op1=ALU.mult),
                 reads=[hres[c], cstres, rstdres], writes=[hres[c]])
        hoch = S.chan("hTo")
        for c4 in range(4):
            S.dma("pool", hoch, out=hTo[:, c4 * 4:(c4 + 1) * 4, :],
                  in_=hT[:, c4 * 4:(c4 + 1) * 4, :], reads=hres[c4 * 4:(c4 + 1) * 4],
                  is_output=True)
        cx.bank_ids = [0, 1, 2, 3, 4, 5, 6, 7]
        NO = 4
        ost = [bigb[:, i * 512:(i + 1) * 512] for i in range(NO)]
        ostres = [Res("ost%d" % i) for i in range(NO)]
        ostch = [S.chan("o%d" % i) for i in range(NO)]
        ostf = bigb[0:16, 4 * 512:4 * 512 + T]
        ostfres = Res("ostf")
        stt_ = {"n": 0}

        def epi_out(rowbase):
            def epi(c0, cw, half, ps, psres):
                i = stt_["n"] % NO
                stt_["n"] += 1
                evac(ost[i][:cw, :], ps, psres, [ostres[i]])
                S.dma("pool", ostch[i], out=ya[rowbase + c0:rowbase + c0 + cw,
                                               half * 512:(half + 1) * 512],
                      in_=ost[i][:cw, :], reads=[ostres[i]], is_output=True)
            return epi
        dense_fm(cx, wan, 0, 2048, D, hT, hres, epi_out(0))
        dense_fm(cx, wan, 2048, 1536, D, hT, hres, epi_out(2048))

        def epi_ad(c0, cw, half, ps, psres):
            S.op("act", lambda e: e.copy(out=ostf[:cw, half * 512:(half + 1) * 512], in_=ps),
                 reads=[psres], writes=[ostfres])
        dense_fm(cx, wan, 3584, 16, D, hT, hres, epi_ad)
        S.dma("pool", hoch, out=yad[:, :], in_=ostf, reads=[ostfres], is_output=True)
        S.finish()
        cx.close()
        return nc
    fo = [ar_f.take(T) for _ in range(2)]
    fores = [Res("fo%d" % i) for i in range(2)]
    foch = [S.chan("fo%d" % i) for i in range(2)]
    for c in range(KC):
        sl = c % 2
        S.dma("sp", xchan[sl], out=xring[sl][:], in_=x2T[c * 128:(c + 1) * 128, :],
              writes=[xres[sl]])
        S.op("dve", lambda e: e.scalar_tensor_tensor(out=fo[sl], in0=xring[sl][:],
                                                   scalar=nwt[:, 2 * KC + c:2 * KC + c + 1],
                                                   in1=rstd[:, :], op0=ALU.mult, op1=ALU.mult),
             reads=[xres[sl], cstres, rstdres], writes=[fores[sl]])
        S.dma("sp", foch[sl], out=xnT[c * 128:(c + 1) * 128, :], in_=fo[sl], reads=[fores[sl]],
              is_output=True)
    S.finish()
    cx.close()
    return nc


def gla_phase(cx, qgT, qgres, kgT, kgres, kg_d, vg_d, ad_d, wup, wupres, ogT, ogres, ar_b, ar_f,
              cstf, cstres, chans):
    nc, S = cx.nc, cx.S
    NCHK = 128
    OWN0 = NCHK - T // CH
    k4 = [ar_b.take(2048)[0:64, :].rearrange("p (a b) -> p a b", a=4) for _ in range(2)]
    v4 = [ar_b.take(4096)[0:64, :].rearrange("p (a b) -> p a b", a=4) for _ in range(2)]
    kres4 = [Res("k4_%d" % i) for i in range(2)]
    vres4 = [Res("v4_%d" % i) for i in range(2)]
    adres4 = [Res("ad4_%d" % i) for i in range(2)]
    kch = [S.chan("k4_%d" % i) for i in range(2)]
    vch = [S.chan("v4_%d" % i) for i in range(2)]
    adch = [S.chan("ad4_%d" % i) for i in range(2)]
    kdec = [ar_b.take(512)[0:64, :] for _ in range(2)]
    kdres = [Res("kdec%d" % i) for i in range(2)]
    qin = [ar_b.take(256).rearrange("p (a b) -> p a b", a=4) for _ in range(2)]
    kin = [ar_b.take(256).rearrange("p (a b) -> p a b", a=4) for _ in range(2)]
    qkres = [Res("qk%d" % i) for i in range(2)]
    atm = [ar_b.take(256)[0:64, :].rearrange("p (a b) -> p a b", a=4) for _ in range(2)]
    atres = [Res("atm%d" % i) for i in range(2)]
    Sb = ar_b.take(1024).rearrange("p (a b) -> p a b", a=4)
    sbres = Res("Sb")
    ad4 = [ar_b.take(256)[0:17, :] for _ in range(2)]
    NL = 3
    Lr = [ar_b.take(512)[0:64, :] for _ in range(NL)]
    lres = [Res("L%d" % i) for i in range(NL)]
    et = [ar_f.take(512)[0:64, :] for _ in range(2)]
    etres = [Res("et%d" % i) for i in range(2)]
    cb = ar_b.take(136)
    cbres = Res("cb")
    wupb = ar_b.take(512)[0:17, :]
    E3 = [ar_f.take(512)[0:64, :] for _ in range(2)]
    e3res = [Res("E3_%d" % i) for i in range(2)]
    E1 = [ar_f.take(256).rearrange("p (a b) -> p a b", a=4) for _ in range(2)]
    E2 = [ar_f.take(256).rearrange("p (a b) -> p a b", a=4) for _ in range(2)]
    e12res = [Res("E12_%d" % i) for i in range(2)]
    dec = [ar_f.take(4) for _ in range(2)]
    decres = [Res("dec%d" % i) for i in range(2)]
    St = ar_f.take(1024).rearrange("p (a b) -> p a b", a=4)
    stres = [Res("S%d" % i) for i in range(4)]
    chans += kch + vch + adch
    ZB, RB, BB, OB = 0, 1, 2, 3
    KVB = [[4, 5], [6, 7]]
    S.op("dve", lambda e: e.tensor_copy(out=cb[0:64, 0:128], in_=cstf[0:64, CF_TRI16:CF_TRI16 + 128]),
         reads=[cstres], writes=[cbres])
    S.op("dve", lambda e: e.tensor_copy(out=cb[0:64, 128:129], in_=cstf[0:64, CF_NC:CF_NC + 1]),
         reads=[cstres], writes=[cbres])
    S.op("dve", lambda e: e.tensor_copy(out=wupb, in_=wup), reads=[wupres], writes=[cbres])
    tri16 = cb[0:64, 0:64]
    su16 = cb[0:64, 64:128]
    ncol = cb[0:64, 128:129]
    tri1 = cstf[0:64, CF_TRI1:CF_TRI1 + 256].rearrange("p (a b) -> p a b", a=4)
    for h in range(4):
        S.op("pool", lambda e: e.memset(St[:, h, :], 0.0), writes=[stres[h]])
    S.op("pool", lambda e: e.memset(Sb, 0.0), writes=[sbres])

    def stageA(n):
        g4, j = n // 4, n % 4
        sl = g4 % 2
        if j == 0:
            S.dma("sp", kch[sl], out=k4[sl], in_=kg_d[:, n:n + 4, :], writes=[kres4[sl]])
            S.dma("sp", vch[sl], out=v4[sl], in_=vg_d[:, n:n + 4, :], writes=[vres4[sl]])
            S.dma("sp", adch[sl], out=ad4[sl], in_=ad_d[:, n * CH:(n + 4) * CH],
                  writes=[adres4[sl]])
        S.op("pe", lambda e: e.matmul(cx.bank[ZB][0:64, :], lhsT=ad4[sl][:, j * CH:(j + 1) * CH],
                                      rhs=wupb, start=True, stop=True),
             reads=[adres4[sl], cbres], writes=[cx.bankres[ZB]])
        li = n % NL
        ei = n % 2
        S.op("act", lambda e: e.activation(out=et[ei], in_=cx.bank[ZB][0:64, :], func=AF.Exp,
                                           scale=-1.0), reads=[cx.bankres[ZB]], writes=[etres[ei]])
        S.op("act", lambda e: e.activation(out=Lr[li], in_=et[ei], func=AF.Ln, bias=1.0),
             reads=[etres[ei]], writes=[lres[li]])

    def stageB(n):
        own = n >= OWN0
        g4, j = n // 4, n % 4
        sl = g4 % 2
        li = n % NL
        p2 = n % 2
        S.op("pe", lambda e: e.matmul(cx.bank[RB][0:64, :], lhsT=su16, rhs=Lr[li], start=True,
                                      stop=True),
             reads=[cbres, lres[li]], writes=[cx.bankres[RB]])
        for h in range(4):
            if own:
                S.op("pe", lambda e: e.matmul(cx.bank[BB][:, h * 64:(h + 1) * 64],
                                              lhsT=Lr[li][:, h * 128:(h + 1) * 128], rhs=tri16,
                                              start=True, stop=True),
                     reads=[cbres, lres[li]], writes=[cx.bankres[BB]])
            else:
                S.op("pe", lambda e: e.matmul(cx.bank[BB][:, h:h + 1],
                                              lhsT=Lr[li][:, h * 128:(h + 1) * 128], rhs=ncol,
                                              start=True, stop=True),
                     reads=[cbres, lres[li]], writes=[cx.bankres[BB]])
        S.op("act", lambda e: e.activation(out=E3[p2], in_=cx.bank[RB][0:64, :], func=AF.Exp),
             reads=[cx.bankres[RB]], writes=[e3res[p2]])
        if own:
            S.op("act", lambda e: e.activation(out=E1[p2].rearrange("p a b -> p (a b)"),
                                               in_=cx.bank[BB][:, 0:256], func=AF.Exp),
                 reads=[cx.bankres[BB]], writes=[e12res[p2]])
            S.op("act", lambda e: e.activation(out=E2[p2].rearrange("p a b -> p (a b)"),
                                               in_=cx.bank[BB][:, 0:256], func=AF.Exp, scale=-1.0),
                 reads=[cx.bankres[BB]], writes=[e12res[p2]])
            S.op("act", lambda e: e.copy(out=dec[p2], in_=E1[p2][:, :, 63]),
                 reads=[e12res[p2]], writes=[decres[p2]])
            t0 = (n - OWN0) * CH
            S.op("dve", lambda e: e.scalar_tensor_tensor(
                out=qin[p2], in0=qgT[:, :, t0:t0 + CH], scalar=float(GDK) ** -0.5, in1=E1[p2],
                op0=ALU.mult, op1=ALU.mult), reads=[qgres, e12res[p2]], writes=[qkres[p2]])
            S.op("dve", lambda e: e.tensor_tensor(out=kin[p2], in0=kgT[:, :, t0:t0 + CH],
                                                  in1=E2[p2], op=ALU.mult),
                 reads=[kgres, e12res[p2]], writes=[qkres[p2]])
        else:
            S.op("act", lambda e: e.activation(out=dec[p2], in_=cx.bank[BB][:, 0:4], func=AF.Exp),
                 reads=[cx.bankres[BB]], writes=[decres[p2]])
        S.op("dve", lambda e: e.tensor_tensor(out=kdec[p2], in0=k4[sl][:, j, :], in1=E3[p2],
                                              op=ALU.mult),
             reads=[kres4[sl], e3res[p2]], writes=[kdres[p2]])

    def stageC(n):
        own = n >= OWN0
        g4, j = n // 4, n % 4
        sl = g4 % 2
        p2 = n % 2
        if own:
            t0 = (n - OWN0) * CH
            for h in range(4):
                S.op("pe", lambda e: e.matmul(cx.bank[BB][0:64, 256 + h * 64:256 + (h + 1) * 64],
                                              lhsT=kin[p2][:, h, :], rhs=qin[p2][:, h, :],
                                              start=True, stop=True),
                     reads=[qkres[p2]], writes=[cx.bankres[BB]])
            S.op("dve", lambda e: e.tensor_tensor(
                out=atm[p2], in0=cx.bank[BB][0:64, 256:512].rearrange("p (a b) -> p a b", a=4),
                in1=tri1, op=ALU.mult), reads=[cx.bankres[BB], cstres], writes=[atres[p2]])
            for h in range(4):
                for half in range(2):
                    col = (h * 2 + half) * 64
                    S.op("pe", lambda e: e.matmul(
                        cx.bank[OB][:, col:col + 64],
                        lhsT=v4[sl][:, j, h * 256 + half * 128:h * 256 + (half + 1) * 128],
                        rhs=atm[p2][:, h, :], start=True, stop=False),
                        reads=[vres4[sl], atres[p2]], writes=[cx.bankres[OB]])
                    S.op("pe", lambda e: e.matmul(
                        cx.bank[OB][:, col:col + 64], lhsT=Sb[:, h, half * 128:(half + 1) * 128],
                        rhs=qin[p2][:, h, :], start=False, stop=True),
                        reads=[sbres, qkres[p2]], writes=[cx.bankres[OB]])
            S.op("act", lambda e: e.copy(out=ogT[:, :, t0:t0 + CH],
                                         in_=cx.bank[OB][:, :].rearrange("p (a b) -> p a b", a=8)),
                 reads=[cx.bankres[OB]], writes=[ogres])
        kb = KVB[n % 2]
        for h in range(4):
            b = kb[h // 2]
            S.op("pe", lambda e: e.matmul(cx.bank[b][:, (h % 2) * 256:(h % 2) * 256 + 256],
                                          lhsT=kdec[p2][:, h * 128:(h + 1) * 128],
                                          rhs=v4[sl][:, j, h * 256:(h + 1) * 256],
                                          start=True, stop=True),
                 reads=[kdres[p2], vres4[sl]], writes=[cx.bankres[b]])
        for h in range(4):
            b = kb[h // 2]
            S.op("dve", lambda e: e.scalar_tensor_tensor(
                out=St[:, h, :], in0=St[:, h, :], scalar=dec[p2][:, h:h + 1],
                in1=cx.bank[b][:, (h % 2) * 256:(h % 2) * 256 + 256], op0=ALU.mult, op1=ALU.add),
                reads=[stres[h], decres[p2], cx.bankres[b]], writes=[stres[h]])
        if n >= OWN0 - 1 and n < NCHK - 1:
            S.op("act", lambda e: e.copy(out=Sb.rearrange("p a b -> p (a b)"),
                                         in_=St.rearrange("p a b -> p (a b)")),
                 reads=stres, writes=[sbres])

    for i in range(-2, NCHK):
        if i + 2 < NCHK:
            stageA(i + 2)
        if 0 <= i + 1 < NCHK:
            stageB(i + 1)
        if i >= 0:
            stageC(i)


_PROGS = {}


def _prog(name):
    if name not in _PROGS:
        if name == "A":
            _PROGS[name] = build_A()
        else:
            _PROGS[name] = build_B(tail=(name == "Bt"))
    return _PROGS[name]


def _fm(w):
    w = np.asarray(w, np.float32)
    return np.ascontiguousarray(w.reshape(-1, 128).T)


def _slots(a, c):
    pad = np.zeros((7 * T,) + a.shape[1:], a.dtype)
    return np.concatenate([pad, a], 0)[c * T:c * T + SEQ]


def _wa_cols(wl):
    return np.ascontiguousarray(np.concatenate(
        [wl[:, C_KA:C_QG], wl[:, C_KG:C_RG], wl[:, C_AD:C_B]], 1))


def kernel(x, norm1_w, w_in, gla_w_up, gla_b, gla_norm_w, conv_w, w_br_attn, w_br_gla, w_br_conv,
           w_out, norm2_w, w_ffn_gate, w_ffn_up, w_ffn_down, final_norm_w):
    f32 = np.float32
    bf = ml_dtypes.bfloat16
    x = np.asarray(x, f32)
    xcur = x[0]
    cst = host_consts()
    msk = host_masks()
    cores = list(range(NCORES))
    in_a = [{"xT": np.ascontiguousarray(xcur[c * T:(c + 1) * T].T), "nw": _fm(norm1_w[0]),
             "wa": _wa_cols(np.asarray(w_in[0], f32))} for c in cores]
    prev = run_bass_kernel_spmd(_prog("A"), in_a, core_ids=cores).results
    xn = None
    for l in range(DEPTH):
        last = l == DEPTH - 1
        wl = np.asarray(w_in[l], f32)
        ya = np.concatenate([np.asarray(r["ya"]).T for r in prev], 0)
        ad = np.concatenate([np.asarray(r["yad"]).T for r in prev], 0)
        hts = [np.asarray(r["hTo"]) for r in prev]
        k_a, v_a = ya[:, 0:1024], ya[:, 1024:2048]
        k_g, v_g = ya[:, 2048:2560], ya[:, 2560:3584]
        wbm = np.ascontiguousarray(np.concatenate(
            [wl[:, C_QA:C_KA], wl[:, C_QG:C_KG], wl[:, C_RG:C_AD], wl[:, C_B:C_M], wl[:, C_M:]], 1))
        nxt = norm1_w[l + 1] if not last else final_norm_w
        nws = np.ascontiguousarray(np.concatenate(
            [_fm(nxt), _fm(norm2_w[l]), _fm(final_norm_w)], 1))
        wup = np.ascontiguousarray(np.concatenate(
            [np.asarray(gla_w_up[l], f32), np.asarray(gla_b[l], f32)[None, :]], 0))
        gnw = _fm(gla_norm_w[l])
        cw = np.ascontiguousarray(np.asarray(conv_w[l], f32).T.reshape(8, 128, 3).transpose(1, 0, 2))
        shared = {"nws": nws, "wb": wbm, "cst": cst, "msk": msk, "wup": wup, "gnw": gnw, "cw": cw,
                  "wbr0": np.asarray(w_br_attn[l], f32), "wbr1": np.asarray(w_br_gla[l], f32),
                  "wbr2": np.asarray(w_br_conv[l], f32), "w_out": np.asarray(w_out[l], f32),
                  "w_g": np.asarray(w_ffn_gate[l], f32), "w_u": np.asarray(w_ffn_up[l], f32),
                  "w_d": np.asarray(w_ffn_down[l], f32)}
        if not last:
            shared["wan"] = _wa_cols(np.asarray(w_in[l + 1], f32))
        in_b = []
        for c in cores:
            ks = _slots(k_a, c).reshape(SEQ, HEADS, DH)
            vs = _slots(v_a, c).reshape(SEQ, HEADS, DH)
            vaug = np.concatenate([vs, np.ones((SEQ, HEADS, 1), bf)], -1)
            kgs = _slots(k_g, c)
            vgs = _slots(v_g, c)
            ads = _slots(ad, c)
            blk = np.zeros((NBLK,), f32)
            blk[:max(0, 28 - 4 * c)] = NEG
            hh = np.zeros((128, KC, 2), bf)
            if c > 0:
                hh = np.ascontiguousarray(hts[c - 1][:, :, T - 2:T])
            m = dict(shared)
            m.update({
                "xT": np.ascontiguousarray(xcur[c * T:(c + 1) * T].T), "hTi": hts[c], "hhi": hh,
                "blk": np.ascontiguousarray(np.tile(blk, 8)[None, :].repeat(128, 0)),
                "KT": np.ascontiguousarray(ks.transpose(1, 2, 0)),
                "VA": np.ascontiguousarray(vaug.reshape(64, 128, HEADS, DH + 1).transpose(2, 1, 0, 3)),
                "kgt": np.ascontiguousarray(kgs.reshape(128, CH, 512).transpose(1, 0, 2)),
                "vgt": np.ascontiguousarray(vgs.reshape(128, CH, 1024).transpose(1, 0, 2)),
                "adT": np.ascontiguousarray(np.concatenate([ads.T, np.ones((1, SEQ), bf)], 0)),
                "kgT": np.ascontiguousarray(
                    k_g[c * T:(c + 1) * T].T.reshape(GH, 128, T).transpose(1, 0, 2)),
            })
            in_b.append(m)
        prev = run_bass_kernel_spmd(_prog("Bf" if last else "Bt"), in_b, core_ids=cores).results
        if last:
            xn = np.concatenate([np.asarray(r["xnT"]).T for r in prev], 0)
        else:
            xcur = np.concatenate([np.asarray(r["x2T"]).T for r in prev], 0)
    return np.ascontiguousarray(xn[None].astype(f32))
```

```python
import numpy as np
import ml_dtypes
import concourse.bass as bass
import concourse.mybir as mybir
from concourse.bass_utils import run_bass_kernel_spmd

F32 = mybir.dt.float32
BF16 = mybir.dt.bfloat16
AF = mybir.ActivationFunctionType
ALU = mybir.AluOpType
AX = mybir.AxisListType

NCORES = 8
D = 2048
SEQ = 8192
DEPTH = 4
T = SEQ // NCORES
KC = D // 128
NH = T // 512
DFF = 5632
EPS = 1e-6
HEADS = 8
DH = 128
NBLK = 32
GH = 4
GDK = 128
GDV = 256
CH = 64
C_QA, C_KA, C_VA, C_QG, C_KG, C_VG, C_RG, C_AD, C_B, C_C, C_U, C_M = (
    0, 1024, 2048, 3072, 3584, 4096, 5120, 6144, 6160, 7184, 8208, 9232)
NA_COLS = 2048 + 1536 + 16
NB_COLS = 1024 + 512 + 1024 + 3072 + 6144


class Res:
    __slots__ = ("name", "w", "r")

    def __init__(self, name=""):
        self.name = name
        self.w = None
        self.r = {}


class Chan:
    def __init__(self, sch, name):
        self.key = "c_" + name
        self.sem = sch.nc.alloc_semaphore(name=self.key)
        self.cnt = 0
        sch.semobj[self.key] = self.sem


class Sched:
    def __init__(self, nc):
        self.nc = nc
        self.eng = {"pe": nc.tensor, "act": nc.scalar, "dve": nc.vector, "pool": nc.gpsimd,
                    "sp": nc.sync}
        self.semobj = {}
        self.cnt = {}
        self.seen = {k: {} for k in self.eng}
        for k in self.eng:
            self.semobj[k] = nc.alloc_semaphore(name="s_" + k)
            self.cnt[k] = 0
        self.out_tickets = []

    def chan(self, name):
        return Chan(self, name)

    def _wait(self, e, key, val):
        if key == "pe" and e == "pe":
            return
        if self.seen[e].get(key, 0) >= val:
            return
        self.eng[e].wait_ge(self.semobj[key], val)
        self.seen[e][key] = val

    def _deps(self, e, reads, writes):
        for r in reads:
            if r.w is not None:
                self._wait(e, *r.w)
        for w in writes:
            if w.w is not None:
                self._wait(e, *w.w)
            for k, v in w.r.items():
                self._wait(e, k, v)

    def _mark(self, t, reads, writes):
        for r in reads:
            if r.r.get(t[0], 0) < t[1]:
                r.r[t[0]] = t[1]
        for w in writes:
            w.w = t
            w.r = {}

    def op(self, e, fn, reads=(), writes=()):
        self._deps(e, reads, writes)
        ins = fn(self.eng[e])
        self.cnt[e] += 1
        ins.then_inc(self.semobj[e], 1)
        t = (e, self.cnt[e])
        self._mark(t, reads, writes)
        return t

    def dma(self, q, chan, out, in_, reads=(), writes=(), is_output=False):
        self._deps(q, reads, writes)
        ins = self.eng[q].dma_start(out=out, in_=in_)
        chan.cnt += 16
        ins.then_inc(chan.sem, 16)
        t = (chan.key, chan.cnt)
        self._mark(t, reads, writes)
        if is_output:
            self.out_tickets.append(t)
        return t

    def finish(self):
        last = {}
        for k, v in self.out_tickets:
            last[k] = max(last.get(k, 0), v)
        for k, v in last.items():
            self._wait("sp", k, v)


class Ctx:
    def __init__(self, nc):
        self.nc = nc
        self.S = Sched(nc)
        self.stack = []

    def sb(self, name, shape, dt):
        g = self.nc.sbuf_tensor(name, shape, dt)
        t = g.__enter__()
        self.stack.append(g)
        return t

    def ps(self, name, shape, dt):
        g = self.nc.psum_tensor(name, shape, dt)
        t = g.__enter__()
        self.stack.append(g)
        return t

    def close(self):
        for g in reversed(self.stack):
            g.__exit__(None, None, None)
        self.stack = []


def setup_common(cx):
    nc, S = cx.nc, cx.S
    cx.bank = [cx.ps("bank%d" % i, [128, 512], F32) for i in range(8)]
    cx.bankres = [Res("bank%d" % i) for i in range(8)]
    cx.NW = 3
    cx.NWF = 2
    cx.wf = [cx.sb("wf%d" % i, [128, 8, 256], F32) for i in range(cx.NWF)]
    cx.wb = [cx.sb("wb%d" % i, [128, 8, 256], BF16) for i in range(cx.NW)]
    cx.wfres = [Res("wf%d" % i) for i in range(cx.NWF)]
    cx.wbres = [Res("wb%d" % i) for i in range(cx.NW)]
    cx.wchan = [S.chan("w%d" % i) for i in range(cx.NWF)]
    cx.wcount = 0
    cx.gcount = 0
    cx.ones = cx.sb("ones_bf", [128, 128], BF16)
    cx.onesres = Res("ones")
    S.op("pool", lambda e: e.memset(cx.ones[:], 1.0), writes=[cx.onesres])
    cx.epi_flip = 0


def dense_fm(cx, W, col0, ncols, K, act, actres, epi, banks=None, extra=None):
    nc, S = cx.nc, cx.S
    kcs = K // 128
    pieces = [(k0, min(8, kcs - k0)) for k0 in range(0, kcs, 8)]
    ngroups = (ncols + 255) // 256
    for g in range(ngroups):
        gw = min(256, ncols - g * 256)
        if banks is None:
            nsets = len(cx.bank_ids) // 4
            bsel = cx.bank_ids[(cx.gcount % nsets) * 4:(cx.gcount % nsets) * 4 + 4]
        else:
            bsel = banks
        cx.gcount += 1
        nocs = (gw + 127) // 128
        for pi, (k0, kcn) in enumerate(pieces):
            slot = cx.wcount % cx.NW
            fs = cx.wcount % cx.NWF
            cx.wcount += 1
            src = W[k0 * 128:(k0 + kcn) * 128, col0 + g * 256:col0 + g * 256 + gw]
            S.dma("sp", cx.wchan[fs], out=cx.wf[fs][:, :kcn, :gw],
                  in_=src.rearrange("(kc p) n -> p kc n", p=128), writes=[cx.wfres[fs]])
            if cx.wcount % 2 == 0:
                S.op("dve", lambda e: e.tensor_copy(out=cx.wb[slot][:, :kcn, :gw],
                                                    in_=cx.wf[fs][:, :kcn, :gw]),
                     reads=[cx.wfres[fs]], writes=[cx.wbres[slot]])
            else:
                S.op("act", lambda e: e.copy(out=cx.wb[slot][:, :kcn, :gw],
                                             in_=cx.wf[fs][:, :kcn, :gw]),
                     reads=[cx.wfres[fs]], writes=[cx.wbres[slot]])
            for oc in range(nocs):
                cw = min(128, gw - oc * 128)
                for half in range(NH):
                    b = bsel[oc * NH + half]
                    for kc in range(kcn):
                        first = (pi == 0 and kc == 0)
                        last = (pi == len(pieces) - 1 and kc == kcn - 1)
                        S.op("pe", lambda e: e.matmul(
                            cx.bank[b][:cw, :], lhsT=cx.wb[slot][:, kc, oc * 128:oc * 128 + cw],
                            rhs=act[:, k0 + kc, half * 512:(half + 1) * 512],
                            start=first, stop=last),
                            reads=[cx.wbres[slot], actres[k0 + kc] if isinstance(actres, list) else actres],
                            writes=[cx.bankres[b]])
                if extra is not None:
                    xr, xrres, xb, xepi = extra
                    for kc in range(kcn):
                        first = (pi == 0 and kc == 0)
                        last = (pi == len(pieces) - 1 and kc == kcn - 1)
                        S.op("pe", lambda e: e.matmul(
                            cx.bank[xb[oc]][:cw, 0:2],
                            lhsT=cx.wb[slot][:, kc, oc * 128:oc * 128 + cw],
                            rhs=xr[:, k0 + kc, :], start=first, stop=last),
                            reads=[cx.wbres[slot], xrres], writes=[cx.bankres[xb[oc]]])
        for oc in range(nocs):
            cw = min(128, gw - oc * 128)
            for half in range(NH):
                b = bsel[oc * NH + half]
                epi(g * 256 + oc * 128, cw, half, cx.bank[b][:cw, :], cx.bankres[b])
            if extra is not None:
                xr, xrres, xb, xepi = extra
                xepi(g * 256 + oc * 128, cw, cx.bank[xb[oc]][:cw, 0:2], cx.bankres[xb[oc]])


def norm_fm(cx, xsrc, wcol, wres, hT, hres, xring, xres, xchan, sq, sqres, rstd, rstdres, sbanks):
    nc, S = cx.nc, cx.S
    nx = len(xring)
    for c in range(KC):
        sl = c % nx
        S.dma("sp", xchan[sl], out=xring[sl][:], in_=xsrc[c * 128:(c + 1) * 128, :],
              writes=[xres[sl]])
        q = c % 2
        S.op("act", lambda e: e.activation(out=sq[q][:], in_=xring[sl][:], func=AF.Square),
             reads=[xres[sl]], writes=[sqres[q]])
        S.op("dve", lambda e: e.tensor_copy(out=hT[:, c, :], in_=xring[sl][:]),
             reads=[xres[sl]], writes=[hres[c]])
        for half in range(NH):
            b = sbanks[half]
            S.op("pe", lambda e: e.matmul(cx.bank[b][:, :], lhsT=cx.ones[:, :],
                                          rhs=sq[q][:, half * 512:(half + 1) * 512],
                                          start=(c == 0), stop=(c == KC - 1)),
                 reads=[cx.onesres, sqres[q]], writes=[cx.bankres[b]])
    norm_finish(cx, sbanks, rstd, rstdres, D)
    for c in range(KC):
        S.op("dve", lambda e: e.scalar_tensor_tensor(out=hT[:, c, :], in0=hT[:, c, :],
                                                   scalar=wcol[:, c:c + 1], in1=rstd[:, :],
                                                   op0=ALU.mult, op1=ALU.mult),
             reads=[hres[c], wres, rstdres], writes=[hres[c]])


def norm_finish(cx, sbanks, rstd, rstdres, n):
    S = cx.S
    for half in range(NH):
        b = sbanks[half]
        hs = slice(half * 512, (half + 1) * 512)
        S.op("dve", lambda e: e.tensor_scalar(out=rstd[:, hs], in0=cx.bank[b][:, :],
                                              scalar1=1.0 / n, scalar2=EPS, op0=ALU.mult,
                                              op1=ALU.add),
             reads=[cx.bankres[b]], writes=[rstdres])
        S.op("act", lambda e: e.sqrt(out=rstd[:, hs], in_=rstd[:, hs]),
             reads=[rstdres], writes=[rstdres])
        S.op("dve", lambda e: e.reciprocal(out=rstd[:, hs], in_=rstd[:, hs]),
             reads=[rstdres], writes=[rstdres])


def build_A():
    nc = bass.Bass("TRN2", target_bir_lowering=False)
    xT = nc.dram_tensor("xT", [D, T], F32, kind="ExternalInput").ap()
    nw = nc.dram_tensor("nw", [128, KC], F32, kind="ExternalInput").ap()
    wa = nc.dram_tensor("wa", [D, NA_COLS], F32, kind="ExternalInput").ap()
    ya = nc.dram_tensor("ya", [3584, T], BF16, kind="ExternalOutput").ap()
    yad = nc.dram_tensor("yad", [16, T], BF16, kind="ExternalOutput").ap()
    hTo = nc.dram_tensor("hTo", [128, KC, T], BF16, kind="ExternalOutput").ap()
    cx = Ctx(nc)
    S = cx.S
    setup_common(cx)
    cx.bank_ids = [0, 1, 2, 3, 4, 5, 6, 7]
    hT = cx.sb("hT", [128, KC, T], BF16)
    hres = [Res("hT%d" % i) for i in range(KC)]
    wcol = cx.sb("wcol", [128, KC], F32)
    wres = Res("wcol")
    cchan = S.chan("const")
    S.dma("sp", cchan, out=wcol[:], in_=nw[:, :], writes=[wres])
    xring = [cx.sb("xr%d" % i, [128, T], F32) for i in range(3)]
    xres = [Res("xr%d" % i) for i in range(3)]
    xchan = [S.chan("x%d" % i) for i in range(3)]
    sq = [cx.sb("sq%d" % i, [128, T], BF16) for i in range(2)]
    sqres = [Res("sq%d" % i) for i in range(2)]
    rstd = cx.sb("rstd", [128, T], F32)
    rstdres = Res("rstd")
    norm_fm(cx, xT, wcol, wres, hT, hres, xring, xres, xchan, sq, sqres, rstd, rstdres, [6, 7])
    hch = S.chan("hTo")
    for c4 in range(4):
        S.dma("pool", hch, out=hTo[:, c4 * 4:(c4 + 1) * 4, :], in_=hT[:, c4 * 4:(c4 + 1) * 4, :],
              reads=hres[c4 * 4:(c4 + 1) * 4], is_output=True)
    NO = 4
    ost = [cx.sb("ost%d" % i, [128, 512], BF16) for i in range(NO)]
    ores = [Res("ost%d" % i) for i in range(NO)]
    ochan = [S.chan("o%d" % i) for i in range(NO)]
    ostf = cx.sb("ostf", [16, T], BF16)
    ostfres = Res("ostf")
    st = {"n": 0}

    def epi_out(rowbase):
        def epi(c0, cw, half, ps, psres):
            i = st["n"] % NO
            st["n"] += 1
            eng = "act" if (st["n"] % 2) else "dve"
            if eng == "act":
                S.op("act", lambda e: e.copy(out=ost[i][:cw, :], in_=ps), reads=[psres],
                     writes=[ores[i]])
            else:
                S.op("dve", lambda e: e.tensor_copy(out=ost[i][:cw, :], in_=ps), reads=[psres],
                     writes=[ores[i]])
            S.dma("pool", ochan[i], out=ya[rowbase + c0:rowbase + c0 + cw,
                                         half * 512:(half + 1) * 512],
                  in_=ost[i][:cw, :], reads=[ores[i]], is_output=True)
        return epi

    dense_fm(cx, wa, 0, 2048, D, hT, hres, epi_out(0))
    dense_fm(cx, wa, 2048, 1536, D, hT, hres, epi_out(2048))

    def epi_ad(c0, cw, half, ps, psres):
        S.op("act", lambda e: e.copy(out=ostf[:cw, half * 512:(half + 1) * 512], in_=ps),
             reads=[psres], writes=[ostfres])
    dense_fm(cx, wa, 3584, 16, D, hT, hres, epi_ad)
    S.dma("sp", cchan, out=yad[:, :], in_=ostf[:, :], reads=[ostfres], is_output=True)
    S.finish()
    cx.close()
    return nc


class Arena:
    def __init__(self, tile, lo, hi):
        self.t, self.lo, self.hi, self.p = tile, lo, hi, lo

    def take(self, n):
        a = self.p
        self.p += n
        assert self.p <= self.hi, (self.p, self.hi)
        return self.t[:, a:a + n]

    def reset(self):
        self.p = self.lo


def barrier(S, chans):
    for e in S.eng:
        for f in S.eng:
            if f != e and S.cnt[f] > 0:
                S._wait(e, f, S.cnt[f])
        for ch in chans:
            if ch.cnt > 0:
                S._wait(e, ch.key, ch.cnt)


SLOPES = [2.0 ** (-(h + 1)) for h in range(HEADS)]
QK_SCALE = 1.0 / float(np.sqrt(DH))
BIGD = 1.0e6
NEG = -1.0e30


def host_consts():
    cf = np.zeros((128, CF_TOTAL), np.float32)
    k = np.arange(128)[:, None].astype(np.float64)
    for h in range(HEADS):
        for m in range(-3, 64):
            cf[:, CF_BIAS + h * 67 + m + 3] = (SLOPES[h] * (k - 128.0 * m))[:, 0]
    m = np.zeros((8, 32), np.float32)
    own = np.zeros((8, 32), np.float32)
    for qs in range(8):
        m[qs, 28 + qs // 2:] = NEG
        own[qs, 28 + qs // 2] = 1.0
    cf[:, CF_CMASK:CF_CMASK + 256] = m.reshape(1, 256)
    cf[:, CF_OWN:CF_OWN + 256] = own.reshape(1, 256)
    s_ = np.arange(64)[:, None]
    t_ = np.arange(64)[None, :]
    cf[:64, CF_TRI16:CF_TRI16 + 64] = np.where(s_ <= t_, -1.0 / 16.0, 0.0)
    cf[:64, CF_SU16:CF_SU16 + 64] = np.where(s_ > t_, -1.0 / 16.0, 0.0)
    cf[:64, CF_TRI1:CF_TRI1 + 256] = np.tile((s_ <= t_).astype(np.float32), (1, 4))
    cf[:64, CF_NC] = -1.0 / 16.0
    cf[np.arange(128), CF_ID + np.arange(128)] = 1.0
    return cf


def host_masks():
    k = np.arange(128)[:, None]
    q = np.arange(512)[None, :]
    m = np.zeros((128, 4 * 512 + 128), np.float32)
    for i in range(4):
        m[:, i * 512:(i + 1) * 512] = (q >= 128 * i + k)
    m[:, 2048:2176] = (q[:, :128] >= k)
    return m.astype(ml_dtypes.bfloat16)


CF_BIAS = 0
CF_CMASK = 8 * 67
CF_OWN = CF_CMASK + 256
CF_TRI16 = CF_OWN + 256
CF_SU16 = CF_TRI16 + 64
CF_TRI1 = CF_SU16 + 64
CF_NC = CF_TRI1 + 256
CF_ONES = CF_NC + 8
CF_ID = CF_ONES + 128
CF_TOTAL = CF_ID + 128


def attention_phase(cx, qT, qres, KT_d, V_d, oT, ores, ar_b, ar_f, cstf, cstres, ident, blkm, chans,
                    cb16, dbgf=None):
    nc, S = cx.nc, cx.S
    kts = [ar_b.take(8192) for _ in range(2)]
    ktres = [Res("kt%d" % i) for i in range(2)]
    ktchan = [S.chan("kt%d" % i) for i in range(2)]
    NV = 3
    vts = [ar_b.take(16 * 129).rearrange("p (a b) -> p a b", a=16) for _ in range(NV)]
    vres = [Res("v%d" % i) for i in range(NV)]
    vchan = [S.chan("v%d" % i) for i in range(NV)]
    NP = 4
    pts = [ar_b.take(512) for _ in range(NP)]
    pres = [[Res("pt%d_%d" % (i, j)) for j in range(4)] for i in range(NP)]
    kmT = ar_b.take(32)
    kmres = Res("km")
    onorm = [ar_f.take(128) for _ in range(2)]
    onres = [Res("on%d" % i) for i in range(2)]
    chans += ktchan + vchan
    acc = ar_f.take(8 * 129).rearrange("p (a b) -> p a b", a=8)
    accres = [Res("acc%d" % i) for i in range(8)]
    gm = ar_f.take(256)
    gmres = Res("gm")
    sel = ar_f.take(256)
    selres = Res("sel")
    valid = ar_f.take(256)
    mfull = ar_f.take(256)
    mres = Res("mfull")
    top8 = ar_f.take(8)
    top8res = Res("top8")
    kms = ar_f.take(32)
    kmsres = Res("kms")
    rec = ar_f.take(8)
    recres = Res("rec")
    sb_ids = [0, 1]
    ob_ids = [[2, 3], [4, 5]]
    gate_b = 6
    tr_b = 7
    S.op("dve", lambda e: e.tensor_tensor(
        out=mfull.rearrange("p (a b) -> p a b", a=8),
        in0=cstf[:, CF_CMASK:CF_CMASK + 256].rearrange("p (a b) -> p a b", a=8),
        in1=blkm.rearrange("p (a b) -> p a b", a=8), op=ALU.add),
        reads=[cstres], writes=[mres])
    S.op("dve", lambda e: e.tensor_scalar(out=valid, in0=mfull, scalar1=-1.0e29, scalar2=None,
                                          op0=ALU.is_gt), reads=[mres], writes=[mres])
    pcount = 0
    tcount = 0
    vcount = 0
    ocount = 0
    sel2 = [sel, ar_f.take(256)]
    selres2 = [selres, Res("sel1")]

    def gating(h):
        ks = h % 2
        sel = sel2[h % 2]
        selres = selres2[h % 2]
        S.dma("sp", ktchan[ks], out=kts[ks], in_=KT_d[h], writes=[ktres[ks]])
        S.op("dve", lambda e: e.tensor_reduce(out=kms, in_=kts[ks].rearrange("p (a b) -> p a b", a=32),
                                              axis=AX.X, op=ALU.add),
             reads=[ktres[ks]], writes=[kmsres])
        S.op("act", lambda e: e.mul(out=kmT, in_=kms, mul=1.0 / 256.0), reads=[kmsres],
             writes=[kmres])
        for qs in range(8):
            S.op("pe", lambda e: e.matmul(cx.bank[gate_b][:, qs * 32:(qs + 1) * 32],
                                          lhsT=qT[:, h, qs * 128:(qs + 1) * 128], rhs=kmT,
                                          start=True, stop=True),
                 reads=[qres, kmres], writes=[cx.bankres[gate_b]])
        S.op("dve", lambda e: e.tensor_tensor(out=gm, in0=cx.bank[gate_b][:, 0:256], in1=mfull,
                                              op=ALU.add),
             reads=[cx.bankres[gate_b], mres], writes=[gmres])
        for qs in range(8):
            S.op("dve", lambda e: e.max(out=top8, in_=gm[:, qs * 32:(qs + 1) * 32]),
                 reads=[gmres], writes=[top8res])
            S.op("dve", lambda e: e.scalar_tensor_tensor(
                out=sel[:, qs * 32:(qs + 1) * 32], in0=gm[:, qs * 32:(qs + 1) * 32],
                scalar=top8[:, 2:3], in1=valid[:, qs * 32:(qs + 1) * 32], op0=ALU.is_ge,
                op1=ALU.mult), reads=[gmres, top8res, mres], writes=[selres])
        S.op("dve", lambda e: e.tensor_tensor(out=sel, in0=sel, in1=cstf[:, CF_OWN:CF_OWN + 256],
                                              op=ALU.add), reads=[selres, cstres],
             writes=[selres])

    gating(0)
    for h in range(HEADS):
        ks = h % 2
        sel = sel2[h % 2]
        selres = selres2[h % 2]
        if h + 1 < HEADS:
            gating(h + 1)
        for qs in range(8):
            S.op("pool", lambda e: e.memset(acc[:, qs, :], 0.0), writes=[accres[qs]])
        nslope = -SLOPES[h]
        blocks = [(g, s_) for g in range(2) for s_ in range(28 + 2 * g + 2)]
        bstate = {}

        def stage1(bi):
            nonlocal pcount, tcount, vcount, ocount
            g, s_ = blocks[bi]
            qpos0 = 28 * 256 + g * 512
            pis, vsl = [], []
            for kk in range(2):
                kt = 2 * s_ + kk
                if kt % 16 == 0:
                    vs = vcount % NV
                    vcount += 1
                    S.dma("sp", vchan[vs], out=vts[vs], in_=V_d[h, :, kt:kt + 16, :],
                          writes=[vres[vs]])
                    bstate["cur_v"] = vs
                sbk = sb_ids[tcount % 2]
                S.op("pe", lambda e: e.matmul(cx.bank[sbk][:, :],
                                              lhsT=kts[ks][:, kt * 128:(kt + 1) * 128],
                                              rhs=qT[:, h, g * 512:(g + 1) * 512],
                                              start=True, stop=True),
                     reads=[ktres[ks], qres], writes=[cx.bankres[sbk]])
                m0 = (qpos0 - kt * 128) // 128
                tcount += 1
                pi = pcount % NP
                pcount += 1
                if h >= 3:
                    S.op("act", lambda e: e.activation(
                        out=pts[pi], in_=cx.bank[sbk][:, :], func=AF.Exp,
                        bias=cstf[:, CF_BIAS + h * 67 + m0 + 3:CF_BIAS + h * 67 + m0 + 4],
                        scale=QK_SCALE), reads=[cx.bankres[sbk], cstres], writes=pres[pi])
                    if m0 <= 0:
                        i_ = -m0
                        S.op("pool", lambda e: e.tensor_tensor(
                            out=pts[pi], in0=pts[pi], in1=cb16[:, i_ * 512:(i_ + 1) * 512],
                            op=ALU.mult), reads=pres[pi] + [cstres], writes=pres[pi])
                elif h >= 1:
                    for hq in range(2):
                        mh = m0 + 2 * hq
                        dst = pts[pi][:, hq * 256:(hq + 1) * 256]
                        rr = pres[pi][2 * hq:2 * hq + 2]
                        if mh < -1:
                            S.op("pool", lambda e: e.memset(dst, 0.0), writes=rr)
                            continue
                        S.op("act", lambda e: e.activation(
                            out=dst, in_=cx.bank[sbk][:, hq * 256:(hq + 1) * 256], func=AF.Exp,
                            bias=cstf[:, CF_BIAS + h * 67 + mh + 3:CF_BIAS + h * 67 + mh + 4],
                            scale=QK_SCALE), reads=[cx.bankres[sbk], cstres], writes=rr)
                        if mh <= 0:
                            i_ = -mh
                            S.op("pool", lambda e: e.tensor_tensor(
                                out=dst, in0=dst, in1=cb16[:, i_ * 512:i_ * 512 + 256],
                                op=ALU.mult), reads=rr + [cstres], writes=rr)
                else:
                    for qq in range(4):
                        mq = m0 + qq
                        dst = pts[pi][:, qq * 128:(qq + 1) * 128]
                        if mq < 0:
                            S.op("pool", lambda e: e.memset(dst, 0.0), writes=[pres[pi][qq]])
                            continue
                        S.op("act", lambda e: e.activation(
                            out=dst, in_=cx.bank[sbk][:, qq * 128:(qq + 1) * 128], func=AF.Exp,
                            bias=cstf[:, CF_BIAS + h * 67 + mq + 3:CF_BIAS + h * 67 + mq + 4],
                            scale=QK_SCALE), reads=[cx.bankres[sbk], cstres],
                            writes=[pres[pi][qq]])
                        if mq == 0:
                            S.op("pool", lambda e: e.tensor_tensor(
                                out=dst, in0=dst, in1=cb16[:, 2048:2176], op=ALU.mult),
                                reads=[pres[pi][qq], cstres], writes=[pres[pi][qq]])
                pis.append(pi)
                vsl.append((bstate["cur_v"], kt % 16))
            bstate[bi] = (pis, vsl)

        def stage2(bi):
            nonlocal ocount
            g, s_ = blocks[bi]
            pis, vsl = bstate.pop(bi)
            obs = ob_ids[ocount % 2]
            ocount += 1
            for qq in range(4):
                ob = obs[qq // 2]
                for kk in range(2):
                    S.op("pe", lambda e: e.matmul(
                        cx.bank[ob][:, (qq % 2) * 129:(qq % 2) * 129 + 129],
                        lhsT=pts[pis[kk]][:, qq * 128:(qq + 1) * 128],
                        rhs=vts[vsl[kk][0]][:, vsl[kk][1], :], start=(kk == 0), stop=(kk == 1)),
                        reads=[pres[pis[kk]][qq], vres[vsl[kk][0]]], writes=[cx.bankres[ob]])
            for qq in range(4):
                ob = obs[qq // 2]
                qs = g * 4 + qq
                S.op("dve", lambda e: e.scalar_tensor_tensor(
                    out=acc[:, qs, :], in0=cx.bank[ob][:, (qq % 2) * 129:(qq % 2) * 129 + 129],
                    scalar=sel[:, qs * 32 + s_:qs * 32 + s_ + 1], in1=acc[:, qs, :],
                    op0=ALU.mult, op1=ALU.add),
                    reads=[cx.bankres[ob], selres, accres[qs]], writes=[accres[qs]])

        stage1(0)
        for bi in range(len(blocks)):
            if bi + 1 < len(blocks):
                stage1(bi + 1)
            stage2(bi)
        if dbgf is not None and h == HEADS - 1:
            dch = S.chan("dbgf")
            S.dma("sp", dch, out=dbgf[:, 0:256], in_=sel, reads=[selres], is_output=True)
            S.dma("sp", dch, out=dbgf[:, 256:256 + 1032], in_=acc.rearrange("p a b -> p (a b)"),
                  reads=accres, is_output=True)
            S.dma("sp", dch, out=dbgf[:, 1288:1288 + 256], in_=gm, reads=[gmres], is_output=True)
            S.dma("sp", dch, out=dbgf[:, 1544:1552], in_=top8, reads=[top8res], is_output=True)
            S.dma("sp", dch, out=dbgf[:, 1552:1552 + 32], in_=kms, reads=[kmsres], is_output=True)
        S.op("dve", lambda e: e.reciprocal(out=rec, in_=acc[:, :, 128]),
             reads=accres, writes=[recres])
        for half in range(2):
            for qq in range(4):
                qs = half * 4 + qq
                oi = qs % 2
                S.op("dve", lambda e: e.tensor_scalar(out=onorm[oi], in0=acc[:, qs, 0:128],
                                                      scalar1=rec[:, qs:qs + 1], scalar2=None,
                                                      op0=ALU.mult),
                     reads=[accres[qs], recres], writes=[onres[oi]])
                S.op("pe", lambda e: e.transpose(out=cx.bank[tr_b][:, qq * 128:(qq + 1) * 128],
                                                 in_=onorm[oi], identity=ident),
                     reads=[onres[oi], cstres], writes=[cx.bankres[tr_b]])
            S.op("act", lambda e: e.copy(out=oT[:, h, half * 512:(half + 1) * 512],
                                         in_=cx.bank[tr_b][:, :]),
                 reads=[cx.bankres[tr_b]], writes=[ores])


def build_B(stop_after=None, tail=False):
    nc = bass.Bass("TRN2", target_bir_lowering=False)
    dt = nc.dram_tensor
    xT = dt("xT", [D, T], F32, kind="ExternalInput").ap()
    hTi = dt("hTi", [128, KC, T], BF16, kind="ExternalInput").ap()
    hhi = dt("hhi", [128, KC, 2], BF16, kind="ExternalInput").ap()
    nws = dt("nws", [128, 3 * KC], F32, kind="ExternalInput").ap()
    wb_ = dt("wb", [D, NB_COLS], F32, kind="ExternalInput").ap()
    cst = dt("cst", [128, CF_TOTAL], F32, kind="ExternalInput").ap()
    blk = dt("blk", [128, 256], F32, kind="ExternalInput").ap()
    msk_d = dt("msk", [128, 2176], BF16, kind="ExternalInput").ap()
    KT_d = dt("KT", [HEADS, 128, SEQ], BF16, kind="ExternalInput").ap()
    V_d = dt("VA", [HEADS, 128, 64, 129], BF16, kind="ExternalInput").ap()
    kg_d = dt("kgt", [64, 128, 512], BF16, kind="ExternalInput").ap()
    vg_d = dt("vgt", [64, 128, 1024], BF16, kind="ExternalInput").ap()
    ad_d = dt("adT", [17, SEQ], BF16, kind="ExternalInput").ap()
    wup_d = dt("wup", [17, 512], F32, kind="ExternalInput").ap()
    kgT_d = dt("kgT", [128, 4, T], BF16, kind="ExternalInput").ap()
    gnw_d = dt("gnw", [128, 2], F32, kind="ExternalInput").ap()
    cw_d = dt("cw", [128, 8, 3], F32, kind="ExternalInput").ap()
    wbr = [dt("wbr%d" % i, [1024, D], F32, kind="ExternalInput").ap() for i in range(3)]
    w_out = dt("w_out", [D, D], F32, kind="ExternalInput").ap()
    w_g = dt("w_g", [D, DFF], F32, kind="ExternalInput").ap()
    w_u = dt("w_u", [D, DFF], F32, kind="ExternalInput").ap()
    w_d = dt("w_d", [DFF, D], F32, kind="ExternalInput").ap()
    x1T = dt("x1T", [D, T], F32).ap()
    if stop_after is None:
        x2T = dt("x2T", [D, T], F32, kind="ExternalOutput").ap()
        if tail:
            wan = dt("wan", [D, NA_COLS], F32, kind="ExternalInput").ap()
            ya = dt("ya", [3584, T], BF16, kind="ExternalOutput").ap()
            yad = dt("yad", [16, T], BF16, kind="ExternalOutput").ap()
            hTo = dt("hTo", [128, KC, T], BF16, kind="ExternalOutput").ap()
        else:
            xnT = dt("xnT", [D, T], F32, kind="ExternalOutput").ap()
    else:
        dbg = dt("dbg", [128, 8 * T], BF16, kind="ExternalOutput").ap()
        dbgf = dt("dbgf", [128, 2048], F32, kind="ExternalOutput").ap()
    cx = Ctx(nc)
    S = cx.S
    setup_common(cx)
    cx.bank_ids = [0, 1, 2, 3, 4, 5, 6, 7]
    chans = []
    cchan = S.chan("const")
    cstf = cx.sb("cstf", [128, CF_TOTAL], F32)
    cstres = Res("cst")
    S.dma("sp", cchan, out=cstf[:], in_=cst[:, :], writes=[cstres])
    cb16 = cx.sb("cb16", [128, 2176], BF16)
    S.dma("sp", cchan, out=cb16[:], in_=msk_d[:, :], writes=[cstres])
    blkm = cx.sb("blkm", [128, 256], F32)
    S.dma("sp", cchan, out=blkm[:], in_=blk[:, :], writes=[cstres])
    nwt = cx.sb("nwt", [128, 3 * KC], F32)
    S.dma("sp", cchan, out=nwt[:], in_=nws[:, :], writes=[cstres])
    gnw = cx.sb("gnw_sb", [128, 2], F32)
    S.dma("sp", cchan, out=gnw[:], in_=gnw_d[:, :], writes=[cstres])
    cwt = cx.sb("cwt_sb", [128, 8, 3], F32)
    S.dma("sp", cchan, out=cwt[:], in_=cw_d[:, :, :], writes=[cstres])
    ident = cstf[:, CF_ID:CF_ID + 128]
    xring = [cx.sb("xr%d" % i, [128, T], F32) for i in range(2)]
    xres = [Res("xr%d" % i) for i in range(2)]
    xchan = [S.chan("x%d" % i) for i in range(2)]
    sq = [cx.sb("sq%d" % i, [128, T], BF16) for i in range(2)]
    sqres = [Res("sq%d" % i) for i in range(2)]
    rstd = cx.sb("rstd", [128, T], F32)
    rstdres = Res("rstd")
    hT = cx.sb("hT", [128, KC, T], BF16)
    hres = [Res("hT%d" % i) for i in range(KC)]
    bigb = cx.sb("bigb", [128, 44 * T], BF16)
    fscr = cx.sb("fscr", [128, 6144], F32)
    chans += [cchan] + xchan + cx.wchan

    def rows(a, n):
        return bigb[:, a * T:(a + n) * T]

    oT_a = rows(0, 8).rearrange("p (a b) -> p a b", a=8)
    oares = Res("oT_a")
    qT = rows(8, 8).rearrange("p (a b) -> p a b", a=8)
    qres = Res("qT")
    hich = [S.chan("hTi%d" % i) for i in range(4)]
    chans += hich
    for c4 in range(4):
        S.dma("sp", hich[c4], out=hT[:, c4 * 4:(c4 + 1) * 4, :],
              in_=hTi[:, c4 * 4:(c4 + 1) * 4, :], writes=[hres[c4 * 4]])
        for j_ in range(1, 4):
            hres[c4 * 4 + j_].w = hres[c4 * 4].w
    flip = {"n": 0}

    def evac(out_ap, ps, psres, wres_list):
        flip["n"] += 1
        if flip["n"] % 2:
            S.op("act", lambda e: e.copy(out=out_ap, in_=ps), reads=[psres], writes=wres_list)
        else:
            S.op("dve", lambda e: e.tensor_copy(out=out_ap, in_=ps), reads=[psres],
                 writes=wres_list)

    def epi_q(c0, cw, half, ps, psres):
        evac(qT[:, c0 // 128, half * 512:(half + 1) * 512], ps, psres, [qres])
    dense_fm(cx, wb_, 0, 1024, D, hT, hres, epi_q)
    barrier(S, chans)
    ar_b = Arena(bigb, 16 * T, 44 * T)
    ar_f = Arena(fscr, 0, 6144)
    attention_phase(cx, qT, qres, KT_d, V_d, oT_a, oares, ar_b, ar_f, cstf, cstres, ident, blkm,
                    chans, cb16, dbgf=None)
    barrier(S, chans)
    if stop_after == "attn":
        och = S.chan("dbg")
        S.dma("sp", och, out=dbg[:, :], in_=rows(0, 8), reads=[oares], is_output=True)
        S.finish()
        cx.close()
        return nc
    ogT = rows(8, 8).rearrange("p (a b) -> p a b", a=8)
    ogres = Res("ogT")
    ar_b.reset()
    ar_f.reset()
    qgT = ar_b.take(4 * T).rearrange("p (a b) -> p a b", a=4)
    qgres = Res("qgT")
    kgT = ar_b.take(4 * T).rearrange("p (a b) -> p a b", a=4)
    kgres = Res("kgT")
    wup = ar_f.take(512)[0:17, :]
    wupres = Res("wup")
    gch = S.chan("gla_in")
    chans.append(gch)
    S.dma("sp", gch, out=kgT, in_=kgT_d[:, :, :], writes=[kgres])
    S.dma("sp", gch, out=wup, in_=wup_d[:, :], writes=[wupres])
    for r_ in (kgres, wupres):
        r_.w = (gch.key, gch.cnt)

    def epi_qg(c0, cw, half, ps, psres):
        evac(qgT[:, c0 // 128, half * 512:(half + 1) * 512], ps, psres, [qgres])
    dense_fm(cx, wb_, 1024, 512, D, hT, hres, epi_qg)
    barrier(S, chans)
    gla_phase(cx, qgT, qgres, kgT, kgres, kg_d, vg_d, ad_d, wup, wupres, ogT, ogres, ar_b, ar_f,
              cstf, cstres, chans)
    barrier(S, chans)
    if stop_after == "gla":
        och = S.chan("dbg")
        S.dma("sp", och, out=dbg[:, :], in_=rows(8, 8), reads=[ogres], is_output=True)
        S.finish()
        cx.close()
        return nc
    ar_b.reset()
    ar_f.reset()
    for h in range(GH):
        for half in range(2):
            q_ = half
            S.op("act", lambda e: e.activation(out=sq[q_][:], in_=ogT[:, 2 * h + half, :],
                                               func=AF.Square), reads=[ogres], writes=[sqres[q_]])
            for th in range(NH):
                S.op("pe", lambda e: e.matmul(cx.bank[6 + th][:, :], lhsT=cx.ones[:, :],
                                              rhs=sq[q_][:, th * 512:(th + 1) * 512],
                                              start=(half == 0), stop=(half == 1)),
                     reads=[cx.onesres, sqres[q_]], writes=[cx.bankres[6 + th]])
        norm_finish(cx, [6, 7], rstd, rstdres, GDV)
        for half in range(2):
            S.op("dve", lambda e: e.scalar_tensor_tensor(
                out=ogT[:, 2 * h + half, :], in0=ogT[:, 2 * h + half, :],
                scalar=gnw[:, half:half + 1], in1=rstd[:, :], op0=ALU.mult, op1=ALU.mult),
                reads=[ogres, cstres, rstdres], writes=[ogres])
    gtl = [ar_f.take(512) for _ in range(4)]
    gtres = [Res("gt%d" % i) for i in range(4)]
    gcnt = {"n": 0}

    def epi_rg(c0, cw, half, ps, psres):
        i = gcnt["n"] % 4
        gcnt["n"] += 1
        S.op("act", lambda e: e.activation(out=gtl[i], in_=ps, func=AF.Silu), reads=[psres],
             writes=[gtres[i]])
        dst = ogT[:, c0 // 128, half * 512:(half + 1) * 512]
        S.op("dve", lambda e: e.tensor_tensor(out=dst, in0=dst, in1=gtl[i], op=ALU.mult),
             reads=[gtres[i], ogres], writes=[ogres])
    dense_fm(cx, wb_, 1536, 1024, D, hT, hres, epi_rg)
    barrier(S, chans)
    if stop_after == "glapost":
        och = S.chan("dbg")
        S.dma("sp", och, out=dbg[:, :], in_=rows(8, 8), reads=[ogres], is_output=True)
        S.finish()
        cx.close()
        return nc
    yT = rows(16, 8).rearrange("p (a b) -> p a b", a=8)
    yres = Res("yT")
    cT = rows(24, 8).rearrange("p (a b) -> p a b", a=8)
    cres = Res("cT")
    cu = bigb[:, 32 * T:32 * T + 8 * (T + 2)].rearrange("p (a b) -> p a b", a=8)
    cures = Res("cu")
    ar_f.reset()
    ar_b.p = 41 * T + 64
    hh = ar_b.take(2 * KC).rearrange("p (a b) -> p a b", a=KC)
    hhres = Res("hh")
    hch = S.chan("halo")
    chans.append(hch)
    S.dma("sp", hch, out=hh, in_=hhi[:, :, :], writes=[hhres])
    chalo = ar_f.take(2 * 8).rearrange("p (a b) -> p a b", a=8)
    chres = Res("chalo")

    def epi_c(c0, cw, half, ps, psres):
        evac(cT[:, c0 // 128, half * 512:(half + 1) * 512], ps, psres, [cres])

    def xepi_c(c0, cw, ps, psres):
        S.op("act", lambda e: e.copy(out=chalo[:, c0 // 128, :], in_=ps), reads=[psres],
             writes=[chres])

    def epi_u(c0, cw, half, ps, psres):
        r_ = c0 // 128
        S.op("dve", lambda e: e.tensor_tensor(
            out=cu[:, r_, 2 + half * 512:2 + (half + 1) * 512], in0=ps,
            in1=cT[:, r_, half * 512:(half + 1) * 512], op=ALU.mult),
            reads=[psres, cres], writes=[cures])

    def xepi_u(c0, cw, ps, psres):
        r_ = c0 // 128
        S.op("dve", lambda e: e.tensor_tensor(out=cu[:, r_, 0:2], in0=ps, in1=chalo[:, r_, :],
                                              op=ALU.mult),
             reads=[psres, chres], writes=[cures])
    dense_fm(cx, wb_, 3584, 1024, D, hT, hres, epi_c, banks=[0, 1, 2, 3],
             extra=(hh, hhres, [4, 5], xepi_c))
    dense_fm(cx, wb_, 4608, 1024, D, hT, hres, epi_u, banks=[0, 1, 2, 3],
             extra=(hh, hhres, [4, 5], xepi_u))
    cacc = ar_f.take(T)
    caccres = Res("cacc")
    for r_ in range(8):
        S.op("dve", lambda e: e.tensor_scalar(out=cacc, in0=cu[:, r_, 2:T + 2],
                                              scalar1=cwt[:, r_, 2:3], scalar2=None,
                                              op0=ALU.mult),
             reads=[cures, cstres], writes=[caccres])
        S.op("dve", lambda e: e.scalar_tensor_tensor(out=cacc, in0=cu[:, r_, 1:T + 1],
                                                   scalar=cwt[:, r_, 1:2], in1=cacc,
                                                   op0=ALU.mult, op1=ALU.add),
             reads=[cures, caccres], writes=[caccres])
        S.op("dve", lambda e: e.scalar_tensor_tensor(out=yT[:, r_, :], in0=cu[:, r_, 0:T],
                                                   scalar=cwt[:, r_, 0:1], in1=cacc,
                                                   op0=ALU.mult, op1=ALU.add),
             reads=[cures, caccres], writes=[yres])

    def epi_b(c0, cw, half, ps, psres):
        dst = yT[:, c0 // 128, half * 512:(half + 1) * 512]
        S.op("dve", lambda e: e.tensor_tensor(out=dst, in0=ps, in1=dst, op=ALU.mult),
             reads=[psres, yres], writes=[yres])
    dense_fm(cx, wb_, 2560, 1024, D, hT, hres, epi_b)
    barrier(S, chans)
    if stop_after == "conv":
        och = S.chan("dbg")
        S.dma("sp", och, out=dbg[:, :], in_=rows(16, 8), reads=[yres], is_output=True)
        S.finish()
        cx.close()
        return nc
    mixedT = rows(24, 16).rearrange("p (a b) -> p a b", a=16)
    mixres = Res("mixedT")
    ar_f.reset()
    gt4 = [ar_f.take(512) for _ in range(4)]
    gt4res = [Res("g4_%d" % i) for i in range(4)]
    macc = [ar_f.take(512) for _ in range(4)]
    maccres = [Res("macc%d" % i) for i in range(4)]
    mtmp = [ar_f.take(512) for _ in range(2)]
    mtres = [Res("mtmp%d" % i) for i in range(2)]
    mcnt = {"n": 0}
    branches = [(wbr[0], oT_a, oares), (wbr[1], ogT, ogres), (wbr[2], yT, yres)]
    for jg in range(D // 256):
        for br, (wd, actb, actbres) in enumerate(branches):
            def epi_gate(c0, cw, half, ps, psres):
                i = (c0 // 128) * NH + half
                S.op("act", lambda e: e.activation(out=gt4[i], in_=ps, func=AF.Sigmoid),
                     reads=[psres], writes=[gt4res[i]])
            dense_fm(cx, wb_, 5632 + br * D + jg * 256, 256, D, hT, hres, epi_gate)

            def epi_br(c0, cw, half, ps, psres):
                i = (c0 // 128) * NH + half
                if br == 0:
                    S.op("dve", lambda e: e.tensor_tensor(out=macc[i], in0=ps, in1=gt4[i],
                                                          op=ALU.mult),
                         reads=[psres, gt4res[i]], writes=[maccres[i]])
                else:
                    k_ = mcnt["n"] % 2
                    mcnt["n"] += 1
                    S.op("dve", lambda e: e.tensor_tensor(out=mtmp[k_], in0=ps, in1=gt4[i],
                                                          op=ALU.mult),
                         reads=[psres, gt4res[i]], writes=[mtres[k_]])
                    if br == 1:
                        S.op("pool", lambda e: e.tensor_tensor(out=macc[i], in0=macc[i],
                                                               in1=mtmp[k_], op=ALU.add),
                             reads=[maccres[i], mtres[k_]], writes=[maccres[i]])
                    else:
                        S.op("pool", lambda e: e.tensor_tensor(
                            out=mixedT[:, jg * 2 + c0 // 128, half * 512:(half + 1) * 512],
                            in0=macc[i], in1=mtmp[k_], op=ALU.add),
                            reads=[maccres[i], mtres[k_]], writes=[mixres])
            dense_fm(cx, wd, jg * 256, 256, 1024, actb, actbres, epi_br)
    barrier(S, chans)
    cx.bank_ids = [0, 1, 2, 3, 6, 7, 0, 1, 2, 3, 6, 7]
    ar_f.reset()
    x1s = [ar_f.take(512) for _ in range(3)]
    x1res = [Res("x1s%d" % i) for i in range(3)]
    x1ch = [S.chan("x1o%d" % i) for i in range(3)]
    chans += x1ch
    st7 = {"n": 0, "slot": 0}

    def make_resid_epi(src, dst, want_h, is_out):
        def prefetch(r_):
            sl = r_ % 2
            S.dma("pool", xchan[sl], out=xring[sl][:], in_=src[r_ * 128:(r_ + 1) * 128, :],
                  writes=[xres[sl]])
        prefetch(0)

        def epi(c0, cw, half, ps, psres):
            r_ = c0 // 128
            if half == 0 and r_ + 1 < KC:
                prefetch(r_ + 1)
            sl = r_ % 2
            i = st7["n"] % 3
            st7["n"] += 1
            hs = slice(half * 512, (half + 1) * 512)
            S.op("dve", lambda e: e.tensor_tensor(out=x1s[i], in0=ps, in1=xring[sl][:, hs],
                                                  op=ALU.add),
                 reads=[psres, xres[sl]], writes=[x1res[i]])
            S.dma("pool", x1ch[i], out=dst[r_ * 128:(r_ + 1) * 128, hs], in_=x1s[i],
                  reads=[x1res[i]], is_output=is_out)
            q_ = st7["n"] % 2
            S.op("act", lambda e: e.activation(out=sq[q_][:, 0:512], in_=x1s[i], func=AF.Square),
                 reads=[x1res[i]], writes=[sqres[q_]])
            S.op("pe", lambda e: e.matmul(cx.bank[4 + half][:, :], lhsT=cx.ones[:, :],
                                          rhs=sq[q_][:, 0:512], start=(r_ == 0),
                                          stop=(r_ == KC - 1)),
                 reads=[cx.onesres, sqres[q_]], writes=[cx.bankres[4 + half]])
            if want_h:
                S.op("pool", lambda e: e.tensor_copy(out=hT[:, r_, hs], in_=x1s[i]),
                     reads=[x1res[i]], writes=[hres[r_]])
        return epi
    dense_fm(cx, w_out, 0, D, D, mixedT, mixres, make_resid_epi(xT, x1T, True, False))
    norm_finish(cx, [4, 5], rstd, rstdres, D)
    barrier(S, chans)
    for c in range(KC):
        S.op("dve", lambda e: e.scalar_tensor_tensor(out=hT[:, c, :], in0=hT[:, c, :],
                                                   scalar=nwt[:, KC + c:KC + c + 1],
                                                   in1=rstd[:, :], op0=ALU.mult, op1=ALU.mult),
             reads=[hres[c], cstres, rstdres], writes=[hres[c]])
    actT = bigb[:, :].rearrange("p (a b) -> p a b", a=44)
    actres = Res("actT")
    cx.bank_ids = [0, 1, 2, 3, 4, 5, 6, 7]
    ar_f.reset()
    gs4 = [ar_f.take(512) for _ in range(4)]
    gs4res = [Res("gs4_%d" % i) for i in range(4)]
    for fg in range(DFF // 256):
        def epi_g(c0, cw, half, ps, psres):
            i = (c0 // 128) * NH + half
            S.op("act", lambda e: e.activation(out=gs4[i], in_=ps, func=AF.Silu), reads=[psres],
                 writes=[gs4res[i]])
        dense_fm(cx, w_g, fg * 256, 256, D, hT, hres, epi_g)

        def epi_up(c0, cw, half, ps, psres):
            i = (c0 // 128) * NH + half
            S.op("dve", lambda e: e.tensor_tensor(
                out=actT[:, fg * 2 + c0 // 128, half * 512:(half + 1) * 512], in0=ps, in1=gs4[i],
                op=ALU.mult), reads=[psres, gs4res[i]], writes=[actres])
        dense_fm(cx, w_u, fg * 256, 256, D, hT, hres, epi_up)
    barrier(S, chans)
    cx.bank_ids = [0, 1, 2, 3, 6, 7, 0, 1, 2, 3, 6, 7]
    st7["n"] = 0
    x1s_ = x1s
    dense_fm(cx, w_d, 0, D, DFF, actT, actres, make_resid_epi(x1T, x2T, tail, True))
    norm_finish(cx, [4, 5], rstd, rstdres, D)
    barrier(S, chans)
    if tail:
        for c in range(KC):
            S.op("dve", lambda e: e.scalar_tensor_tensor(out=hT[:, c, :], in0=hT[:, c, :],
                                                       scalar=nwt[:, c:c + 1], in1=rstd[:, :],
                                                       op0=ALU.mult, op1=ALU.mult),
                 reads=[hres[c], cstres, rstdres], writes=[hres[c]])
        hoch = S.chan("hTo")
        for c4 in range(4):
            S.dma("pool", hoch, out=hTo[:, c4 * 4:(c4 + 1) * 4, :],
                  in_=hT[:, c4 * 4:(c4 + 1) * 4, :], reads=hres[c4 * 4:(c4 + 1) * 4],
                  is_output=True)
        cx.bank_ids = [0, 1, 2, 3, 4, 5, 6, 7]
        NO = 4
        ost = [bigb[:, i * 512:(i + 1) * 512] for i in range(NO)]
        ostres = [Res("ost%d" % i) for i in range(NO)]
        ostch = [S.chan("o%d" % i) for i in range(NO)]
        ostf = bigb[0:16, 4 * 512:4 * 512 + T]
        ostfres = Res("ostf")
        stt_ = {"n": 0}

        def epi_out(rowbase):
            def epi(c0, cw, half, ps, psres):
                i = stt_["n"] % NO
                stt_["n"] += 1
                evac(ost[i][:cw, :], ps, psres, [ostres[i]])
                S.dma("pool", ostch[i], out=ya[rowbase + c0:rowbase + c0 + cw,
                                               half * 512:(half + 1) * 512],
                      in_=ost[i][:cw, :], reads=[ostres[i]], is_output=True)
            return epi
        dense_fm(cx, wan, 0, 2048, D, hT, hres, epi_out(0))
        dense_fm(cx, wan, 2048, 1536, D, hT, hres, epi_out(2048))

        def epi_ad(c0, cw, half, ps, psres):
            S.op("act", lambda e: e.copy(out=ostf[:cw, half * 512:(half + 1) * 512], in_=ps),
                 reads=[psres], writes=[ostfres])
        dense_fm(cx, wan, 3584, 16, D, hT, hres, epi_ad)
        S.dma("pool", hoch, out=yad[:, :], in_=ostf, reads=[ostfres], is_output=True)
        S.finish()
        cx.close()
        return nc
    fo = [ar_f.take(T) for _ in range(2)]
    fores = [Res("fo%d" % i) for i in range(2)]
    foch = [S.chan("fo%d" % i) for i in range(2)]
    for c in range(KC):
        sl = c % 2
        S.dma("sp", xchan[sl], out=xring[sl][:], in_=x2T[c * 128:(c + 1) * 128, :],
              writes=[xres[sl]])
        S.op("dve", lambda e: e.scalar_tensor_tensor(out=fo[sl], in0=xring[sl][:],
                                                   scalar=nwt[:, 2 * KC + c:2 * KC + c + 1],
                                                   in1=rstd[:, :], op0=ALU.mult, op1=ALU.mult),
             reads=[xres[sl], cstres, rstdres], writes=[fores[sl]])
        S.dma("sp", foch[sl], out=xnT[c * 128:(c + 1) * 128, :], in_=fo[sl], reads=[fores[sl]],
              is_output=True)
    S.finish()
    cx.close()
    return nc


def gla_phase(cx, qgT, qgres, kgT, kgres, kg_d, vg_d, ad_d, wup, wupres, ogT, ogres, ar_b, ar_f,
              cstf, cstres, chans):
    nc, S = cx.nc, cx.S
    NCHK = 128
    OWN0 = NCHK - T // CH
    k4 = [ar_b.take(2048)[0:64, :].rearrange("p (a b) -> p a b", a=4) for _ in range(2)]
    v4 = [ar_b.take(4096)[0:64, :].rearrange("p (a b) -> p a b", a=4) for _ in range(2)]
    kres4 = [Res("k4_%d" % i) for i in range(2)]
    vres4 = [Res("v4_%d" % i) for i in range(2)]
    adres4 = [Res("ad4_%d" % i) for i in range(2)]
    kch = [S.chan("k4_%d" % i) for i in range(2)]
    vch = [S.chan("v4_%d" % i) for i in range(2)]
    adch = [S.chan("ad4_%d" % i) for i in range(2)]
    kdec = [ar_b.take(512)[0:64, :] for _ in range(2)]
    kdres = [Res("kdec%d" % i) for i in range(2)]
    qin = [ar_b.take(256).rearrange("p (a b) -> p a b", a=4) for _ in range(2)]
    kin = [ar_b.take(256).rearrange("p (a b) -> p a b", a=4) for _ in range(2)]
    qkres = [Res("qk%d" % i) for i in range(2)]
    atm = [ar_b.take(256)[0:64, :].rearrange("p (a b) -> p a b", a=4) for _ in range(2)]
    atres = [Res("atm%d" % i) for i in range(2)]
    Sb = ar_b.take(1024).rearrange("p (a b) -> p a b", a=4)
    sbres = Res("Sb")
    ad4 = [ar_b.take(256)[0:17, :] for _ in range(2)]
    NL = 3
    Lr = [ar_b.take(512)[0:64, :] for _ in range(NL)]
    lres = [Res("L%d" % i) for i in range(NL)]
    et = [ar_f.take(512)[0:64, :] for _ in range(2)]
    etres = [Res("et%d" % i) for i in range(2)]
    cb = ar_b.take(136)
    cbres = Res("cb")
    wupb = ar_b.take(512)[0:17, :]
    E3 = [ar_f.take(512)[0:64, :] for _ in range(2)]
    e3res = [Res("E3_%d" % i) for i in range(2)]
    E1 = [ar_f.take(256).rearrange("p (a b) -> p a b", a=4) for _ in range(2)]
    E2 = [ar_f.take(256).rearrange("p (a b) -> p a b", a=4) for _ in range(2)]
    e12res = [Res("E12_%d" % i) for i in range(2)]
    dec = [ar_f.take(4) for _ in range(2)]
    decres = [Res("dec%d" % i) for i in range(2)]
    St = ar_f.take(1024).rearrange("p (a b) -> p a b", a=4)
    stres = [Res("S%d" % i) for i in range(4)]
    chans += kch + vch + adch
    ZB, RB, BB, OB = 0, 1, 2, 3
    KVB = [[4, 5], [6, 7]]
    S.op("dve", lambda e: e.tensor_copy(out=cb[0:64, 0:128], in_=cstf[0:64, CF_TRI16:CF_TRI16 + 128]),
         reads=[cstres], writes=[cbres])
    S.op("dve", lambda e: e.tensor_copy(out=cb[0:64, 128:129], in_=cstf[0:64, CF_NC:CF_NC + 1]),
         reads=[cstres], writes=[cbres])
    S.op("dve", lambda e: e.tensor_copy(out=wupb, in_=wup), reads=[wupres], writes=[cbres])
    tri16 = cb[0:64, 0:64]
    su16 = cb[0:64, 64:128]
    ncol = cb[0:64, 128:129]
    tri1 = cstf[0:64, CF_TRI1:CF_TRI1 + 256].rearrange("p (a b) -> p a b", a=4)
    for h in range(4):
        S.op("pool", lambda e: e.memset(St[:, h, :], 0.0), writes=[stres[h]])
    S.op("pool", lambda e: e.memset(Sb, 0.0), writes=[sbres])

    def stageA(n):
        g4, j = n // 4, n % 4
        sl = g4 % 2
        if j == 0:
            S.dma("sp", kch[sl], out=k4[sl], in_=kg_d[:, n:n + 4, :], writes=[kres4[sl]])
            S.dma("sp", vch[sl], out=v4[sl], in_=vg_d[:, n:n + 4, :], writes=[vres4[sl]])
            S.dma("sp", adch[sl], out=ad4[sl], in_=ad_d[:, n * CH:(n + 4) * CH],
                  writes=[adres4[sl]])
        S.op("pe", lambda e: e.matmul(cx.bank[ZB][0:64, :], lhsT=ad4[sl][:, j * CH:(j + 1) * CH],
                                      rhs=wupb, start=True, stop=True),
             reads=[adres4[sl], cbres], writes=[cx.bankres[ZB]])
        li = n % NL
        ei = n % 2
        S.op("act", lambda e: e.activation(out=et[ei], in_=cx.bank[ZB][0:64, :], func=AF.Exp,
                                           scale=-1.0), reads=[cx.bankres[ZB]], writes=[etres[ei]])
        S.op("act", lambda e: e.activation(out=Lr[li], in_=et[ei], func=AF.Ln, bias=1.0),
             reads=[etres[ei]], writes=[lres[li]])

    def stageB(n):
        own = n >= OWN0
        g4, j = n // 4, n % 4
        sl = g4 % 2
        li = n % NL
        p2 = n % 2
        S.op("pe", lambda e: e.matmul(cx.bank[RB][0:64, :], lhsT=su16, rhs=Lr[li], start=True,
                                      stop=True),
             reads=[cbres, lres[li]], writes=[cx.bankres[RB]])
        for h in range(4):
            if own:
                S.op("pe", lambda e: e.matmul(cx.bank[BB][:, h * 64:(h + 1) * 64],
                                              lhsT=Lr[li][:, h * 128:(h + 1) * 128], rhs=tri16,
                                              start=True, stop=True),
                     reads=[cbres, lres[li]], writes=[cx.bankres[BB]])
            else:
                S.op("pe", lambda e: e.matmul(cx.bank[BB][:, h:h + 1],
                                              lhsT=Lr[li][:, h * 128:(h + 1) * 128], rhs=ncol,
                                              start=True, stop=True),
                     reads=[cbres, lres[li]], writes=[cx.bankres[BB]])
        S.op("act", lambda e: e.activation(out=E3[p2], in_=cx.bank[RB][0:64, :], func=AF.Exp),
             reads=[cx.bankres[RB]], writes=[e3res[p2]])
        if own:
            S.op("act", lambda e: e.activation(out=E1[p2].rearrange("p a b -> p (a b)"),
                                               in_=cx.bank[BB][:, 0:256], func=AF.Exp),
                 reads=[cx.bankres[BB]], writes=[e12res[p2]])
            S.op("act", lambda e: e.activation(out=E2[p2].rearrange("p a b -> p (a b)"),
                                               in_=cx.bank[BB][:, 0:256], func=AF.Exp, scale=-1.0),
                 reads=[cx.bankres[BB]], writes=[e12res[p2]])
            S.op("act", lambda e: e.copy(out=dec[p2], in_=E1[p2][:, :, 63]),
                 reads=[e12res[p2]], writes=[decres[p2]])
            t0 = (n - OWN0) * CH
            S.op("dve", lambda e: e.scalar_tensor_tensor(
                out=qin[p2], in0=qgT[:, :, t0:t0 + CH], scalar=float(GDK) ** -0.5, in1=E1[p2],
                op0=ALU.mult, op1=ALU.mult), reads=[qgres, e12res[p2]], writes=[qkres[p2]])
            S.op("dve", lambda e: e.tensor_tensor(out=kin[p2], in0=kgT[:, :, t0:t0 + CH],
                                                  in1=E2[p2], op=ALU.mult),
                 reads=[kgres, e12res[p2]], writes=[qkres[p2]])
        else:
            S.op("act", lambda e: e.activation(out=dec[p2], in_=cx.bank[BB][:, 0:4], func=AF.Exp),
                 reads=[cx.bankres[BB]], writes=[decres[p2]])
        S.op("dve", lambda e: e.tensor_tensor(out=kdec[p2], in0=k4[sl][:, j, :], in1=E3[p2],
                                              op=ALU.mult),
             reads=[kres4[sl], e3res[p2]], writes=[kdres[p2]])

    def stageC(n):
        own = n >= OWN0
        g4, j = n // 4, n % 4
        sl = g4 % 2
        p2 = n % 2
        if own:
            t0 = (n - OWN0) * CH
            for h in range(4):
                S.op("pe", lambda e: e.matmul(cx.bank[BB][0:64, 256 + h * 64:256 + (h + 1) * 64],
                                              lhsT=kin[p2][:, h, :], rhs=qin[p2][:, h, :],
                                              start=True, stop=True),
                     reads=[qkres[p2]], writes=[cx.bankres[BB]])
            S.op("dve", lambda e: e.tensor_tensor(
                out=atm[p2], in0=cx.bank[BB][0:64, 256:512].rearrange("p (a b) -> p a b", a=4),
                in1=tri1, op=ALU.mult), reads=[cx.bankres[BB], cstres], writes=[atres[p2]])
            for h in range(4):
                for half in range(2):
                    col = (h * 2 + half) * 64
                    S.op("pe", lambda e: e.matmul(
                        cx.bank[OB][:, col:col + 64],
                        lhsT=v4[sl][:, j, h * 256 + half * 128:h * 256 + (half + 1) * 128],
                        rhs=atm[p2][:, h, :], start=True, stop=False),
                        reads=[vres4[sl], atres[p2]], writes=[cx.bankres[OB]])
                    S.op("pe", lambda e: e.matmul(
                        cx.bank[OB][:, col:col + 64], lhsT=Sb[:, h, half * 128:(half + 1) * 128],
                        rhs=qin[p2][:, h, :], start=False, stop=True),
                        reads=[sbres, qkres[p2]], writes=[cx.bankres[OB]])
            S.op("act", lambda e: e.copy(out=ogT[:, :, t0:t0 + CH],
                                         in_=cx.bank[OB][:, :].rearrange("p (a b) -> p a b", a=8)),
                 reads=[cx.bankres[OB]], writes=[ogres])
        kb = KVB[n % 2]
        for h in range(4):
            b = kb[h // 2]
            S.op("pe", lambda e: e.matmul(cx.bank[b][:, (h % 2) * 256:(h % 2) * 256 + 256],
                                          lhsT=kdec[p2][:, h * 128:(h + 1) * 128],
                                          rhs=v4[sl][:, j, h * 256:(h + 1) * 256],
                                          start=True, stop=True),
                 reads=[kdres[p2], vres4[sl]], writes=[cx.bankres[b]])
        for h in range(4):
            b = kb[h // 2]
            S.op("dve", lambda e: e.scalar_tensor_tensor(
                out=St[:, h, :], in0=St[:, h, :], scalar=dec[p2][:, h:h + 1],
                in1=cx.bank[b][:, (h % 2) * 256:(h % 2) * 256 + 256], op0=ALU.mult, op1=ALU.add),
                reads=[stres[h], decres[p2], cx.bankres[b]], writes=[stres[h]])
        if n >= OWN0 - 1 and n < NCHK - 1:
            S.op("act", lambda e: e.copy(out=Sb.rearrange("p a b -> p (a b)"),
                                         in_=St.rearrange("p a b -> p (a b)")),
                 reads=stres, writes=[sbres])

    for i in range(-2, NCHK):
        if i + 2 < NCHK:
            stageA(i + 2)
        if 0 <= i + 1 < NCHK:
            stageB(i + 1)
        if i >= 0:
            stageC(i)


_PROGS = {}


def _prog(name):
    if name not in _PROGS:
        if name == "A":
            _PROGS[name] = build_A()
        else:
            _PROGS[name] = build_B(tail=(name == "Bt"))
    return _PROGS[name]


def _fm(w):
    w = np.asarray(w, np.float32)
    return np.ascontiguousarray(w.reshape(-1, 128).T)


def _slots(a, c):
    pad = np.zeros((7 * T,) + a.shape[1:], a.dtype)
    return np.concatenate([pad, a], 0)[c * T:c * T + SEQ]


def _wa_cols(wl):
    return np.ascontiguousarray(np.concatenate(
        [wl[:, C_KA:C_QG], wl[:, C_KG:C_RG], wl[:, C_AD:C_B]], 1))


def kernel(x, norm1_w, w_in, gla_w_up, gla_b, gla_norm_w, conv_w, w_br_attn, w_br_gla, w_br_conv,
           w_out, norm2_w, w_ffn_gate, w_ffn_up, w_ffn_down, final_norm_w):
    f32 = np.float32
    bf = ml_dtypes.bfloat16
    x = np.asarray(x, f32)
    xcur = x[0]
    cst = host_consts()
    msk = host_masks()
    cores = list(range(NCORES))
    in_a = [{"xT": np.ascontiguousarray(xcur[c * T:(c + 1) * T].T), "nw": _fm(norm1_w[0]),
             "wa": _wa_cols(np.asarray(w_in[0], f32))} for c in cores]
    prev = run_bass_kernel_spmd(_prog("A"), in_a, core_ids=cores).results
    xn = None
    for l in range(DEPTH):
        last = l == DEPTH - 1
        wl = np.asarray(w_in[l], f32)
        ya = np.concatenate([np.asarray(r["ya"]).T for r in prev], 0)
        ad = np.concatenate([np.asarray(r["yad"]).T for r in prev], 0)
        hts = [np.asarray(r["hTo"]) for r in prev]
        k_a, v_a = ya[:, 0:1024], ya[:, 1024:2048]
        k_g, v_g = ya[:, 2048:2560], ya[:, 2560:3584]
        wbm = np.ascontiguousarray(np.concatenate(
            [wl[:, C_QA:C_KA], wl[:, C_QG:C_KG], wl[:, C_RG:C_AD], wl[:, C_B:C_M], wl[:, C_M:]], 1))
        nxt = norm1_w[l + 1] if not last else final_norm_w
        nws = np.ascontiguousarray(np.concatenate(
            [_fm(nxt), _fm(norm2_w[l]), _fm(final_norm_w)], 1))
        wup = np.ascontiguousarray(np.concatenate(
            [np.asarray(gla_w_up[l], f32), np.asarray(gla_b[l], f32)[None, :]], 0))
        gnw = _fm(gla_norm_w[l])
        cw = np.ascontiguousarray(np.asarray(conv_w[l], f32).T.reshape(8, 128, 3).transpose(1, 0, 2))
        shared = {"nws": nws, "wb": wbm, "cst": cst, "msk": msk, "wup": wup, "gnw": gnw, "cw": cw,
                  "wbr0": np.asarray(w_br_attn[l], f32), "wbr1": np.asarray(w_br_gla[l], f32),
                  "wbr2": np.asarray(w_br_conv[l], f32), "w_out": np.asarray(w_out[l], f32),
                  "w_g": np.asarray(w_ffn_gate[l], f32), "w_u": np.asarray(w_ffn_up[l], f32),
                  "w_d": np.asarray(w_ffn_down[l], f32)}
        if not last:
            shared["wan"] = _wa_cols(np.asarray(w_in[l + 1], f32))
        in_b = []
        for c in cores:
            ks = _slots(k_a, c).reshape(SEQ, HEADS, DH)
            vs = _slots(v_a, c).reshape(SEQ, HEADS, DH)
            vaug = np.concatenate([vs, np.ones((SEQ, HEADS, 1), bf)], -1)
            kgs = _slots(k_g, c)
            vgs = _slots(v_g, c)
            ads = _slots(ad, c)
            blk = np.zeros((NBLK,), f32)
            blk[:max(0, 28 - 4 * c)] = NEG
            hh = np.zeros((128, KC, 2), bf)
            if c > 0:
                hh = np.ascontiguousarray(hts[c - 1][:, :, T - 2:T])
            m = dict(shared)
            m.update({
                "xT": np.ascontiguousarray(xcur[c * T:(c + 1) * T].T), "hTi": hts[c], "hhi": hh,
                "blk": np.ascontiguousarray(np.tile(blk, 8)[None, :].repeat(128, 0)),
                "KT": np.ascontiguousarray(ks.transpose(1, 2, 0)),
                "VA": np.ascontiguousarray(vaug.reshape(64, 128, HEADS, DH + 1).transpose(2, 1, 0, 3)),
                "kgt": np.ascontiguousarray(kgs.reshape(128, CH, 512).transpose(1, 0, 2)),
                "vgt": np.ascontiguousarray(vgs.reshape(128, CH, 1024).transpose(1, 0, 2)),
                "adT": np.ascontiguousarray(np.concatenate([ads.T, np.ones((1, SEQ), bf)], 0)),
                "kgT": np.ascontiguousarray(
                    k_g[c * T:(c + 1) * T].T.reshape(GH, 128, T).transpose(1, 0, 2)),
            })
            in_b.append(m)
        prev = run_bass_kernel_spmd(_prog("Bf" if last else "Bt"), in_b, core_ids=cores).results
        if last:
            xn = np.concatenate([np.asarray(r["xnT"]).T for r in prev], 0)
        else:
            xcur = np.concatenate([np.asarray(r["x2T"]).T for r in prev], 0)
    return np.ascontiguousarray(xn[None].astype(f32))
```
